# Optimizing a Trainium2 kernel written in Bass

```python
import numpy as np
import jax
import jax.numpy as jnp
from jax import lax

D_MODEL = 1024
BATCH = 16
SEQ = 2048
DEPTH = 2

CTX_LEN = 256
GRID_W = 64
N_EVEN = (DEPTH + 1) // 2
N_ODD = DEPTH // 2
RMS_EPS = 1e-6
LN_EPS = 1e-5
ADA_CHUNKS = 6

GLA_HEADS = 4
GLA_DK = 64
GLA_DV = 128
GLA_LOWRANK = 16
GLA_GATE_NORM = 16.0
GLA_CHUNK = 64
ROPE_BASE = 10000.0
GLA_QK = GLA_HEADS * GLA_DK
GLA_V = GLA_HEADS * GLA_DV

CONV_CH = 512
CONV_WIDTH = 31

AB_SPLIT = (GLA_QK, GLA_QK, GLA_V, GLA_V, GLA_LOWRANK, GLA_LOWRANK, CONV_CH, CONV_CH)
AB_IN = sum(AB_SPLIT)
AB_MIX = GLA_V + CONV_CH

NA_HEADS = 16
NA_DH = 64
NA_WIN_ROWS = 8
NA_WIN_COLS = 16
NA_QCOLS = 16
NA_KCOLS = 32

N_EXPERTS = 256
TOP_K = 8
N_GROUPS = 8
TOPK_GROUPS = 4
D_EXPERT = 256
D_SHARED = 256
ROUTE_SCALE = 2.5
MOE_BLOCK = 128

kernel_name = 'hybrid_gla_conformer_natten_moe_dit'


def rms_norm(h, g):
    hf = h.astype(jnp.float32)
    hf = hf * lax.rsqrt(jnp.mean(hf * hf, axis=-1, keepdims=True) + RMS_EPS)
    return (hf * g.astype(jnp.float32)).astype(h.dtype)


def layer_norm(h, g, b):
    hf = h.astype(jnp.float32)
    mu = jnp.mean(hf, axis=-1, keepdims=True)
    var = jnp.mean(jnp.square(hf - mu), axis=-1, keepdims=True)
    out = (hf - mu) * lax.rsqrt(var + LN_EPS) * g.astype(jnp.float32) + b.astype(jnp.float32)
    return out.astype(h.dtype)


def axial_rope(t, rows, cols):
    half = t.shape[-1] // 2
    nf = half // 2
    inv = ROPE_BASE ** (-jnp.arange(nf, dtype=jnp.float32) / nf)

    def rot(u, pos):
        ang = pos[:, None] * inv[None, :]
        cos, sin = jnp.cos(ang), jnp.sin(ang)
        u1, u2 = u[..., :nf], u[..., nf:]
        return jnp.concatenate([u1 * cos - u2 * sin, u1 * sin + u2 * cos], axis=-1)

    return jnp.concatenate([rot(t[..., :half], rows), rot(t[..., half:], cols)], axis=-1)


def gla_chunked(q, k, v, logd, s0):
    B, H, T, dk = q.shape
    dv = v.shape[-1]
    L = GLA_CHUNK
    n = T // L
    q, k, logd = (t.reshape(B, H, n, L, dk) for t in (q, k, logd))
    v = v.reshape(B, H, n, L, dv)
    b = jnp.cumsum(logd, axis=3)
    b_mid = b[:, :, :, L // 2 - 1:L // 2, :]
    qe = q * jnp.exp(b - b_mid)
    ke = k * jnp.exp(b_mid - b)
    a = jnp.einsum('bhntk,bhnsk->bhnts', qe, ke)
    lower = jnp.tril(jnp.ones((L, L), dtype=bool))
    a = jnp.where(lower, a, 0.0)
    o_intra = jnp.einsum('bhnts,bhnsv->bhntv', a, v)
    b_last = b[:, :, :, -1:, :]
    q_in = q * jnp.exp(b)
    k_out = k * jnp.exp(b_last - b)
    dec = jnp.exp(b_last[:, :, :, 0, :])

    def step(s, inp):
        qi, ko, vi, di = inp
        o = jnp.einsum('bhtk,bhkv->bhtv', qi, s)
        s = s * di[..., None] + jnp.einsum('bhtk,bhtv->bhkv', ko, vi)
        return s, o

    mv = lambda t: jnp.moveaxis(t, 2, 0)
    s_fin, o_inter = lax.scan(step, s0, (mv(q_in), mv(k_out), mv(v), mv(dec)))
    o = o_intra + jnp.moveaxis(o_inter, 0, 2)
    return o.reshape(B, H, T, dv), s_fin


def depthwise_conv(u, w, b):
    ch = u.shape[-1]
    out = lax.conv_general_dilated(
        u, w[:, None, :].astype(u.dtype), window_strides=(1,),
        padding=[(CONV_WIDTH // 2, CONV_WIDTH // 2)],
        dimension_numbers=('NWC', 'WIO', 'NWC'), feature_group_count=ch)
    return out + b


def gla_conv_project(h, w_in, dw_f, db_f, dw_b, db_b):
    B, T, _ = h.shape
    z = h @ w_in
    q, k, v, g, lr_f, lr_b, glu_a, glu_b = jnp.split(z, list(np.cumsum(AB_SPLIT)[:-1]), axis=-1)
    heads = lambda t, d: t.reshape(B, T, GLA_HEADS, d).transpose(0, 2, 1, 3).astype(jnp.float32)
    q = heads(q, GLA_DK) * (GLA_DK ** -0.5)
    k = heads(k, GLA_DK)
    v = heads(v, GLA_DV)
    logd_f = heads(jax.nn.log_sigmoid((lr_f @ dw_f + db_f).astype(jnp.float32)) / GLA_GATE_NORM, GLA_DK)
    logd_b = heads(jax.nn.log_sigmoid((lr_b @ dw_b + db_b).astype(jnp.float32)) / GLA_GATE_NORM, GLA_DK)
    u = glu_a * jax.nn.sigmoid(glu_b)
    return q, k, v, logd_f, logd_b, g, u


def ab_mixer(hx, hc, w_in, w_out, dw_f, db_f, dw_b, db_b, gla_g, conv_w, conv_b, ln_g, ln_b):
    B, S, _ = hx.shape
    qx, kx, vx, lfx, lbx, gx, ux = gla_conv_project(hx, w_in, dw_f, db_f, dw_b, db_b)
    qc, kc, vc, lfc, lbc, gc, uc = gla_conv_project(hc, w_in, dw_f, db_f, dw_b, db_b)
    t = jnp.arange(S)
    rows = (t // GRID_W).astype(jnp.float32)
    cols = (t % GRID_W).astype(jnp.float32)
    qx = axial_rope(qx, rows, cols)
    kx = axial_rope(kx, rows, cols)
    zero = jnp.zeros((B, GLA_HEADS, GLA_DK, GLA_DV), jnp.float32)
    fl = lambda a: jnp.flip(a, axis=2)
    oc_f, sc_f = gla_chunked(qc, kc, vc, lfc, zero)
    ox_f, _ = gla_chunked(qx, kx, vx, lfx, sc_f)
    oc_b, sc_b = gla_chunked(fl(qc), fl(kc), fl(vc), fl(lbc), zero)
    ox_b, _ = gla_chunked(fl(qx), fl(kx), fl(vx), fl(lbx), sc_b)

    def finish(o, g, u):
        T = o.shape[2]
        o = o * lax.rsqrt(jnp.mean(o * o, axis=-1, keepdims=True) + RMS_EPS)
        o = o.transpose(0, 2, 1, 3) * gla_g.astype(jnp.float32).reshape(GLA_HEADS, GLA_DV)
        a_out = o.reshape(B, T, GLA_V).astype(g.dtype) * jax.nn.silu(g)
        cv = depthwise_conv(u, conv_w, conv_b)
        cv = jax.nn.silu(layer_norm(cv, ln_g, ln_b))
        return jnp.concatenate([a_out, cv], axis=-1) @ w_out

    return finish(ox_f + fl(ox_b), gx, ux), finish(oc_f + fl(oc_b), gc, uc)


def na_mixer(hx, hc, w_in, w_out, rpb, need_ctx):
    B, S, _ = hx.shape
    C = hc.shape[1]
    n_rows = S // GRID_W
    wr = min(NA_WIN_ROWS, n_rows)
    H, dh = NA_HEADS, NA_DH
    scale = dh ** -0.5

    def qkv(h):
        T = h.shape[1]
        z = (h @ w_in).reshape(B, T, 3, H, dh)
        return [z[:, :, j].transpose(0, 2, 1, 3) for j in range(3)]

    q, k, v = qkv(hx)
    qc, kc, vc = qkv(hc)

    n_cb = GRID_W // NA_QCOLS
    qcols = np.arange(GRID_W).reshape(n_cb, NA_QCOLS)
    cstart = np.clip(qcols - NA_WIN_COLS // 2, 0, GRID_W - NA_WIN_COLS)
    band = np.minimum(cstart[:, 0], GRID_W - NA_KCOLS)
    kcols = band[:, None] + np.arange(NA_KCOLS)[None, :]
    col_ok = (kcols[:, None, :] >= cstart[:, :, None]) & (kcols[:, None, :] < cstart[:, :, None] + NA_WIN_COLS)
    col_idx = np.clip(kcols[:, None, :] - qcols[:, :, None] + NA_WIN_COLS - 1, 0, 2 * NA_WIN_COLS - 2)
    rpb_c = rpb[:, :, col_idx]
    nw = wr * NA_KCOLS
    mask = jnp.asarray(np.repeat(col_ok[:, :, None, :], wr, axis=2).reshape(n_cb, NA_QCOLS, nw))

    qg = q.reshape(B, H, n_rows, GRID_W, dh)
    kg = k.reshape(B, H, n_rows, GRID_W, dh)
    vg = v.reshape(B, H, n_rows, GRID_W, dh)

    def row_block(r):
        r0 = jnp.clip(r - wr // 2, 0, n_rows - wr)
        qb = lax.dynamic_index_in_dim(qg, r, axis=2, keepdims=False).reshape(B, H, n_cb, NA_QCOLS, dh)

        def band_of(t):
            t = lax.dynamic_slice_in_dim(t, r0, wr, axis=2)[:, :, :, kcols]
            return t.transpose(0, 1, 3, 2, 4, 5).reshape(B, H, n_cb, nw, dh)

        kb, vb = band_of(kg), band_of(vg)
        ridx = r0 + jnp.arange(wr) - r + NA_WIN_ROWS - 1
        bias = rpb_c[:, ridx].transpose(0, 2, 3, 1, 4).reshape(H, n_cb, NA_QCOLS, nw)
        s_win = jnp.einsum('bhcqd,bhckd->bhcqk', qb, kb).astype(jnp.float32) * scale + bias.astype(jnp.float32)
        s_win = jnp.where(mask, s_win, -jnp.inf)
        s_ctx = jnp.einsum('bhcqd,bhkd->bhcqk', qb, kc).astype(jnp.float32) * scale
        p = jax.nn.softmax(jnp.concatenate([s_win, s_ctx], axis=-1), axis=-1).astype(vb.dtype)
        o = (jnp.einsum('bhcqk,bhckd->bhcqd', p[..., :nw], vb)
             + jnp.einsum('bhcqk,bhkd->bhcqd', p[..., nw:], vc))
        return o.reshape(B, H, GRID_W, dh)

    o = lax.map(row_block, jnp.arange(n_rows))
    yx = o.transpose(1, 0, 3, 2, 4).reshape(B, S, H * dh) @ w_out
    yc = None
    if need_ctx:
        sc = jnp.einsum('bhqd,bhkd->bhqk', qc, kc).astype(jnp.float32) * scale
        oc = jnp.einsum('bhqk,bhkd->bhqd', jax.nn.softmax(sc, axis=-1).astype(vc.dtype), vc)
        yc = oc.transpose(0, 2, 1, 3).reshape(B, C, H * dh) @ w_out
    return yx, yc


def moe_ffn(h, router_w, router_b, w_gate, w_up, w_down, ws_gate, ws_up, ws_down):
    N, D = h.shape
    E = w_gate.shape[0]
    scores = jax.nn.sigmoid(h.astype(jnp.float32) @ router_w.astype(jnp.float32))
    biased = scores + router_b.astype(jnp.float32)
    grp = biased.reshape(N, N_GROUPS, E // N_GROUPS)
    grp_score = jnp.sum(lax.top_k(grp, 2)[0], axis=-1)
    _, grp_idx = lax.top_k(grp_score, TOPK_GROUPS)
    grp_keep = jnp.sum(jax.nn.one_hot(grp_idx, N_GROUPS, dtype=jnp.float32), axis=1) > 0
    masked = jnp.where(grp_keep[:, :, None], grp, -jnp.inf).reshape(N, E)
    _, e_idx = lax.top_k(masked, TOP_K)
    gates = jnp.take_along_axis(scores, e_idx, axis=-1)
    gates = gates / jnp.sum(gates, axis=-1, keepdims=True) * ROUTE_SCALE

    y0 = ((jax.nn.silu(h @ ws_gate) * (h @ ws_up)) @ ws_down).astype(jnp.float32)

    A = N * TOP_K
    flat_e = e_idx.reshape(-1)
    flat_tok = jnp.repeat(jnp.arange(N, dtype=jnp.int32), TOP_K)
    flat_w = gates.reshape(-1)
    order = jnp.argsort(flat_e)
    se, stok, sw = flat_e[order], flat_tok[order], flat_w[order]
    counts = jnp.bincount(flat_e, length=E)
    starts = jnp.cumsum(counts) - counts
    padded = (counts + MOE_BLOCK - 1) // MOE_BLOCK * MOE_BLOCK
    pend = jnp.cumsum(padded)
    pstart = pend - padded
    pos = pstart[se] + jnp.arange(A, dtype=jnp.int32) - starts[se]
    P = -(-(A + E * (MOE_BLOCK - 1)) // MOE_BLOCK) * MOE_BLOCK
    nb = P // MOE_BLOCK
    slot_tok = jnp.zeros((P,), jnp.int32).at[pos].set(stok)
    slot_w = jnp.zeros((P,), jnp.float32).at[pos].set(sw)
    block_e = jnp.minimum(jnp.searchsorted(pend, jnp.arange(nb, dtype=jnp.int32) * MOE_BLOCK, side='right'), E - 1)

    def add_block(y, blk):
        tok, wt, e = blk
        xb = h[tok]
        hid = jax.nn.silu(xb @ w_gate[e]) * (xb @ w_up[e])
        out = (hid @ w_down[e]).astype(jnp.float32) * wt[:, None]
        return y.at[tok].add(out), None

    y, _ = lax.scan(add_block, y0, (slot_tok.reshape(nb, MOE_BLOCK), slot_w.reshape(nb, MOE_BLOCK), block_e))
    return y.astype(h.dtype)


def setup_inputs(seed: int = 0) -> dict:
    key = jax.random.key(seed)
    ks = jax.random.split(key, 40)
    D = D_MODEL

    def nrm(i, shape, std):
        return std * jax.random.normal(ks[i], shape, jnp.float32)

    def gain(i, shape):
        return 1.0 + nrm(i, shape, 0.02)

    return {
        'x': nrm(0, (BATCH, SEQ, D), 1.0),
        'c': nrm(1, (BATCH, D), 1.0),
        'ctx': nrm(2, (BATCH, CTX_LEN, D), 1.0),
        'c_ctx': nrm(3, (D,), 1.0),
        'ada_w': nrm(4, (DEPTH, D, ADA_CHUNKS * D), 0.2 * D ** -0.5),
        'ada_b': nrm(5, (DEPTH, ADA_CHUNKS * D), 0.02),
        'g_pre_mix': gain(6, (DEPTH, D)),
        'g_post_mix': gain(7, (DEPTH, D)),
        'g_pre_ffn': gain(8, (DEPTH, D)),
        'g_post_ffn': gain(9, (DEPTH, D)),
        'ab_w_in': nrm(10, (N_EVEN, D, AB_IN), D ** -0.5),
        'ab_w_out': nrm(11, (N_EVEN, AB_MIX, D), AB_MIX ** -0.5),
        'gla_dw_f': nrm(12, (N_EVEN, GLA_LOWRANK, GLA_QK), GLA_LOWRANK ** -0.5),
        'gla_db_f': nrm(13, (N_EVEN, GLA_QK), 0.1),
        'gla_dw_b': nrm(14, (N_EVEN, GLA_LOWRANK, GLA_QK), GLA_LOWRANK ** -0.5),
        'gla_db_b': nrm(15, (N_EVEN, GLA_QK), 0.1),
        'gla_norm_g': gain(16, (N_EVEN, GLA_V)),
        'conv_w': nrm(17, (N_EVEN, CONV_WIDTH, CONV_CH), CONV_WIDTH ** -0.5),
        'conv_b': nrm(18, (N_EVEN, CONV_CH), 0.02),
        'conv_ln_g': gain(19, (N_EVEN, CONV_CH)),
        'conv_ln_b': nrm(20, (N_EVEN, CONV_CH), 0.02),
        'na_w_in': nrm(21, (N_ODD, D, 3 * NA_HEADS * NA_DH), D ** -0.5),
        'na_w_out': nrm(22, (N_ODD, NA_HEADS * NA_DH, D), (NA_HEADS * NA_DH) ** -0.5),
        'na_rpb': nrm(23, (N_ODD, NA_HEADS, 2 * NA_WIN_ROWS - 1, 2 * NA_WIN_COLS - 1), 0.1),
        'moe_router_w': nrm(24, (DEPTH, D, N_EXPERTS), D ** -0.5),
        'moe_router_b': nrm(25, (DEPTH, N_EXPERTS), 0.01),
        'moe_w_gate': nrm(26, (DEPTH, N_EXPERTS, D, D_EXPERT), D ** -0.5),
        'moe_w_up': nrm(27, (DEPTH, N_EXPERTS, D, D_EXPERT), D ** -0.5),
        'moe_w_down': nrm(28, (DEPTH, N_EXPERTS, D_EXPERT, D), D_EXPERT ** -0.5),
        'moe_ws_gate': nrm(29, (DEPTH, D, D_SHARED), D ** -0.5),
        'moe_ws_up': nrm(30, (DEPTH, D, D_SHARED), D ** -0.5),
        'moe_ws_down': nrm(31, (DEPTH, D_SHARED, D), D_SHARED ** -0.5),
    }


def reference(x, c, ctx, c_ctx, ada_w, ada_b, g_pre_mix, g_post_mix, g_pre_ffn, g_post_ffn,
              ab_w_in, ab_w_out, gla_dw_f, gla_db_f, gla_dw_b, gla_db_b, gla_norm_g,
              conv_w, conv_b, conv_ln_g, conv_ln_b, na_w_in, na_w_out, na_rpb,
              moe_router_w, moe_router_b, moe_w_gate, moe_w_up, moe_w_down,
              moe_ws_gate, moe_ws_up, moe_ws_down):
    B, S, D = x.shape
    C = ctx.shape[1]
    for layer in range(DEPTH):
        last = layer == DEPTH - 1
        i = layer // 2
        mod_x = jax.nn.silu(c) @ ada_w[layer] + ada_b[layer]
        mod_c = jax.nn.silu(c_ctx) @ ada_w[layer] + ada_b[layer]
        shx1, scx1, gx1, shx2, scx2, gx2 = jnp.split(mod_x[:, None, :], ADA_CHUNKS, axis=-1)
        shc1, scc1, gc1, shc2, scc2, gc2 = jnp.split(mod_c[None, None, :], ADA_CHUNKS, axis=-1)

        hx = rms_norm(x, g_pre_mix[layer]) * (1.0 + scx1) + shx1
        hc = rms_norm(ctx, g_pre_mix[layer]) * (1.0 + scc1) + shc1
        if layer % 2 == 0:
            yx, yc = ab_mixer(hx, hc, ab_w_in[i], ab_w_out[i], gla_dw_f[i], gla_db_f[i], gla_dw_b[i], gla_db_b[i],
                              gla_norm_g[i], conv_w[i], conv_b[i], conv_ln_g[i], conv_ln_b[i])
        else:
            yx, yc = na_mixer(hx, hc, na_w_in[i], na_w_out[i], na_rpb[i], not last)
        x = x + gx1 * rms_norm(yx, g_post_mix[layer])
        if not last:
            ctx = ctx + gc1 * rms_norm(yc, g_post_mix[layer])

        hx = (rms_norm(x, g_pre_ffn[layer]) * (1.0 + scx2) + shx2).reshape(B * S, D)
        if last:
            tokens = hx
        else:
            hc = (rms_norm(ctx, g_pre_ffn[layer]) * (1.0 + scc2) + shc2).reshape(B * C, D)
            tokens = jnp.concatenate([hx, hc], axis=0)
        yt = moe_ffn(tokens, moe_router_w[layer], moe_router_b[layer], moe_w_gate[layer], moe_w_up[layer],
                     moe_w_down[layer], moe_ws_gate[layer], moe_ws_up[layer], moe_ws_down[layer])
        x = x + gx2 * rms_norm(yt[:B * S].reshape(B, S, D), g_post_ffn[layer])
        if not last:
            ctx = ctx + gc2 * rms_norm(yt[B * S:].reshape(B, C, D), g_post_ffn[layer])
    return x
```

```python
from contextlib import ExitStack
import os
DBG = int(os.environ.get('GLA_DBG', '9'))
import numpy as np
import concourse.bass as bass
import concourse.mybir as mybir
from concourse.bass_utils import run_bass_kernel_spmd

F32 = mybir.dt.float32
BF16 = mybir.dt.bfloat16
AF = mybir.ActivationFunctionType
ALU = mybir.AluOpType
AX = mybir.AxisListType

D = 1024
S = 2048
C = 256
T = S + C
NT = T // 128
NBL = 2
E = 256
RMS_EPS = 1e-6
LN_EPS = 1e-5
NEG = -30000.0


class Buf:
    __slots__ = ("name", "w", "r", "dsem")

    def __init__(self, name):
        self.name = name
        self.w = {}
        self.r = {}
        self.dsem = None


class KB:
    def __init__(self, nc):
        self.nc = nc
        self.E = {"pe": nc.tensor, "dve": nc.vector, "act": nc.scalar, "pool": nc.gpsimd, "sp": nc.sync}
        self.esem = {e: nc.alloc_semaphore("es_" + e) for e in self.E}
        self.ecnt = {e: 0 for e in self.E}
        self.seen = {e: {} for e in self.E}
        self.dcnt = {}
        self.dpool = []
        self.dnext = 0
        self.nbuf = 0
        self.allbufs = []

    def buf(self, name=None):
        self.nbuf += 1
        b = Buf("%s_%d" % (name or "b", self.nbuf))
        self.allbufs.append(b)
        return b

    def bufs(self, n, name="b"):
        return [self.buf(name) for _ in range(n)]

    def _wait(self, eng, deps):
        for key, (sem, val, src) in deps.items():
            if src == "pe" and eng == "pe":
                continue
            if src == "dma":
                val = self.dcnt[key[2:]]
            if self.seen[eng].get(key, 0) >= val:
                continue
            self.E[eng].wait_ge(sem, val)
            self.seen[eng][key] = val

    @staticmethod
    def _deps(reads, writes):
        deps = {}
        for b in reads:
            for k, t in b.w.items():
                if k not in deps or deps[k][1] < t[1]:
                    deps[k] = t
        for b in writes:
            for d in (b.w, b.r):
                for k, t in d.items():
                    if k not in deps or deps[k][1] < t[1]:
                        deps[k] = t
        return deps

    @staticmethod
    def _mark(tok, key, reads, writes):
        for b in writes:
            b.w = {key: tok}
            b.r = {}
        for b in reads:
            if b in writes:
                continue
            b.r[key] = tok

    def op(self, eng, fn, reads=(), writes=()):
        self._wait(eng, self._deps(reads, writes))
        ins = fn(self.E[eng])
        self.ecnt[eng] += 1
        ins.then_inc(self.esem[eng], 1)
        self._mark((self.esem[eng], self.ecnt[eng], eng), "e_" + eng, reads, writes)
        return ins

    def dma(self, q, out, in_, reads=(), writes=(), **kw):
        self._wait(q, self._deps(reads, writes))
        tgt = writes[0]
        if tgt.dsem is None:
            if len(self.dpool) < 72:
                nm = "ds%d" % len(self.dpool)
                self.dpool.append((self.nc.alloc_semaphore(nm), nm))
                self.dcnt[nm] = 0
            tgt.dsem = self.dpool[self.dnext % len(self.dpool)] if len(self.dpool) == 72 else self.dpool[-1]
            self.dnext += 1
        sem, nm = tgt.dsem
        ins = self.E[q].dma_start(out=out, in_=in_, **kw)
        self.dcnt[nm] += 16
        ins.then_inc(sem, 16)
        self._mark((sem, self.dcnt[nm], "dma"), "d_" + nm, reads, writes)
        return ins

    def barrier(self):
        deps = {}
        for e in self.E:
            if self.ecnt[e]:
                deps["e_" + e] = (self.esem[e], self.ecnt[e], "x")
        for (sem, nm) in self.dpool:
            deps["d_" + nm] = (sem, self.dcnt[nm], "dma")
        self.allbufs = []
        for e in self.E:
            self._wait(e, {k: v for k, v in deps.items() if k != "e_" + e})

    def mm(self, out, lhsT, rhs, start, stop, reads, writes):
        return self.op("pe", lambda e: e.matmul(out, lhsT, rhs, start=start, stop=stop), reads, writes)

    def tr(self, out, in_, ident, reads, writes):
        return self.op("pe", lambda e: e.transpose(out, in_, ident), reads, writes)

    def act(self, out, in_, func, reads, writes, **kw):
        return self.op("act", lambda e: e.activation(out, in_, func, **kw), reads, writes)

    def v(self, fn, reads, writes, eng="dve"):
        return self.op(eng, fn, reads, writes)


def blocks(t0, t1, n):
    out = []
    while t0 < t1:
        m = min(n, t1 - t0)
        out.append((t0, m))
        t0 += m
    return out


class Prog:
    def __init__(self, stop="end", experts=None, nbl=NBL):
        self.stop = stop
        self.experts = list(range(E)) if experts is None else experts
        self.nbl = nbl
        nc = self.nc = bass.Bass("TRN2", target_bir_lowering=False)
        self.k = KB(nc)
        self.es = ExitStack()
        self.build()

    def din(self, name, shape, dt=F32):
        return self.nc.dram_tensor(name, list(shape), dt, kind="ExternalInput").ap()

    def sb(self, es, name, shape, dt):
        if not hasattr(self, "arena"):
            self.AW = 52800
            self.arena = self.nc.alloc_sbuf_tensor("arena", [128, self.AW], F32).ap()
            self.free_list = [(0, self.AW)]
        esz = 2 if dt == BF16 else 4
        nfree = int(np.prod(shape[1:]))
        words = (nfree * esz + 3) // 4
        words = (words + 7) // 8 * 8
        for idx, (o, n) in enumerate(self.free_list):
            if n >= words:
                break
        else:
            raise RuntimeError("SBUF arena exhausted allocating %s %s; free=%s" % (name, shape, self.free_list))
        if n == words:
            self.free_list.pop(idx)
        else:
            self.free_list[idx] = (o + words, n - words)
        P = shape[0]
        ap = self.arena[0:P, o:o + words]
        if dt == BF16:
            ap = ap.bitcast(BF16)
        ap = ap[:, 0:nfree]
        if len(shape) == 3:
            ap = ap.rearrange("p (a b) -> p a b", a=shape[1])
        elif len(shape) == 4:
            ap = ap.rearrange("p (a b c) -> p a b c", a=shape[1], b=shape[2])
        elif len(shape) == 5:
            ap = ap.rearrange("p (a b c d) -> p a b c d", a=shape[1], b=shape[2], c=shape[3])

        def _free():
            fl = self.free_list
            fl.append((o, words))
            fl.sort()
            merged = []
            for (a, n_) in fl:
                if merged and merged[-1][0] + merged[-1][1] == a:
                    merged[-1] = (merged[-1][0], merged[-1][1] + n_)
                else:
                    merged.append((a, n_))
            self.free_list[:] = merged
        es.callback(_free)
        return ap

    def load_const(self, es, name, src, shape, dt=F32, q="sp"):
        t = self.sb(es, name, shape, dt)
        b = self.k.buf(name)
        self.k.dma(q, t, src, [], [b])
        return t, b

    def build(self):
        nc, k = self.nc, self.k
        nbl = self.nbl
        I = self.I = {}
        I["x"] = self.din("x", [nbl, S, D])
        I["ctx"] = self.din("ctx", [nbl, C, D])
        I["cvec"] = self.din("cvec", [128, 8, 4])
        I["ada_w"] = self.din("ada_w", [2, D, 6 * D])
        I["ada_bT"] = self.din("ada_bT", [2, 128, 48])
        I["gvec"] = self.din("gvec", [2, 128, 4, 8])
        I["w_qk"] = self.din("w_qk", [D, 1024])
        I["w_vg"] = self.din("w_vg", [D, 1024])
        I["w_lr"] = self.din("w_lr", [D, 32])
        I["w_glu"] = self.din("w_glu", [D, 1024])
        I["ab_w_out"] = self.din("ab_w_out", [D, D])
        I["dwb_f"] = self.din("dwb_f", [17, 256])
        I["dwb_b"] = self.din("dwb_b", [17, 256])
        I["gla_g"] = self.din("gla_g", [1, 512])
        I["convw"] = self.din("convw", [128, 4, 31])
        I["convv"] = self.din("convv", [128, 3, 4])
        I["rope"] = self.din("rope", [4, 128, S])
        I["tri"] = self.din("tri", [2, 128, 128])
        I["amask"] = self.din("amask", [2, 128, 64])
        I["ident"] = self.din("ident", [128, 128])
        I["na_w_in"] = self.din("na_w_in", [D, 3 * D])
        I["na_w_out"] = self.din("na_w_out", [D, D])
        I["na_bias"] = self.din("na_bias", [16, 64, 960])
        I["router_w"] = self.din("router_w", [2, D, E])
        I["router_b"] = self.din("router_b", [2, 1, E])
        ne = self.ne_decl = (max(self.experts) + 1) if self.experts else 1
        I["w_gate"] = self.din("w_gate", [2, ne, D, 256])
        I["w_up"] = self.din("w_up", [2, ne, D, 256])
        I["w_down"] = self.din("w_down", [2, ne, 256, D])
        I["ws_gate"] = self.din("ws_gate", [2, D, 256])
        I["ws_up"] = self.din("ws_up", [2, D, 256])
        I["ws_down"] = self.din("ws_down", [2, 256, D])
        self.out = nc.dram_tensor("out", [nbl, S, D], F32, kind="ExternalOutput").ap()
        self.xres = [nc.dram_tensor("xres%d" % b, [128, 8, T], F32).ap() for b in range(nbl)]
        self.bxres = [[k.buf("xres") for _ in range(NT)] for b in range(nbl)]
        self.bout = k.buf("out")

        self.PS = [nc.alloc_psum_tensor("ps%d" % i, [128, 512], F32).ap() for i in range(8)]
        self.bPS = k.bufs(8, "ps")
        self.PSB = self.PS[7].bitcast(BF16)

        es = self.es
        self.ident, self.bident = self.load_const(es, "ident", I["ident"], [128, 128])
        self.identb = self.sb(es, "identb", [128, 128], BF16)
        self.bidentb = k.buf("identb")
        k.v(lambda e: e.tensor_copy(self.identb, self.ident), [self.bident], [self.bidentb])
        self.ones = self.sb(es, "ones", [128, 128], F32)
        self.bones = k.buf("ones")
        k.v(lambda e: e.memset(self.ones, 1.0), [], [self.bones])

        self.compute_mods()
        for b in range(nbl):
            self.load_x(b)
            if self.stop != "load":
                self.sequence(b)
            self.dump_x(b)
        deps = dict(self.bout.w)
        k._wait("sp", deps)
        es.close()

    def compute_mods(self):
        nc, k, I = self.nc, self.k, self.I
        self.mod = []
        self.bmod = k.buf("mod")
        self.modv = self.sb(self.es, "modv", [128, 2, 6, 3, 8], F32)
        with ExitStack() as es:
            cv, bcv = self.load_const(es, "cvec", I["cvec"], [128, 8, 4])
            sc = self.sb(es, "silc", [128, 8, 4], F32)
            bsc = k.buf("silc")
            k.act(sc, cv, AF.Silu, [bcv], [bsc])
            wbuf = [self.sb(es, "adaw", [128, 8, 512], F32) for _ in range(2)]
            bw = k.bufs(2, "adaw")
            gv, bgv = self.load_const(es, "gvec", I["gvec"].rearrange("l p a c -> p l a c"), [128, 2, 4, 8])
            abt, babt = self.load_const(es, "adab", I["ada_bT"].rearrange("l p j -> p l j"), [128, 2, 48])
            modraw = self.sb(es, "modraw", [128, 2, 3, 48], F32)
            bmr = k.buf("modraw")
            ps = self.PS[0]
            bps = self.bPS[0]
            i = 0
            for l in range(2):
                for ch in range(12):
                    w = wbuf[i % 2]
                    k.dma("sp", w, I["ada_w"][l, :, ch * 512:(ch + 1) * 512].rearrange("(c p) n -> p c n", p=128),
                          [], [bw[i % 2]])
                    for jj in range(4):
                        j = ch * 4 + jj
                        for kc in range(8):
                            k.mm(ps[:, j * 4:(j + 1) * 4], w[:, kc, jj * 128:(jj + 1) * 128], sc[:, kc, :],
                                 kc == 0, kc == 7, [bw[i % 2], bsc], [bps])
                    i += 1
                for v in range(3):
                    k.v(lambda e: e.tensor_tensor(modraw[:, l, v, :],
                                                  ps[:, 0:192].rearrange("p (j v) -> p j v", v=4)[:, :, v],
                                                  abt[:, l, :], ALU.add), [bps, babt], [bmr])
            mv = self.modv
            for l in range(2):
                for v in range(3):
                    for s_ in range(2):
                        sh = modraw[:, l, v, (3 * s_ + 0) * 8:(3 * s_ + 0) * 8 + 8]
                        scl = modraw[:, l, v, (3 * s_ + 1) * 8:(3 * s_ + 1) * 8 + 8]
                        gt = modraw[:, l, v, (3 * s_ + 2) * 8:(3 * s_ + 2) * 8 + 8]
                        gpre = gv[:, l, 2 * s_ + 0, :]
                        gpost = gv[:, l, 2 * s_ + 1, :]
                        A = mv[:, l, 3 * s_ + 0, v, :]
                        k.v(lambda e: e.scalar_tensor_tensor(A, scl, 1.0, gpre, ALU.add, ALU.mult), [bmr, bgv], [self.bmod])
                        k.v(lambda e: e.tensor_copy(mv[:, l, 3 * s_ + 1, v, :], sh), [bmr], [self.bmod])
                        k.v(lambda e: e.tensor_tensor(mv[:, l, 3 * s_ + 2, v, :], gt, gpost, ALU.mult), [bmr, bgv], [self.bmod])
            k.barrier()

    def modvec(self, l, s_, kind, v):
        return self.modv[:, l, 3 * s_ + kind, v, :]

    def load_x(self, b):
        k, I = self.k, self.I
        with ExitStack() as es:
            xin = [self.sb(es, "xin", [128, D], F32) for _ in range(2)]
            bxin = k.bufs(2, "xin")
            xt = [self.sb(es, "xt", [128, 8, 128], F32) for _ in range(2)]
            bxt = k.bufs(2, "xt")
            for i in range(NT):
                src = I["x"][b, i * 128:(i + 1) * 128, :] if i < 16 else I["ctx"][b, (i - 16) * 128:(i - 15) * 128, :]
                p = i % 2
                k.dma("sp", xin[p], src, [], [bxin[p]])
                pa, pb = self.PS[2 * p], self.PS[2 * p + 1]
                for c in range(8):
                    ps = (pa if c < 4 else pb)[:, (c % 4) * 128:(c % 4 + 1) * 128]
                    k.tr(ps, xin[p][:, c * 128:(c + 1) * 128], self.ident, [bxin[p], self.bident],
                         [self.bPS[2 * p + (c // 4)]])
                k.act(xt[p][:, 0:4, :], pa.rearrange("p (c t) -> p c t", c=4), AF.Copy, [self.bPS[2 * p]], [bxt[p]])
                k.v(lambda e: e.tensor_copy(xt[p][:, 4:8, :], pb.rearrange("p (c t) -> p c t", c=4)),
                    [self.bPS[2 * p + 1]], [bxt[p]])
                k.dma("sp", self.xres[b][:, :, i * 128:(i + 1) * 128], xt[p], [bxt[p]], [self.bxres[b][i]])
            k.barrier()

    def dump_x(self, b):
        k = self.k
        with ExitStack() as es:
            xt = [self.sb(es, "dxt", [128, 8, 128], F32) for _ in range(2)]
            bxt = k.bufs(2, "dxt")
            xo = [self.sb(es, "dxo", [128, D], F32) for _ in range(2)]
            bxo = k.bufs(2, "dxo")
            for i in range(16):
                p = i % 2
                k.dma("sp", xt[p], self.xres[b][:, :, i * 128:(i + 1) * 128], [self.bxres[b][i]], [bxt[p]])
                pa, pb = self.PS[2 * p], self.PS[2 * p + 1]
                for c in range(8):
                    ps = (pa if c < 4 else pb)[:, (c % 4) * 128:(c % 4 + 1) * 128]
                    k.tr(ps, xt[p][:, c, :], self.ident, [bxt[p], self.bident], [self.bPS[2 * p + (c // 4)]])
                k.act(xo[p][:, 0:512], pa, AF.Copy, [self.bPS[2 * p]], [bxo[p]])
                k.v(lambda e: e.tensor_copy(xo[p][:, 512:1024], pb), [self.bPS[2 * p + 1]], [bxo[p]])
                k.dma("sp", self.out[b, i * 128:(i + 1) * 128, :], xo[p], [bxo[p]], [self.bout])
            k.barrier()

    def norm_mod(self, hT, bh, b, l, s_, t_end, router=None):
        k = self.k
        NBK = 256
        with ExitStack() as es:
            xb = [self.sb(es, "nx", [128, 8, NBK], F32) for _ in range(2)]
            bxb = k.bufs(2, "nx")
            sq = self.sb(es, "nsq", [128, 8, NBK], F32)
            bsq = k.buf("nsq")
            rs = self.sb(es, "nrs", [128, NBK], F32)
            brs = k.buf("nrs")
            tmp = self.sb(es, "ntmp", [128, NBK], F32)
            btmp = k.buf("ntmp")
            h32 = self.sb(es, "nh32", [128, 8, NBK], F32) if router else None
            bh32 = k.buf("nh32")
            blks = blocks(0, min(t_end, S), NBK) + (blocks(S, t_end, NBK) if t_end > S else [])
            for bi, (t0, n) in enumerate(blks):
                v = b if t0 < S else 2
                A = self.modvec(l, s_, 0, v)
                sh = self.modvec(l, s_, 1, v)
                p = bi % 2
                tiles = list(range(t0 // 128, (t0 + n) // 128))
                k.dma("sp", xb[p][:, :, 0:n], self.xres[b][:, :, t0:t0 + n], [self.bxres[b][i] for i in tiles], [bxb[p]])
                k.act(sq[:, :, 0:n], xb[p][:, :, 0:n], AF.Square, [bxb[p]], [bsq])
                ps = self.PS[6]
                for c in range(8):
                    k.mm(ps[:, 0:n], self.ones, sq[:, c, 0:n], c == 0, c == 7, [self.bones, bsq], [self.bPS[6]])
                k.act(rs[:, 0:n], ps[:, 0:n], AF.Sqrt, [self.bPS[6]], [brs], scale=1.0 / D, bias=RMS_EPS)
                k.v(lambda e: e.reciprocal(rs[:, 0:n], rs[:, 0:n]), [brs], [brs])
                for c in range(8):
                    k.v(lambda e: e.tensor_tensor(tmp[:, 0:n], xb[p][:, c, 0:n], rs[:, 0:n], ALU.mult), [bxb[p], brs], [btmp])
                    if router:
                        k.act(h32[:, c, 0:n], tmp[:, 0:n], AF.Identity, [btmp, self.bmod], [bh32],
                              scale=A[:, c:c + 1], bias=sh[:, c:c + 1])
                    else:
                        k.act(hT[:, c, t0:t0 + n], tmp[:, 0:n], AF.Identity, [btmp, self.bmod], [bh[i] for i in tiles],
                              scale=A[:, c:c + 1], bias=sh[:, c:c + 1])
                if router:
                    k.v(lambda e: e.tensor_copy(hT[:, :, t0:t0 + n], h32[:, :, 0:n]), [bh32], [bh[i] for i in tiles], eng="pool")
                    rw, brw, cb = router
                    for ti, tile in enumerate(tiles):
                        pr = self.PS[4 + ti % 2]
                        bpr = self.bPS[4 + ti % 2]
                        for c in range(8):
                            k.mm(pr[:, 0:E], h32[:, c, ti * 128:(ti + 1) * 128], rw[:, c, :], c == 0, c == 7, [bh32, brw], [bpr])
                        cb(tile, pr[:, 0:E], bpr)
            k.barrier()

    def lin_fm(self, ps, bps, W, bW, col0, M, inT, bin_, t0, n, nk=8):
        for kc in range(nk):
            self.k.mm(ps[0:M, 0:n], W[:, kc, col0:col0 + M], inT[:, kc, t0:t0 + n], kc == 0, kc == nk - 1,
                      [bW] + bin_, [bps])

    def out_proj(self, b, l, W, bW, mixT, bmix_of_tile, t_end):
        k = self.k
        with ExitStack() as es:
            yb = self.sb(es, "oy", [128, 8, 256], F32)
            byb = k.buf("oy")
            sq = self.sb(es, "osq", [128, 8, 256], F32)
            bsq = k.buf("osq")
            rs = self.sb(es, "ors", [128, 256], F32)
            brs = k.buf("ors")
            xb = [self.sb(es, "ox", [128, 8, 256], F32) for _ in range(2)]
            bxb = k.bufs(2, "ox")
            tmp = self.sb(es, "otmp", [128, 256], F32)
            btmp = k.buf("otmp")
            blks = blocks(0, min(t_end, S), 256) + (blocks(S, t_end, 256) if t_end > S else [])
            for bi, (t0, n) in enumerate(blks):
                v = b if t0 < S else 2
                G = self.modvec(l, 0, 2, v)
                p = bi % 2
                tiles = list(range(t0 // 128, (t0 + n) // 128))
                bin_ = [bb for i in tiles for bb in bmix_of_tile(i)]
                k.dma("sp", xb[p], self.xres[b][:, :, t0:t0 + n], [self.bxres[b][i] for i in tiles], [bxb[p]])
                for m in range(8):
                    ps = self.PS[m // 2][:, (m % 2) * 256:(m % 2) * 256 + 256]
                    self.lin_fm(ps, self.bPS[m // 2], W, bW, m * 128, 128, mixT, bin_, t0, n)
                for q in range(4):
                    eng = "act" if q % 2 == 0 else "dve"
                    src = self.PS[q].rearrange("p (c t) -> p c t", c=2)
                    if eng == "act":
                        k.act(yb[:, 2 * q:2 * q + 2, :], src, AF.Copy, [self.bPS[q]], [byb])
                    else:
                        k.v(lambda e: e.tensor_copy(yb[:, 2 * q:2 * q + 2, :], src), [self.bPS[q]], [byb])
                k.act(sq, yb, AF.Square, [byb], [bsq])
                ps = self.PS[6]
                for c in range(8):
                    k.mm(ps[:, 0:n], self.ones, sq[:, c, :], c == 0, c == 7, [self.bones, bsq], [self.bPS[6]])
                k.act(rs, ps[:, 0:n], AF.Sqrt, [self.bPS[6]], [brs], scale=1.0 / D, bias=RMS_EPS)
                k.v(lambda e: e.reciprocal(rs, rs), [brs], [brs])
                for c in range(8):
                    k.v(lambda e: e.tensor_tensor(tmp, yb[:, c, :], rs, ALU.mult), [byb, brs], [btmp])
                    k.v(lambda e: e.scalar_tensor_tensor(xb[p][:, c, :], tmp, G[:, c:c + 1], xb[p][:, c, :], ALU.mult, ALU.add),
                        [btmp, self.bmod, bxb[p]], [bxb[p]])
                k.dma("sp", self.xres[b][:, :, t0:t0 + n], xb[p], [bxb[p]], [self.bxres[b][i] for i in tiles])
            k.barrier()

    def sequence(self, b):
        self.mixer0(b)
        if self.stop in ("mix0", "m0_norm", "m0_conv", "m0_proj", "m0_scan"):
            return
        self.moe(b, 0, T)
        if self.stop == "moe0":
            return
        self.mixer1(b)
        if self.stop == "mix1":
            return
        self.moe(b, 1, S)

    def mixer0(self, b):
        nc, k, I = self.nc, self.k, self.I
        with ExitStack() as esm:
            cvT = self.sb(esm, "cvT", [128, 4, T], BF16)
            bcv = k.bufs(NT, "cvT")
            with ExitStack() as esg:
                with ExitStack() as esh:
                    hT = self.sb(esh, "hT", [128, 8, T], BF16)
                    bh = k.bufs(NT, "hT")
                    self.norm_mod(hT, bh, b, 0, 0, T)
                    if self.stop == "m0_norm":
                        return
                    self.conv_branch(hT, bh, cvT, bcv)
                    if self.stop == "m0_conv":
                        return
                    qT = self.sb(esg, "qT", [128, 2, T], BF16)
                    kT = self.sb(esg, "kT", [128, 2, T], BF16)
                    bqk = k.bufs(NT, "qk")
                    v_tm = self.sb(esg, "v_tm", [128, NT, 512], BF16)
                    sg_tm = self.sb(esg, "sg_tm", [128, NT, 512], BF16)
                    bvg = k.bufs(NT, "vg")
                    lrT = [self.sb(esg, "lrT", [32, T], F32) for _ in range(2)]
                    blr = k.bufs(2, "lrT")
                    self.gla_proj(hT, bh, qT, kT, bqk, v_tm, sg_tm, bvg, lrT, blr)
                    if self.stop == "m0_proj":
                        return
                aoT = self.sb(esm, "aoT", [128, 4, T], BF16)
                bao = k.bufs(NT, "aoT")
                self.gla_scan(qT, kT, bqk, v_tm, sg_tm, bvg, lrT, blr, aoT, bao)
                if self.stop == "m0_scan":
                    return
            with ExitStack() as es:
                W = self.sb(es, "wout", [128, 8, D], BF16)
                bW = k.buf("wout")
                k.dma("pool", W, I["ab_w_out"].rearrange("(c p) n -> p c n", p=128), [], [bW])

                class Mix:
                    def __getitem__(s, idx):
                        p_, kc, tsl = idx
                        return aoT[:, kc, tsl] if kc < 4 else cvT[:, kc - 4, tsl]
                self.out_proj(b, 0, W, bW, Mix(), lambda i: [bao[i], bcv[i]], T)

    def conv_branch(self, hT, bh, cvT, bcv):
        k, I = self.k, self.I
        with ExitStack() as es:
            W = self.sb(es, "wglu", [128, 8, 256], BF16)
            bW = k.buf("wglu")
            cw, bcw = self.load_const(es, "convw", I["convw"], [128, 4, 31])
            cvv, bcvv = self.load_const(es, "convv", I["convv"], [128, 3, 4])
            u = self.sb(es, "cu", [128, T], F32)
            bu = k.buf("cu")
            acc = self.sb(es, "cacc", [128, 4, T], F32)
            bacc = k.bufs(4, "cacc")
            sg = self.sb(es, "csg", [128, 512], F32)
            bsg = k.buf("csg")
            blks = blocks(0, S, 512) + blocks(S, T, 512)
            wg = I["w_glu"].rearrange("(c p) n -> p c n", p=128)
            for cc in range(4):
                k.dma("pool", W[:, :, 0:128], wg[:, :, cc * 128:(cc + 1) * 128], [], [bW])
                k.dma("pool", W[:, :, 128:256], wg[:, :, 512 + cc * 128:512 + (cc + 1) * 128], [], [bW])
                for bi, (t0, n) in enumerate(blks):
                    tiles = list(range(t0 // 128, (t0 + n) // 128))
                    bin_ = [bh[i] for i in tiles]
                    pa, pb = self.PS[2 * (bi % 2)], self.PS[2 * (bi % 2) + 1]
                    bpa, bpb = self.bPS[2 * (bi % 2)], self.bPS[2 * (bi % 2) + 1]
                    self.lin_fm(pa, bpa, W, bW, 0, 128, hT, bin_, t0, n)
                    self.lin_fm(pb, bpb, W, bW, 128, 128, hT, bin_, t0, n)
                    k.act(sg[:, 0:n], pb[:, 0:n], AF.Sigmoid, [bpb], [bsg])
                    k.v(lambda e: e.tensor_tensor(u[:, t0:t0 + n], pa[:, 0:n], sg[:, 0:n], ALU.mult), [bpa, bsg], [bu])
                a = acc[:, cc, :]
                k.v(lambda e: e.tensor_scalar(a, u, cw[:, cc, 15:16], cvv[:, 0, cc:cc + 1], ALU.mult, ALU.add),
                    [bu, bcw, bcvv], [bacc[cc]])
                for (lo, hi) in ((0, S), (S, T)):
                    for j in range(31):
                        d = j - 15
                        if d == 0:
                            continue
                        o0, o1 = lo + max(0, -d), hi - max(0, d)
                        k.v(lambda e: e.scalar_tensor_tensor(a[:, o0:o1], u[:, o0 + d:o1 + d], cw[:, cc, j:j + 1], a[:, o0:o1],
                                                             ALU.mult, ALU.add), [bu, bcw, bacc[cc]], [bacc[cc]])
            sq = self.sb(es, "csq", [128, 4, 256], F32)
            bsq = k.buf("csq")
            mean = self.sb(es, "cmean", [128, 256], F32)
            msq = self.sb(es, "cmsq", [128, 256], F32)
            rstd = self.sb(es, "crstd", [128, 256], F32)
            bst = k.buf("cstat")
            tmp = self.sb(es, "ctmp", [128, 256], F32)
            btmp = k.buf("ctmp")
            for bi, (t0, n) in enumerate(blocks(0, T, 256)):
                tiles = list(range(t0 // 128, (t0 + n) // 128))
                ps1, ps2 = self.PS[4], self.PS[5]
                for cc in range(4):
                    k.mm(ps1[:, 0:n], self.ones, acc[:, cc, t0:t0 + n], cc == 0, cc == 3, [self.bones, bacc[cc]], [self.bPS[4]])
                k.act(sq[:, :, 0:n], acc[:, :, t0:t0 + n], AF.Square, bacc, [bsq])
                for cc in range(4):
                    k.mm(ps2[:, 0:n], self.ones, sq[:, cc, 0:n], cc == 0, cc == 3, [self.bones, bsq], [self.bPS[5]])
                k.act(mean[:, 0:n], ps1[:, 0:n], AF.Copy, [self.bPS[4]], [bst], scale=1.0 / 512)
                k.v(lambda e: e.tensor_tensor(msq[:, 0:n], mean[:, 0:n], mean[:, 0:n], ALU.mult), [bst], [bst])
                k.v(lambda e: e.scalar_tensor_tensor(rstd[:, 0:n], ps2[:, 0:n], 1.0 / 512, msq[:, 0:n], ALU.mult, ALU.subtract),
                    [self.bPS[5], bst], [bst])
                k.act(rstd[:, 0:n], rstd[:, 0:n], AF.Sqrt, [bst], [bst], scale=1.0, bias=LN_EPS)
                k.v(lambda e: e.reciprocal(rstd[:, 0:n], rstd[:, 0:n]), [bst], [bst])
                for cc in range(4):
                    k.v(lambda e: e.tensor_tensor(tmp[:, 0:n], acc[:, cc, t0:t0 + n], mean[:, 0:n], ALU.subtract), [bacc[cc], bst], [btmp])
                    k.v(lambda e: e.tensor_tensor(tmp[:, 0:n], tmp[:, 0:n], rstd[:, 0:n], ALU.mult), [btmp, bst], [btmp])
                    k.act(cvT[:, cc, t0:t0 + n], tmp[:, 0:n], AF.Silu, [btmp, bcvv], [bcv[i] for i in tiles],
                          scale=cvv[:, 1, cc:cc + 1], bias=cvv[:, 2, cc:cc + 1])
            k.barrier()

    def gla_proj(self, hT, bh, qT, kT, bqk, v_tm, sg_tm, bvg, lrT, blr):
        k, I = self.k, self.I
        with ExitStack() as es:
            Wqk = self.sb(es, "wqk", [128, 8, 1024], BF16)
            bWqk = k.buf("wqk")
            k.dma("pool", Wqk, I["w_qk"].rearrange("(c p) n -> p c n", p=128), [], [bWqk])
            Wvg, bWvg = Wqk, bWqk
            Wlr = self.sb(es, "wlr", [128, 8, 32], BF16)
            bWlr = k.buf("wlr")
            k.dma("pool", Wlr, I["w_lr"].rearrange("(c p) n -> p c n", p=128), [], [bWlr])
            rope, brope = self.load_const(es, "rope", I["rope"][0:2].rearrange("a p t -> p a t"), [128, 2, S])
            t1 = self.sb(es, "rt1", [128, 512], F32)
            t2 = self.sb(es, "rt2", [128, 512], F32)
            bt = k.bufs(2, "rt")
            blks = blocks(0, S, 512) + blocks(S, T, 512)
            for hp in range(2):
                for bi, (t0, n) in enumerate(blks):
                    tiles = list(range(t0 // 128, (t0 + n) // 128))
                    bin_ = [bh[i] for i in tiles]
                    for qi, dst in enumerate((qT, kT)):
                        pa, pb = self.PS[2 * qi], self.PS[2 * qi + 1]
                        bpa, bpb = self.bPS[2 * qi], self.bPS[2 * qi + 1]
                        self.lin_fm(pa, bpa, Wqk, bWqk, qi * 256 + hp * 128, 128, hT, bin_, t0, n)
                        if t0 < S:
                            self.lin_fm(pb, bpb, Wqk, bWqk, 512 + qi * 256 + hp * 128, 128, hT, bin_, t0, n)
                            k.v(lambda e: e.tensor_tensor(t1[:, 0:n], pa[:, 0:n], rope[:, 0, t0:t0 + n], ALU.mult), [bpa, brope], [bt[0]])
                            k.v(lambda e: e.tensor_tensor(t2[:, 0:n], pb[:, 0:n], rope[:, 1, t0:t0 + n], ALU.mult), [bpb, brope], [bt[1]])
                            k.v(lambda e: e.tensor_tensor(dst[:, hp, t0:t0 + n], t1[:, 0:n], t2[:, 0:n], ALU.add), bt, [bqk[i] for i in tiles])
                        else:
                            k.act(dst[:, hp, t0:t0 + n], pa[:, 0:n], AF.Copy, [bpa], [bqk[i] for i in tiles])
            k.dma("pool", Wvg, I["w_vg"].rearrange("(c p) n -> p c n", p=128), [], [bWvg])
            for i in range(NT):
                pv, pg = self.PS[2 * (i % 2)], self.PS[2 * (i % 2) + 1]
                bpv, bpg = self.bPS[2 * (i % 2)], self.bPS[2 * (i % 2) + 1]
                for kc in range(8):
                    k.mm(pv, hT[:, kc, i * 128:(i + 1) * 128], Wvg[:, kc, 0:512], kc == 0, kc == 7, [bh[i], bWvg], [bpv])
                for kc in range(8):
                    k.mm(pg, hT[:, kc, i * 128:(i + 1) * 128], Wvg[:, kc, 512:1024], kc == 0, kc == 7, [bh[i], bWvg], [bpg])
                k.v(lambda e: e.tensor_copy(v_tm[:, i, :], pv), [bpv], [bvg[i]])
                k.act(sg_tm[:, i, :], pg, AF.Silu, [bpg], [bvg[i]])
            for d in range(2):
                k.v(lambda e: e.memset(lrT[d], 1.0), [], [blr[d]])
                for bi, (t0, n) in enumerate(blks):
                    tiles = list(range(t0 // 128, (t0 + n) // 128))
                    ps = self.PS[4 + bi % 2]
                    bps = self.bPS[4 + bi % 2]
                    self.lin_fm(ps, bps, Wlr, bWlr, d * 16, 16, hT, [bh[i] for i in tiles], t0, n)
                    k.v(lambda e: e.tensor_copy(lrT[d][0:16, t0:t0 + n], ps[0:16, 0:n]), [bps], [blr[d]])
            k.barrier()

    def gla_scan(self, qT, kT, bqk, v_tm, sg_tm, bvg, lrT, blr, aoT, bao):
        k, I = self.k, self.I
        with ExitStack() as es:
            dwb = [self.load_const(es, "dwb", I["dwb_f" if d == 0 else "dwb_b"], [17, 256]) for d in range(2)]
            tri, btri = self.load_const(es, "tri", I["tri"].rearrange("a p t -> p a t"), [128, 2, 128])
            am, bam = self.load_const(es, "amask", I["amask"].rearrange("a p t -> p a t"), [128, 2, 64])
            gg, bgg = self.load_const(es, "glag", I["gla_g"].to_broadcast([128, 512]), [128, 512])
            o_f = self.sb(es, "o_f", [128, NT, 512], F32)
            bof = k.bufs(NT, "o_f")
            S32 = self.sb(es, "S32", [128, 2, 128], F32)
            Sb = self.sb(es, "Sb", [128, 2, 128], BF16)
            bS = [[k.buf("S") for _ in range(2)] for _ in range(2)]
            ex = self.sb(es, "gex", [128, 256], F32)
            ll = self.sb(es, "gll", [128, 256], F32)
            bll = k.buf("gll")
            NB_ = 2
            bT = [[self.sb(es, "gbT", [128, 128], F32) for _ in range(2)] for _ in range(NB_)]
            E3 = [[self.sb(es, "gE3", [128, 128], F32) for _ in range(2)] for _ in range(NB_)]
            E1 = [[self.sb(es, "gE1", [128, 128], F32) for _ in range(2)] for _ in range(NB_)]
            E2 = [[self.sb(es, "gE2", [128, 128], F32) for _ in range(2)] for _ in range(NB_)]
            E4 = [[self.sb(es, "gE4", [128, 128], F32) for _ in range(2)] for _ in range(NB_)]
            qe = [[self.sb(es, "gqe", [128, 128], BF16) for _ in range(2)] for _ in range(NB_)]
            ke = [[self.sb(es, "gke", [128, 128], BF16) for _ in range(2)] for _ in range(NB_)]
            qi_ = [[self.sb(es, "gqi", [128, 128], BF16) for _ in range(2)] for _ in range(NB_)]
            koT = [[self.sb(es, "gko", [128, 128], BF16) for _ in range(2)] for _ in range(NB_)]
            ko_tm = [[self.sb(es, "gkt", [128, 128], BF16) for _ in range(2)] for _ in range(NB_)]
            bprep = [[k.buf("gprep") for _ in range(2)] for _ in range(NB_)]
            aTm = [self.sb(es, "gaTm", [128, 64], BF16) for _ in range(2)]
            baTm = k.bufs(2, "gaTm")
            osum = self.sb(es, "gosum", [128, 512], F32)
            bosum = k.buf("gosum")
            ssq = self.sb(es, "gssq", [128, 4], F32)
            junk = self.sb(es, "gjunk", [128, 128], F32)
            bssq = k.buf("gssq")
            ao = self.sb(es, "gao", [128, 512], F32)
            aob = self.sb(es, "gaob", [128, 512], BF16)
            bao_t = k.buf("gao")

            step = 0
            for d in range(2):
                for hp in range(2):
                    k.v(lambda e: e.memset(S32[:, hp, :], 0.0), [], bS[hp])
                    k.v(lambda e: e.memset(Sb[:, hp, :], 0.0), [], bS[hp])
                order = [16, 17] + list(range(16)) if d == 0 else [17, 16] + list(range(15, -1, -1))
                for i in order:
                    pp = step % NB_
                    step += 1
                    c0 = i * 128
                    px = self.PS[6]
                    k.mm(px[:, 0:256], lrT[d][0:17, c0:c0 + 128], dwb[d][0], True, True, [blr[d], dwb[d][1]], [self.bPS[6]])
                    k.act(ex, px[:, 0:256], AF.Exp, [self.bPS[6]], [bll], scale=-1.0)
                    k.act(ll, ex, AF.Ln, [bll], [bll], scale=1.0, bias=1.0)
                    if DBG < 2:
                        continue
                    for hp in range(2):
                        bp = bprep[pp][hp]
                        pb_ = self.PS[4 + hp]
                        k.mm(pb_[:, 0:128], ll[:, hp * 128:(hp + 1) * 128], tri[:, d, :], True, True, [bll, btri], [self.bPS[4 + hp]])
                        b_ = bT[pp][hp]
                        k.v(lambda e: e.tensor_copy(b_, pb_[:, 0:128]), [self.bPS[4 + hp]], [bp])
                        k.act(E3[pp][hp], b_, AF.Exp, [bp], [bp])
                        for ch in range(2):
                            cs = ch * 64
                            mid = cs + (31 if d == 0 else 32)
                            last = cs + (63 if d == 0 else 0)
                            k.act(E2[pp][hp][:, cs:cs + 64], b_[:, cs:cs + 64], AF.Exp, [bp], [bp], scale=-1.0, bias=b_[:, mid:mid + 1])
                            k.act(E4[pp][hp][:, cs:cs + 64], b_[:, cs:cs + 64], AF.Exp, [bp], [bp], scale=-1.0, bias=b_[:, last:last + 1])
                        k.v(lambda e: e.reciprocal(E1[pp][hp], E2[pp][hp]), [bp], [bp])
                        qs = qT[:, hp, c0:c0 + 128]
                        ks = kT[:, hp, c0:c0 + 128]
                        k.v(lambda e: e.tensor_tensor(qe[pp][hp], qs, E1[pp][hp], ALU.mult), [bp, bqk[i]], [bp])
                        k.v(lambda e: e.tensor_tensor(ke[pp][hp], ks, E2[pp][hp], ALU.mult), [bp, bqk[i]], [bp])
                        k.v(lambda e: e.tensor_tensor(qi_[pp][hp], qs, E3[pp][hp], ALU.mult), [bp, bqk[i]], [bp], eng="pool")
                        k.v(lambda e: e.tensor_tensor(koT[pp][hp], ks, E4[pp][hp], ALU.mult), [bp, bqk[i]], [bp], eng="pool")
                        if DBG < 3:
                            continue
                        ptr = self.PSB[:, hp * 128:(hp + 1) * 128]
                        k.tr(ptr, koT[pp][hp], self.identb, [bp, self.bidentb], [self.bPS[7]])
                        k.v(lambda e: e.tensor_copy(ko_tm[pp][hp], ptr), [self.bPS[7]], [bp])
                    if DBG < 4:
                        continue
                    for ch in ((0, 1) if d == 0 else (1, 0)):
                        cs = ch * 64
                        last = cs + (63 if d == 0 else 0)
                        for hp in range(2):
                            bp = bprep[pp][hp]
                            for h2 in range(2):
                                hd = hp * 2 + h2
                                pr = h2 * 64
                                pa = self.PS[0]
                                bpa = self.bPS[0]
                                am_ = aTm[hd % 2]
                                a0 = (hd % 2) * 64
                                k.mm(pa[cs:cs + 64, a0:a0 + 64], ke[pp][hp][pr:pr + 64, cs:cs + 64], qe[pp][hp][pr:pr + 64, cs:cs + 64],
                                     True, True, [bp], [bpa])
                                k.v(lambda e: e.tensor_tensor(am_[cs:cs + 64, :], pa[cs:cs + 64, a0:a0 + 64], am[cs:cs + 64, d, :], ALU.mult),
                                    [bpa, bam], [baTm[hd % 2]])
                                if DBG < 5:
                                    continue
                                po = self.PS[2]
                                bpo = self.bPS[2]
                                k.mm(po[cs:cs + 64, hd * 128:(hd + 1) * 128], am_[cs:cs + 64, :], v_tm[cs:cs + 64, i, hd * 128:(hd + 1) * 128],
                                     True, True, [baTm[hd % 2], bvg[i]], [bpo])
                                k.mm(self.PS[1][cs:cs + 64, hd * 128:(hd + 1) * 128], qi_[pp][hp][pr:pr + 64, cs:cs + 64], Sb[pr:pr + 64, hp, :],
                                     True, True, [bp, bS[hp][h2]], [self.bPS[1]])
                                if DBG < 6:
                                    continue
                                pS = self.PS[3]
                                bpS = self.bPS[3]
                                k.mm(pS[pr:pr + 64, hp * 128:(hp + 1) * 128], ko_tm[pp][hp][cs:cs + 64, pr:pr + 64],
                                     v_tm[cs:cs + 64, i, hd * 128:(hd + 1) * 128], True, True, [bp, bvg[i]], [bpS])
                                k.v(lambda e: e.scalar_tensor_tensor(S32[pr:pr + 64, hp, :], S32[pr:pr + 64, hp, :],
                                                                     E3[pp][hp][pr:pr + 64, last:last + 1],
                                                                     pS[pr:pr + 64, hp * 128:(hp + 1) * 128], ALU.mult, ALU.add),
                                    [bp, bpS, bS[hp][h2]], [bS[hp][h2]])
                                k.act(Sb[pr:pr + 64, hp, :], S32[pr:pr + 64, hp, :], AF.Copy, [bS[hp][h2]], [bS[hp][h2]])
                    if DBG < 7:
                        continue
                    po = self.PS[2]
                    bpo = self.bPS[2]
                    if d == 0:
                        k.act(o_f[:, i, :], po, AF.Copy, [bpo], [bof[i]])
                        k.v(lambda e: e.tensor_tensor(o_f[:, i, :], self.PS[1], o_f[:, i, :], ALU.add), [self.bPS[1], bof[i]], [bof[i]])
                    else:
                        k.v(lambda e: e.tensor_tensor(osum, po, o_f[:, i, :], ALU.add), [bpo, bof[i]], [bosum])
                        k.v(lambda e: e.tensor_tensor(osum, self.PS[1], osum, ALU.add), [self.bPS[1], bosum], [bosum])
                        for hd in range(4):
                            k.act(junk, osum[:, hd * 128:(hd + 1) * 128], AF.Square, [bosum], [bssq], accum_out=ssq[:, hd:hd + 1])
                        k.act(ssq, ssq, AF.Sqrt, [bssq], [bssq], scale=1.0 / (64.0 * 128.0), bias=RMS_EPS)
                        k.v(lambda e: e.reciprocal(ssq, ssq), [bssq], [bssq])
                        for hd in range(4):
                            k.v(lambda e: e.tensor_scalar(ao[:, hd * 128:(hd + 1) * 128], osum[:, hd * 128:(hd + 1) * 128],
                                                          ssq[:, hd:hd + 1], 0.125, ALU.mult, ALU.mult), [bosum, bssq], [bao_t])
                        k.v(lambda e: e.tensor_tensor(ao, ao, gg, ALU.mult), [bao_t, bgg], [bao_t])
                        k.v(lambda e: e.tensor_tensor(aob, ao, sg_tm[:, i, :], ALU.mult), [bao_t, bvg[i]], [bao_t])
                        for c in range(4):
                            ptr = self.PSB[:, 512 + c * 128:512 + (c + 1) * 128]
                            k.tr(ptr, aob[:, c * 128:(c + 1) * 128], self.identb, [bao_t, self.bidentb], [self.bPS[7]])
                        k.act(aoT[:, :, c0:c0 + 128], self.PSB[:, 512:1024].rearrange("p (c t) -> p c t", c=4), AF.Copy,
                              [self.bPS[7]], [bao[i]])
            k.barrier()

    def mixer1(self, b):
        k, I = self.k, self.I
        with ExitStack() as esm:
          with ExitStack() as esq:
            with ExitStack() as esh:
                hT = self.sb(esh, "hT", [128, 8, T], BF16)
                bh = k.bufs(NT, "hT")
                self.norm_mod(hT, bh, b, 1, 0, T)
                qT = self.sb(esq, "nqT", [128, 8, S], BF16)
                kT = self.sb(esq, "nkT", [128, 8, T], BF16)
                bq = k.bufs(NT, "nq")
                bk = k.bufs(NT, "nk")
                v_tm = self.sb(esq, "nv", [128, NT, D], BF16)
                bv = k.bufs(NT, "nv")
                v_sh = self.sb(esq, "nvs", [128, 15, D], BF16)
                bvs = k.bufs(15, "nvs")
                with ExitStack() as es:
                    w = self.sb(es, "nw", [128, 8, D], BF16)
                    bW = k.buf("nw")
                    for j in range(3):
                        k.dma("pool", w, I["na_w_in"][:, j * D:(j + 1) * D].rearrange("(c p) n -> p c n", p=128), [], [bW])
                        if j < 2:
                            dst, bd, tend = (qT, bq, S) if j == 0 else (kT, bk, T)
                            blks = blocks(0, min(tend, S), 512) + (blocks(S, tend, 512) if tend > S else [])
                            n_ = 0
                            for (t0, n) in blks:
                                tiles = list(range(t0 // 128, (t0 + n) // 128))
                                for m in range(8):
                                    ps = self.PS[n_ % 4]
                                    bps = self.bPS[n_ % 4]
                                    n_ += 1
                                    self.lin_fm(ps, bps, w, bW, m * 128, 128, hT, [bh[i] for i in tiles], t0, n)
                                    if m % 2 == 0:
                                        k.act(dst[:, m, t0:t0 + n], ps[:, 0:n], AF.Copy, [bps], [bd[i] for i in tiles],
                                              scale=(0.125 if j == 0 else 1.0))
                                    else:
                                        k.v(lambda e: e.tensor_scalar(dst[:, m, t0:t0 + n], ps[:, 0:n], (0.125 if j == 0 else 1.0), None, ALU.mult),
                                            [bps], [bd[i] for i in tiles])
                        else:
                            n_ = 0
                            for (dstv, bdv, ntl, toff) in ((v_tm, bv, NT, 0), (v_sh, bvs, 15, 64)):
                                for i in range(ntl):
                                    c0 = toff + i * 128
                                    rd = [bh[c0 // 128], bh[(c0 + 127) // 128], bW]
                                    for hf in range(2):
                                        ps = self.PS[n_ % 4]
                                        bps = self.bPS[n_ % 4]
                                        n_ += 1
                                        for kc in range(8):
                                            k.mm(ps, hT[:, kc, c0:c0 + 128], w[:, kc, hf * 512:(hf + 1) * 512], kc == 0, kc == 7, rd, [bps])
                                        if hf == 0:
                                            k.act(dstv[:, i, 0:512], ps, AF.Copy, [bps], [bdv[i]])
                                        else:
                                            k.v(lambda e: e.tensor_copy(dstv[:, i, 512:1024], ps), [bps], [bdv[i]])
                    k.barrier()
            atT = self.sb(esm, "natT", [128, 8, S], BF16)
            bat = k.bufs(16, "nat")
            self.na_attention(qT, kT, bq, bk, v_tm, bv, v_sh, bvs, atT, bat)
            esq.close()
          if True:
            with ExitStack() as es:
                W = self.sb(es, "nwout", [128, 8, D], BF16)
                bW = k.buf("nwout")
                k.dma("pool", W, I["na_w_out"].rearrange("(c p) n -> p c n", p=128), [], [bW])
                self.out_proj(b, 1, W, bW, atT, lambda i: [bat[i]], S)

    def na_attention(self, qT, kT, bq, bk, v_tm, bv, v_sh, bvs, atT, bat):
        k, I = self.k, self.I
        with ExitStack() as es:
            Bh = [self.sb(es, "nB", [64, 960], F32) for _ in range(2)]
            bBh = k.bufs(2, "nB")
            NP = 2
            sc = [self.sb(es, "nsc", [64, 768], F32) for _ in range(NP)]
            pe_ = [self.sb(es, "npe", [64, 768], F32) for _ in range(NP)]
            pb = [self.sb(es, "npb", [64, 768], BF16) for _ in range(NP)]
            st = [self.sb(es, "nst", [64, 4], F32) for _ in range(NP)]
            bsm = k.bufs(NP, "nsm")
            pT = [self.sb(es, "npT", [128, 7, 64], BF16) for _ in range(NP)]
            bpT = k.bufs(NP, "npT")
            u = 0
            for h in range(16):
                m, pr = h // 2, (h % 2) * 64
                B = Bh[h % 2]
                k.dma("sp", B, I["na_bias"][h], [], [bBh[h % 2]])
                for r in range(32):
                    p = u % NP
                    u += 1
                    r0 = min(max(r - 4, 0), 24)
                    s = r0 - r + 7
                    w0 = r0 * 64
                    ktiles = sorted(set(range(w0 // 128, (w0 + 511) // 128 + 1)))
                    qtile = r // 2
                    qs = qT[pr:pr + 64, m, r * 64:(r + 1) * 64]
                    ps_w, ps_c = self.PS[2 * p], self.PS[2 * p + 1]
                    bpw, bpc = self.bPS[2 * p], self.bPS[2 * p + 1]
                    k.mm(ps_w[0:64, 0:512], qs, kT[pr:pr + 64, m, w0:w0 + 512], True, True, [bq[qtile]] + [bk[i] for i in ktiles], [bpw])
                    k.mm(ps_c[0:64, 0:256], qs, kT[pr:pr + 64, m, S:T], True, True, [bq[qtile], bk[16], bk[17]], [bpc])
                    bs = bsm[p]
                    k.v(lambda e: e.tensor_tensor(sc[p][:, 0:512], ps_w[0:64, 0:512], B[:, s * 64:s * 64 + 512], ALU.add), [bpw, bBh[h % 2]], [bs])
                    k.act(sc[p][:, 512:768], ps_c[0:64, 0:256], AF.Copy, [bpc], [bs])
                    k.v(lambda e: e.reduce_max(st[p][:, 0:1], sc[p], AX.X), [bs], [bs])
                    k.v(lambda e: e.tensor_scalar(st[p][:, 1:2], st[p][:, 0:1], -1.0, None, ALU.mult), [bs], [bs])
                    k.act(pe_[p], sc[p], AF.Exp, [bs], [bs], scale=1.0, bias=st[p][:, 1:2], accum_out=st[p][:, 2:3])
                    k.v(lambda e: e.reciprocal(st[p][:, 3:4], st[p][:, 2:3]), [bs], [bs])
                    k.v(lambda e: e.tensor_scalar(pb[p], pe_[p], st[p][:, 3:4], None, ALU.mult), [bs], [bs])
                    if w0 % 128 == 0:
                        chunks = [(j * 128, v_tm, bv, w0 // 128 + j) for j in range(4)]
                    else:
                        chunks = [(j * 128, v_sh, bvs, (w0 - 64) // 128 + j) for j in range(4)]
                    chunks += [(512, v_tm, bv, 16), (640, v_tm, bv, 17)]
                    ptb = self.PSB[:, 0:384].rearrange("p (j q) -> p j q", q=64)
                    for j, (col, vt, bvt, ti_) in enumerate(chunks):
                        k.tr(ptb[:, j, :], pb[p][:, col:col + 128], self.identb[0:64, 0:64], [bs, self.bidentb], [self.bPS[7]])
                    k.v(lambda e: e.tensor_copy(pT[p][:, 0:6, :], ptb), [self.bPS[7]], [bpT[p]])
                    po = self.PS[4 + p]
                    bpo = self.bPS[4 + p]
                    for j, (col, vt, bvt, ti_) in enumerate(chunks):
                        k.mm(po[pr:pr + 64, 0:64], vt[:, ti_, h * 64:(h + 1) * 64], pT[p][:, j, :],
                             j == 0, j == 5, [bvt[ti_], bpT[p]], [bpo])
                    k.act(atT[pr:pr + 64, m, r * 64:(r + 1) * 64], po[pr:pr + 64, 0:64], AF.Copy, [bpo], [bat[qtile]])
            k.barrier()

    def moe(self, b, l, t_end):
        k, I = self.k, self.I
        nt = t_end // 128
        with ExitStack() as esm:
            gate = self.sb(esm, "gate", [128, NT, E], F32)
            bgate = k.bufs(NT, "gate")
            yacc = self.sb(esm, "yacc", [128, NT, D], F32)
            byacc = k.bufs(NT, "yacc")
            hT = self.sb(esm, "hT", [128, 8, T], BF16)
            bh = k.bufs(NT, "hT")
            with ExitStack() as esr:
                rw, brw = self.load_const(esr, "rw", I["router_w"][l].rearrange("(c p) n -> p c n", p=128), [128, 8, E])
                rb, brb = self.load_const(esr, "rb", I["router_b"][l].to_broadcast([128, E]), [128, E])
                R = {}
                for nm, shp in (("sc", [128, E]), ("bi", [128, E]), ("m8", [128, 8, 8]), ("gs", [128, 8]), ("g8", [128, 8]),
                                ("pen", [128, 8]), ("mk", [128, E]), ("t8", [128, 8]), ("sel", [128, E]), ("gsum", [128, 2])):
                    R[nm] = self.sb(esr, "r" + nm, shp, F32)
                bR = k.buf("rout")

                def route(tile, ps, bps):
                    k.act(R["sc"], ps, AF.Sigmoid, [bps], [bR])
                    k.v(lambda e: e.tensor_tensor(R["bi"], R["sc"], rb, ALU.add), [bR, brb], [bR])
                    for g in range(8):
                        k.v(lambda e: e.max(out=R["m8"][:, g, :], in_=R["bi"][:, g * 32:(g + 1) * 32]), [bR], [bR])
                    k.v(lambda e: e.tensor_tensor(R["gs"], R["m8"][:, :, 0], R["m8"][:, :, 1], ALU.add), [bR], [bR])
                    k.v(lambda e: e.max(out=R["g8"], in_=R["gs"]), [bR], [bR])
                    k.v(lambda e: e.tensor_scalar(R["pen"], R["gs"], R["g8"][:, 3:4], None, ALU.is_ge), [bR], [bR])
                    k.v(lambda e: e.tensor_scalar(R["pen"], R["pen"], -1.0, 1.0e4, ALU.add, ALU.mult), [bR], [bR])
                    for g in range(8):
                        k.v(lambda e: e.tensor_scalar(R["mk"][:, g * 32:(g + 1) * 32], R["bi"][:, g * 32:(g + 1) * 32],
                                                      R["pen"][:, g:g + 1], None, ALU.add), [bR], [bR])
                    k.v(lambda e: e.max(out=R["t8"], in_=R["mk"]), [bR], [bR])
                    k.v(lambda e: e.tensor_scalar(R["sel"], R["mk"], R["t8"][:, 7:8], None, ALU.is_ge), [bR], [bR])
                    k.v(lambda e: e.tensor_tensor(R["sel"], R["sel"], R["sc"], ALU.mult), [bR], [bR])
                    k.v(lambda e: e.reduce_sum(R["gsum"][:, 0:1], R["sel"], AX.X), [bR], [bR])
                    k.v(lambda e: e.reciprocal(R["gsum"][:, 1:2], R["gsum"][:, 0:1]), [bR], [bR])
                    k.v(lambda e: e.tensor_scalar(gate[:, tile, :], R["sel"], R["gsum"][:, 1:2], 2.5, ALU.mult, ALU.mult),
                        [bR], [bgate[tile]])

                self.norm_mod(hT, bh, b, l, 1, t_end, router=(rw, brw, route))
            with ExitStack() as es:
                Wgu = [self.sb(es, "wgu", [128, 8, 512], BF16) for _ in range(2)]
                Wd = [self.sb(es, "wd", [128, 2, D], BF16) for _ in range(2)]
                bWe = k.bufs(2, "we")
                sgl = self.sb(es, "msg", [128, 512], F32)
                bsgl = k.buf("msg")
                hid = [[self.sb(es, "mhid", [128, 512], BF16) for _ in range(2)] for _ in range(2)]
                bhid = k.bufs(2, "mhid")
                blks = blocks(0, min(t_end, S), 512) + (blocks(S, t_end, 512) if t_end > S else [])
                elist = [-1] + list(self.experts)
                yq = 0
                hq = 0
                for ei, e_ in enumerate(elist):
                    p = ei % 2
                    if e_ < 0:
                        sg_, su_, sd_ = I["ws_gate"][l], I["ws_up"][l], I["ws_down"][l]
                    else:
                        sg_, su_, sd_ = I["w_gate"][l, e_], I["w_up"][l, e_], I["w_down"][l, e_]
                    k.dma("pool", Wgu[p][:, :, 0:256], sg_.rearrange("(c p) n -> p c n", p=128), [], [bWe[p]])
                    k.dma("pool", Wgu[p][:, :, 256:512], su_.rearrange("(c p) n -> p c n", p=128), [], [bWe[p]])
                    k.dma("pool", Wd[p], sd_.rearrange("(c p) n -> p c n", p=128), [], [bWe[p]])
                    for (t0, n) in blks:
                        tiles = list(range(t0 // 128, (t0 + n) // 128))
                        bin_ = [bh[i] for i in tiles]
                        for m in range(2):
                            self.lin_fm(self.PS[m], self.bPS[m], Wgu[p], bWe[p], m * 128, 128, hT, bin_, t0, n)
                            self.lin_fm(self.PS[2 + m], self.bPS[2 + m], Wgu[p], bWe[p], 256 + m * 128, 128, hT, bin_, t0, n)
                        hp_ = hq % 2
                        hq += 1
                        for m in range(2):
                            k.act(sgl[:, 0:n], self.PS[m][:, 0:n], AF.Silu, [self.bPS[m]], [bsgl])
                            k.v(lambda e: e.tensor_tensor(hid[hp_][m][:, 0:n], sgl[:, 0:n], self.PS[2 + m][:, 0:n], ALU.mult),
                                [bsgl, self.bPS[2 + m]], [bhid[hp_]])
                        for ti, tile in enumerate(tiles):
                            for hf in range(2):
                                py = self.PS[4 + yq % 3]
                                bpy = self.bPS[4 + yq % 3]
                                yq += 1
                                for m in range(2):
                                    k.mm(py, hid[hp_][m][:, ti * 128:(ti + 1) * 128], Wd[p][:, m, hf * 512:(hf + 1) * 512], m == 0, m == 1,
                                         [bhid[hp_], bWe[p]], [bpy])
                                ya = yacc[:, tile, hf * 512:(hf + 1) * 512]
                                if e_ < 0:
                                    k.act(ya, py, AF.Copy, [bpy], [byacc[tile]])
                                else:
                                    k.v(lambda e: e.scalar_tensor_tensor(ya, py, gate[:, tile, e_:e_ + 1], ya, ALU.mult, ALU.add),
                                        [bpy, bgate[tile], byacc[tile]], [byacc[tile]])
                k.barrier()
            with ExitStack() as es:
                ssq = self.sb(es, "pssq", [128, 2], F32)
                bss = k.buf("pssq")
                junk = self.sb(es, "pjunk", [128, D], F32)
                yn = self.sb(es, "pyn", [128, D], F32)
                byn = k.buf("pyn")
                xb = [self.sb(es, "px", [128, 8, 128], F32) for _ in range(2)]
                bxb = k.bufs(2, "px")
                for tile in range(nt):
                    p = tile % 2
                    v = b if tile < 16 else 2
                    G = self.modvec(l, 1, 2, v)
                    k.dma("sp", xb[p], self.xres[b][:, :, tile * 128:(tile + 1) * 128], [self.bxres[b][tile]], [bxb[p]])
                    k.act(junk, yacc[:, tile, :], AF.Square, [byacc[tile]], [bss], accum_out=ssq[:, 0:1])
                    k.act(ssq[:, 1:2], ssq[:, 0:1], AF.Sqrt, [bss], [bss], scale=1.0 / D, bias=RMS_EPS)
                    k.v(lambda e: e.reciprocal(ssq[:, 1:2], ssq[:, 1:2]), [bss], [bss])
                    k.v(lambda e: e.tensor_scalar(yn, yacc[:, tile, :], ssq[:, 1:2], None, ALU.mult), [bss, byacc[tile]], [byn])
                    pa, pb_ = self.PS[2 * p], self.PS[2 * p + 1]
                    for c in range(8):
                        ps = (pa if c < 4 else pb_)[:, (c % 4) * 128:(c % 4 + 1) * 128]
                        k.tr(ps, yn[:, c * 128:(c + 1) * 128], self.ident, [byn, self.bident], [self.bPS[2 * p + c // 4]])
                    for c in range(8):
                        ps = (pa if c < 4 else pb_)[:, (c % 4) * 128:(c % 4 + 1) * 128]
                        k.v(lambda e: e.scalar_tensor_tensor(xb[p][:, c, :], ps, G[:, c:c + 1], xb[p][:, c, :], ALU.mult, ALU.add),
                            [self.bPS[2 * p + c // 4], self.bmod, bxb[p]], [bxb[p]])
                    k.dma("sp", self.xres[b][:, :, tile * 128:(tile + 1) * 128], xb[p], [bxb[p]], [self.bxres[b][tile]])
                k.barrier()


def host_consts():
    ident = np.eye(128, dtype=np.float32)
    s = np.arange(128)[:, None]
    t = np.arange(128)[None, :]
    same = (s // 64) == (t // 64)
    tri = np.stack([np.where(same & (s <= t), -1.0 / 16, 0.0), np.where(same & (s >= t), -1.0 / 16, 0.0)]).astype(np.float32)
    s6 = np.arange(64)[:, None]
    t6 = np.arange(64)[None, :]
    mf = (s6 <= t6).astype(np.float32)
    mb = (s6 >= t6).astype(np.float32)
    amask = np.stack([np.concatenate([mf, mf], 0), np.concatenate([mb, mb], 0)]).astype(np.float32)
    kk = np.arange(128) % 64
    f = kk % 16
    inv = (10000.0 ** (-(np.arange(16, dtype=np.float32)) / 16)).astype(np.float32)
    tt = np.arange(S)
    rows = (tt // 64).astype(np.float32)
    cols = (tt % 64).astype(np.float32)
    pos = np.where((kk < 32)[:, None], rows[None, :], cols[None, :]).astype(np.float32)
    ang = pos * inv[f][:, None]
    cosT = np.cos(ang).astype(np.float32)
    sinT = np.sin(ang).astype(np.float32)
    sign = np.where((kk % 32) < 16, -1.0, 1.0).astype(np.float32)[:, None]
    rope = np.stack([cosT, sinT * sign, cosT, sinT * sign]).astype(np.float32)
    return ident, tri, amask, rope


def na_bias_table(rpb):
    qc = np.arange(64)
    cstart = np.clip(qc - 8, 0, 64 - 16)
    kc = np.arange(64)
    ok = (kc[None, :] >= cstart[:, None]) & (kc[None, :] < cstart[:, None] + 16)
    idx = np.clip(kc[None, :] - qc[:, None] + 15, 0, 30)
    g = rpb[:, :, idx]
    g = np.transpose(g, (0, 2, 1, 3))
    out = np.where(ok[None, :, None, :], g, np.float32(NEG)).astype(np.float32)
    return np.ascontiguousarray(out.reshape(16, 64, 960))


def fm(vec):
    return np.ascontiguousarray(np.asarray(vec, np.float32).reshape(8, 128).T)


def prep_shared(inp):
    f32 = lambda a: np.ascontiguousarray(np.asarray(a, dtype=np.float32))
    ident, tri, amask, rope = host_consts()
    w_in = f32(inp["ab_w_in"][0])
    q, kk_, v, g, lrf, lrb, ga, gb = np.split(w_in, np.cumsum([256, 256, 512, 512, 16, 16, 512, 512])[:-1], axis=1)
    i64 = np.arange(64)
    partner = np.where((i64 % 32) < 16, i64 + 16, i64 - 16)
    perm = (np.arange(256) // 64) * 64 + partner[np.arange(256) % 64]
    sh = {
        "ada_w": f32(inp["ada_w"]),
        "ada_bT": np.ascontiguousarray(f32(inp["ada_b"]).reshape(2, 48, 128).transpose(0, 2, 1)),
        "gvec": np.ascontiguousarray(np.stack([np.stack([fm(inp[n][l]) for n in ("g_pre_mix", "g_post_mix", "g_pre_ffn", "g_post_ffn")], 1)
                                               for l in range(2)])),
        "w_qk": np.ascontiguousarray(np.concatenate([q, kk_, q[:, perm], kk_[:, perm]], 1)),
        "w_vg": np.ascontiguousarray(np.concatenate([v, g], 1)),
        "w_lr": np.ascontiguousarray(np.concatenate([lrf, lrb], 1)),
        "w_glu": np.ascontiguousarray(np.concatenate([ga, gb], 1)),
        "ab_w_out": f32(inp["ab_w_out"][0]),
        "dwb_f": np.ascontiguousarray(np.concatenate([f32(inp["gla_dw_f"][0]), f32(inp["gla_db_f"][0])[None]], 0)),
        "dwb_b": np.ascontiguousarray(np.concatenate([f32(inp["gla_dw_b"][0]), f32(inp["gla_db_b"][0])[None]], 0)),
        "gla_g": f32(inp["gla_norm_g"][0])[None],
        "convw": np.ascontiguousarray(f32(inp["conv_w"][0]).reshape(31, 4, 128).transpose(2, 1, 0)),
        "convv": np.ascontiguousarray(np.stack([f32(inp[n][0]).reshape(4, 128).T for n in ("conv_b", "conv_ln_g", "conv_ln_b")], 1)),
        "rope": rope, "tri": tri, "amask": amask, "ident": ident,
        "na_w_in": f32(inp["na_w_in"][0]),
        "na_w_out": f32(inp["na_w_out"][0]),
        "na_bias": na_bias_table(f32(inp["na_rpb"][0])),
        "router_w": f32(inp["moe_router_w"]),
        "router_b": f32(inp["moe_router_b"])[:, None, :],
        "w_gate": f32(inp["moe_w_gate"]), "w_up": f32(inp["moe_w_up"]), "w_down": f32(inp["moe_w_down"]),
        "ws_gate": f32(inp["moe_ws_gate"]), "ws_up": f32(inp["moe_ws_up"]), "ws_down": f32(inp["moe_ws_down"]),
    }
    return sh


def core_inputs(inp, sh, b0, nbl):
    x = np.ascontiguousarray(np.asarray(inp["x"], np.float32)[b0:b0 + nbl])
    ctx = np.ascontiguousarray(np.asarray(inp["ctx"], np.float32)[b0:b0 + nbl])
    c = np.asarray(inp["c"], np.float32)
    cols = [c[b0 + j] if j < nbl else np.zeros(D, np.float32) for j in range(2)] + [np.asarray(inp["c_ctx"], np.float32), np.zeros(D, np.float32)]
    cvec = np.ascontiguousarray(np.stack([fm(v) for v in cols], axis=2))
    m = dict(sh)
    m.update({"x": x, "ctx": ctx, "cvec": cvec})
    return m


_PROG = {}


def kernel(**inputs):
    if "full" not in _PROG:
        _PROG["full"] = Prog()
    prog = _PROG["full"]
    sh = prep_shared(inputs)
    in_maps = [core_inputs(inputs, sh, 2 * c, 2) for c in range(8)]
    res = run_bass_kernel_spmd(prog.nc, in_maps, core_ids=list(range(8)))
    return np.concatenate([r["out"] for r in res.results], axis=0).astype(np.float32)
```

```python
from contextlib import ExitStack
import os
DBG = int(os.environ.get('GLA_DBG', '9'))
MDBG = int(os.environ.get('MOE_DBG', '9'))
import numpy as np
import concourse.bass as bass
import concourse.mybir as mybir
from concourse.bass_utils import run_bass_kernel_spmd

F32 = mybir.dt.float32
BF16 = mybir.dt.bfloat16
AF = mybir.ActivationFunctionType
ALU = mybir.AluOpType
AX = mybir.AxisListType

D = 1024
S = 2048
C = 256
T = S + C
NT = T // 128
NBL = 2
E = 256
RMS_EPS = 1e-6
LN_EPS = 1e-5
NEG = -30000.0
CAP = 512
U32 = mybir.dt.uint32


class Buf:
    __slots__ = ("name", "w", "r", "dsem")

    def __init__(self, name):
        self.name = name
        self.w = {}
        self.r = {}
        self.dsem = None


class KB:
    def __init__(self, nc):
        self.nc = nc
        self.E = {"pe": nc.tensor, "dve": nc.vector, "act": nc.scalar, "pool": nc.gpsimd, "sp": nc.sync}
        self.esem = {e: nc.alloc_semaphore("es_" + e) for e in self.E}
        self.ecnt = {e: 0 for e in self.E}
        self.seen = {e: {} for e in self.E}
        self.dcnt = {}
        self.dpool = []
        self.dnext = 0
        self.nbuf = 0
        self.allbufs = []

    def buf(self, name=None):
        self.nbuf += 1
        b = Buf("%s_%d" % (name or "b", self.nbuf))
        self.allbufs.append(b)
        return b

    def bufs(self, n, name="b"):
        return [self.buf(name) for _ in range(n)]

    def _wait(self, eng, deps):
        for key, (sem, val, src) in deps.items():
            if src == "pe" and eng == "pe":
                continue
            if src == "dma":
                val = self.dcnt[key[2:]]
            if self.seen[eng].get(key, 0) >= val:
                continue
            self.E[eng].wait_ge(sem, val)
            self.seen[eng][key] = val

    @staticmethod
    def _deps(reads, writes):
        deps = {}
        for b in reads:
            for k, t in b.w.items():
                if k not in deps or deps[k][1] < t[1]:
                    deps[k] = t
        for b in writes:
            for d in (b.w, b.r):
                for k, t in d.items():
                    if k not in deps or deps[k][1] < t[1]:
                        deps[k] = t
        return deps

    @staticmethod
    def _mark(tok, key, reads, writes):
        for b in writes:
            b.w = {key: tok}
            b.r = {}
        for b in reads:
            if b in writes:
                continue
            b.r[key] = tok

    def op(self, eng, fn, reads=(), writes=()):
        self._wait(eng, self._deps(reads, writes))
        ins = fn(self.E[eng])
        self.ecnt[eng] += 1
        ins.then_inc(self.esem[eng], 1)
        self._mark((self.esem[eng], self.ecnt[eng], eng), "e_" + eng, reads, writes)
        return ins

    def dma(self, q, out, in_, reads=(), writes=(), **kw):
        self._wait(q, self._deps(reads, writes))
        tgt = writes[0]
        if tgt.dsem is None:
            if len(self.dpool) < 72:
                nm = "ds%d" % len(self.dpool)
                self.dpool.append((self.nc.alloc_semaphore(nm), nm))
                self.dcnt[nm] = 0
            tgt.dsem = self.dpool[self.dnext % len(self.dpool)] if len(self.dpool) == 72 else self.dpool[-1]
            self.dnext += 1
        sem, nm = tgt.dsem
        ins = self.E[q].dma_start(out=out, in_=in_, **kw)
        self.dcnt[nm] += 16
        ins.then_inc(sem, 16)
        self._mark((sem, self.dcnt[nm], "dma"), "d_" + nm, reads, writes)
        return ins

    def idma(self, out, out_off, in_, in_off, reads=(), writes=()):
        self._wait("pool", self._deps(reads, writes))
        tgt = writes[0]
        if tgt.dsem is None:
            if len(self.dpool) < 72:
                nm = "ds%d" % len(self.dpool)
                self.dpool.append((self.nc.alloc_semaphore(nm), nm))
                self.dcnt[nm] = 0
            tgt.dsem = self.dpool[self.dnext % len(self.dpool)] if len(self.dpool) == 72 else self.dpool[-1]
            self.dnext += 1
        sem, nm = tgt.dsem
        ins = self.nc.gpsimd.indirect_dma_start(out=out, out_offset=out_off, in_=in_, in_offset=in_off)
        self.dcnt[nm] += 16
        ins.then_inc(sem, 16)
        self._mark((sem, self.dcnt[nm], "dma"), "d_" + nm, reads, writes)
        return ins

    def barrier(self):
        deps = {}
        for e in self.E:
            if self.ecnt[e]:
                deps["e_" + e] = (self.esem[e], self.ecnt[e], "x")
        for (sem, nm) in self.dpool:
            deps["d_" + nm] = (sem, self.dcnt[nm], "dma")
        self.allbufs = []
        for e in self.E:
            self._wait(e, {k: v for k, v in deps.items() if k != "e_" + e})

    def mm(self, out, lhsT, rhs, start, stop, reads, writes):
        return self.op("pe", lambda e: e.matmul(out, lhsT, rhs, start=start, stop=stop), reads, writes)

    def tr(self, out, in_, ident, reads, writes):
        return self.op("pe", lambda e: e.transpose(out, in_, ident), reads, writes)

    def act(self, out, in_, func, reads, writes, **kw):
        return self.op("act", lambda e: e.activation(out, in_, func, **kw), reads, writes)

    def v(self, fn, reads, writes, eng="dve"):
        return self.op(eng, fn, reads, writes)


def blocks(t0, t1, n):
    out = []
    while t0 < t1:
        m = min(n, t1 - t0)
        out.append((t0, m))
        t0 += m
    return out


class Prog:
    def __init__(self, stop="end", experts=None, nbl=NBL):
        self.stop = stop
        self.experts = list(range(E)) if experts is None else experts
        self.nbl = nbl
        nc = self.nc = bass.Bass("TRN2", target_bir_lowering=False)
        self.k = KB(nc)
        self.es = ExitStack()
        self.build()

    def din(self, name, shape, dt=F32):
        return self.nc.dram_tensor(name, list(shape), dt, kind="ExternalInput").ap()

    def sb(self, es, name, shape, dt):
        if not hasattr(self, "arena"):
            self.AW = 52800
            self.arena = self.nc.alloc_sbuf_tensor("arena", [128, self.AW], F32).ap()
            self.free_list = [(0, self.AW)]
        esz = 2 if dt == BF16 else 4
        nfree = int(np.prod(shape[1:]))
        words = (nfree * esz + 3) // 4
        words = (words + 7) // 8 * 8
        for idx, (o, n) in enumerate(self.free_list):
            if n >= words:
                break
        else:
            raise RuntimeError("SBUF arena exhausted allocating %s %s; free=%s" % (name, shape, self.free_list))
        if n == words:
            self.free_list.pop(idx)
        else:
            self.free_list[idx] = (o + words, n - words)
        P = shape[0]
        ap = self.arena[0:P, o:o + words]
        if dt != F32:
            ap = ap.bitcast(dt)
        ap = ap[:, 0:nfree]
        if len(shape) == 3:
            ap = ap.rearrange("p (a b) -> p a b", a=shape[1])
        elif len(shape) == 4:
            ap = ap.rearrange("p (a b c) -> p a b c", a=shape[1], b=shape[2])
        elif len(shape) == 5:
            ap = ap.rearrange("p (a b c d) -> p a b c d", a=shape[1], b=shape[2], c=shape[3])

        def _free():
            fl = self.free_list
            fl.append((o, words))
            fl.sort()
            merged = []
            for (a, n_) in fl:
                if merged and merged[-1][0] + merged[-1][1] == a:
                    merged[-1] = (merged[-1][0], merged[-1][1] + n_)
                else:
                    merged.append((a, n_))
            self.free_list[:] = merged
        es.callback(_free)
        return ap

    def load_const(self, es, name, src, shape, dt=F32, q="sp"):
        t = self.sb(es, name, shape, dt)
        b = self.k.buf(name)
        self.k.dma(q, t, src, [], [b])
        return t, b

    def build(self):
        nc, k = self.nc, self.k
        nbl = self.nbl
        I = self.I = {}
        I["x"] = self.din("x", [nbl, S, D])
        I["ctx"] = self.din("ctx", [nbl, C, D])
        I["cvec"] = self.din("cvec", [128, 8, 4])
        I["ada_w"] = self.din("ada_w", [2, D, 6 * D])
        I["ada_bT"] = self.din("ada_bT", [2, 128, 48])
        I["gvec"] = self.din("gvec", [2, 128, 4, 8])
        I["w_qk"] = self.din("w_qk", [D, 1024])
        I["w_vg"] = self.din("w_vg", [D, 1024])
        I["w_lr"] = self.din("w_lr", [D, 32])
        I["w_glu"] = self.din("w_glu", [D, 1024])
        I["ab_w_out"] = self.din("ab_w_out", [D, D])
        I["dwb_f"] = self.din("dwb_f", [17, 256])
        I["dwb_b"] = self.din("dwb_b", [17, 256])
        I["gla_g"] = self.din("gla_g", [1, 512])
        I["convw"] = self.din("convw", [128, 4, 31])
        I["convv"] = self.din("convv", [128, 3, 4])
        I["rope"] = self.din("rope", [4, 128, S])
        I["tri"] = self.din("tri", [2, 128, 128])
        I["amask"] = self.din("amask", [2, 128, 64])
        I["ident"] = self.din("ident", [128, 128])
        I["ustrict"] = self.din("ustrict", [128, 128])
        I["ebase1"] = self.din("ebase1", [1, E])
        I["na_w_in"] = self.din("na_w_in", [D, 3 * D])
        I["na_w_out"] = self.din("na_w_out", [D, D])
        I["na_bias"] = self.din("na_bias", [16, 64, 960])
        I["router_w"] = self.din("router_w", [2, D, E])
        I["router_b"] = self.din("router_b", [2, 1, E])
        ne = self.ne_decl = (max(self.experts) + 1) if self.experts else 1
        I["w_gate"] = self.din("w_gate", [2, ne, D, 256])
        I["w_up"] = self.din("w_up", [2, ne, D, 256])
        I["w_down"] = self.din("w_down", [2, ne, 256, D])
        I["ws_gate"] = self.din("ws_gate", [2, D, 256])
        I["ws_up"] = self.din("ws_up", [2, D, 256])
        I["ws_down"] = self.din("ws_down", [2, 256, D])
        self.out = nc.dram_tensor("out", [nbl, S, D], F32, kind="ExternalOutput").ap()
        self.xres = [nc.dram_tensor("xres%d" % b, [128, 8, T], F32).ap() for b in range(nbl)]
        self.bxres = [[k.buf("xres") for _ in range(NT)] for b in range(nbl)]
        self.bout = k.buf("out")
        self.NSH = E * CAP
        self.xs = nc.dram_tensor("xs_scr", [E * CAP, D], BF16).ap()
        self.xsh = nc.dram_tensor("xsh_scr", [nbl * NT * 128, D], BF16).ap()
        self.ys2 = [nc.dram_tensor("ys_scr%d" % hf, [E * CAP, 512], F32).ap() for hf in range(2)]
        self.ysh_d = nc.dram_tensor("ysh_scr", [nbl * NT * 128, D], F32).ap()

        self.PS = [nc.alloc_psum_tensor("ps%d" % i, [128, 512], F32).ap() for i in range(8)]
        self.bPS = k.bufs(8, "ps")
        self.PSB = self.PS[7].bitcast(BF16)

        es = self.es
        self.ident, self.bident = self.load_const(es, "ident", I["ident"], [128, 128])
        self.identb = self.sb(es, "identb", [128, 128], BF16)
        self.bidentb = k.buf("identb")
        k.v(lambda e: e.tensor_copy(self.identb, self.ident), [self.bident], [self.bidentb])
        self.ones = self.sb(es, "ones", [128, 128], F32)
        self.bones = k.buf("ones")
        k.v(lambda e: e.memset(self.ones, 1.0), [], [self.bones])

        self.onesb = self.sb(es, "onesb", [128, 128], BF16)
        k.v(lambda e: e.memset(self.onesb, 1.0), [], [self.bones])
        us32, bus = self.load_const(es, "us32", I["ustrict"], [128, 128])
        self.usb = self.sb(es, "usb", [128, 128], BF16)
        k.v(lambda e: e.tensor_copy(self.usb, us32), [bus], [self.bones])
        self.ebase1, _ = self.load_const(es, "ebase1", I["ebase1"].to_broadcast([128, E]), [128, E])
        self.slotI = self.sb(es, "slotI", [128, nbl * NT, 8], U32)
        self.gate8 = self.sb(es, "gate8", [128, nbl * NT, 8], F32)
        self.bslot = k.bufs(nbl * NT, "slot")
        self.dbg_out = None
        if os.environ.get("SLOT_DBG"):
            self.dbg_out = nc.dram_tensor("dbg", [128, nbl * NT * 8], F32, kind="ExternalOutput").ap()
            self.s8all = self.sb(es, "s8all", [128, nbl * NT, 8], F32)
            k.v(lambda e: e.memset(self.s8all, 0.0), [], [self.bones])

        self.compute_mods()
        for b in range(nbl):
            self.load_x(b)
        if self.stop != "load":
            for l in range(2):
                for b in range(nbl):
                    (self.mixer0 if l == 0 else self.mixer1)(b)
                if self.stop in ("mix%d" % l, "m0_norm", "m0_conv", "m0_proj", "m0_scan"):
                    break
                self.moe_layer(l, T if l == 0 else S)
                if self.stop == "moe%d" % l:
                    break
        if self.dbg_out is not None:
            k.barrier()
            k.dma("sp", self.dbg_out, self.s8all.rearrange("p a b -> p (a b)"), [], [self.bout])
        for b in range(nbl):
            self.dump_x(b)
        deps = dict(self.bout.w)
        k._wait("sp", deps)
        es.close()

    def compute_mods(self):
        nc, k, I = self.nc, self.k, self.I
        self.mod = []
        self.bmod = k.buf("mod")
        self.modv = self.sb(self.es, "modv", [128, 2, 6, 3, 8], F32)
        with ExitStack() as es:
            cv, bcv = self.load_const(es, "cvec", I["cvec"], [128, 8, 4])
            sc = self.sb(es, "silc", [128, 8, 4], F32)
            bsc = k.buf("silc")
            k.act(sc, cv, AF.Silu, [bcv], [bsc])
            wbuf = [self.sb(es, "adaw", [128, 8, 512], F32) for _ in range(2)]
            bw = k.bufs(2, "adaw")
            gv, bgv = self.load_const(es, "gvec", I["gvec"].rearrange("l p a c -> p l a c"), [128, 2, 4, 8])
            abt, babt = self.load_const(es, "adab", I["ada_bT"].rearrange("l p j -> p l j"), [128, 2, 48])
            modraw = self.sb(es, "modraw", [128, 2, 3, 48], F32)
            bmr = k.buf("modraw")
            ps = self.PS[0]
            bps = self.bPS[0]
            i = 0
            for l in range(2):
                for ch in range(12):
                    w = wbuf[i % 2]
                    k.dma("sp", w, I["ada_w"][l, :, ch * 512:(ch + 1) * 512].rearrange("(c p) n -> p c n", p=128),
                          [], [bw[i % 2]])
                    for jj in range(4):
                        j = ch * 4 + jj
                        for kc in range(8):
                            k.mm(ps[:, j * 4:(j + 1) * 4], w[:, kc, jj * 128:(jj + 1) * 128], sc[:, kc, :],
                                 kc == 0, kc == 7, [bw[i % 2], bsc], [bps])
                    i += 1
                for v in range(3):
                    k.v(lambda e: e.tensor_tensor(modraw[:, l, v, :],
                                                  ps[:, 0:192].rearrange("p (j v) -> p j v", v=4)[:, :, v],
                                                  abt[:, l, :], ALU.add), [bps, babt], [bmr])
            mv = self.modv
            for l in range(2):
                for v in range(3):
                    for s_ in range(2):
                        sh = modraw[:, l, v, (3 * s_ + 0) * 8:(3 * s_ + 0) * 8 + 8]
                        scl = modraw[:, l, v, (3 * s_ + 1) * 8:(3 * s_ + 1) * 8 + 8]
                        gt = modraw[:, l, v, (3 * s_ + 2) * 8:(3 * s_ + 2) * 8 + 8]
                        gpre = gv[:, l, 2 * s_ + 0, :]
                        gpost = gv[:, l, 2 * s_ + 1, :]
                        A = mv[:, l, 3 * s_ + 0, v, :]
                        k.v(lambda e: e.scalar_tensor_tensor(A, scl, 1.0, gpre, ALU.add, ALU.mult), [bmr, bgv], [self.bmod])
                        k.v(lambda e: e.tensor_copy(mv[:, l, 3 * s_ + 1, v, :], sh), [bmr], [self.bmod])
                        k.v(lambda e: e.tensor_tensor(mv[:, l, 3 * s_ + 2, v, :], gt, gpost, ALU.mult), [bmr, bgv], [self.bmod])
            k.barrier()

    def modvec(self, l, s_, kind, v):
        return self.modv[:, l, 3 * s_ + kind, v, :]

    def load_x(self, b):
        k, I = self.k, self.I
        with ExitStack() as es:
            xin = [self.sb(es, "xin", [128, D], F32) for _ in range(2)]
            bxin = k.bufs(2, "xin")
            xt = [self.sb(es, "xt", [128, 8, 128], F32) for _ in range(2)]
            bxt = k.bufs(2, "xt")
            for i in range(NT):
                src = I["x"][b, i * 128:(i + 1) * 128, :] if i < 16 else I["ctx"][b, (i - 16) * 128:(i - 15) * 128, :]
                p = i % 2
                k.dma("sp", xin[p], src, [], [bxin[p]])
                pa, pb = self.PS[2 * p], self.PS[2 * p + 1]
                for c in range(8):
                    ps = (pa if c < 4 else pb)[:, (c % 4) * 128:(c % 4 + 1) * 128]
                    k.tr(ps, xin[p][:, c * 128:(c + 1) * 128], self.ident, [bxin[p], self.bident],
                         [self.bPS[2 * p + (c // 4)]])
                k.act(xt[p][:, 0:4, :], pa.rearrange("p (c t) -> p c t", c=4), AF.Copy, [self.bPS[2 * p]], [bxt[p]])
                k.v(lambda e: e.tensor_copy(xt[p][:, 4:8, :], pb.rearrange("p (c t) -> p c t", c=4)),
                    [self.bPS[2 * p + 1]], [bxt[p]])
                k.dma("sp", self.xres[b][:, :, i * 128:(i + 1) * 128], xt[p], [bxt[p]], [self.bxres[b][i]])
            k.barrier()

    def dump_x(self, b):
        k = self.k
        with ExitStack() as es:
            xt = [self.sb(es, "dxt", [128, 8, 128], F32) for _ in range(2)]
            bxt = k.bufs(2, "dxt")
            xo = [self.sb(es, "dxo", [128, D], F32) for _ in range(2)]
            bxo = k.bufs(2, "dxo")
            for i in range(16):
                p = i % 2
                k.dma("sp", xt[p], self.xres[b][:, :, i * 128:(i + 1) * 128], [self.bxres[b][i]], [bxt[p]])
                pa, pb = self.PS[2 * p], self.PS[2 * p + 1]
                for c in range(8):
                    ps = (pa if c < 4 else pb)[:, (c % 4) * 128:(c % 4 + 1) * 128]
                    k.tr(ps, xt[p][:, c, :], self.ident, [bxt[p], self.bident], [self.bPS[2 * p + (c // 4)]])
                k.act(xo[p][:, 0:512], pa, AF.Copy, [self.bPS[2 * p]], [bxo[p]])
                k.v(lambda e: e.tensor_copy(xo[p][:, 512:1024], pb), [self.bPS[2 * p + 1]], [bxo[p]])
                k.dma("sp", self.out[b, i * 128:(i + 1) * 128, :], xo[p], [bxo[p]], [self.bout])
            k.barrier()

    def norm_mod(self, hT, bh, b, l, s_, t_end, router=None):
        k = self.k
        NBK = 256
        with ExitStack() as es:
            xb = [self.sb(es, "nx", [128, 8, NBK], F32) for _ in range(2)]
            bxb = k.bufs(2, "nx")
            sq = self.sb(es, "nsq", [128, 8, NBK], F32)
            bsq = k.buf("nsq")
            rs = self.sb(es, "nrs", [128, NBK], F32)
            brs = k.buf("nrs")
            tmp = self.sb(es, "ntmp", [128, NBK], F32)
            btmp = k.buf("ntmp")
            h32 = self.sb(es, "nh32", [128, 8, NBK], F32) if router else None
            bh32 = k.buf("nh32")
            blks = blocks(0, min(t_end, S), NBK) + (blocks(S, t_end, NBK) if t_end > S else [])
            for bi, (t0, n) in enumerate(blks):
                v = b if t0 < S else 2
                A = self.modvec(l, s_, 0, v)
                sh = self.modvec(l, s_, 1, v)
                p = bi % 2
                tiles = list(range(t0 // 128, (t0 + n) // 128))
                k.dma("sp", xb[p][:, :, 0:n], self.xres[b][:, :, t0:t0 + n], [self.bxres[b][i] for i in tiles], [bxb[p]])
                k.act(sq[:, :, 0:n], xb[p][:, :, 0:n], AF.Square, [bxb[p]], [bsq])
                ps = self.PS[6]
                for c in range(8):
                    k.mm(ps[:, 0:n], self.ones, sq[:, c, 0:n], c == 0, c == 7, [self.bones, bsq], [self.bPS[6]])
                k.act(rs[:, 0:n], ps[:, 0:n], AF.Sqrt, [self.bPS[6]], [brs], scale=1.0 / D, bias=RMS_EPS)
                k.v(lambda e: e.reciprocal(rs[:, 0:n], rs[:, 0:n]), [brs], [brs])
                for c in range(8):
                    k.v(lambda e: e.tensor_tensor(tmp[:, 0:n], xb[p][:, c, 0:n], rs[:, 0:n], ALU.mult), [bxb[p], brs], [btmp])
                    if router:
                        k.act(h32[:, c, 0:n], tmp[:, 0:n], AF.Identity, [btmp, self.bmod], [bh32],
                              scale=A[:, c:c + 1], bias=sh[:, c:c + 1])
                    else:
                        k.act(hT[:, c, t0:t0 + n], tmp[:, 0:n], AF.Identity, [btmp, self.bmod], [bh[i] for i in tiles],
                              scale=A[:, c:c + 1], bias=sh[:, c:c + 1])
                if router:
                    rw, brw, cb = router
                    for ti, tile in enumerate(tiles):
                        pr = self.PS[4 + ti % 2]
                        bpr = self.bPS[4 + ti % 2]
                        for c in range(8):
                            k.mm(pr[:, 0:E], h32[:, c, ti * 128:(ti + 1) * 128], rw[:, c, :], c == 0, c == 7, [bh32, brw], [bpr])
                        cb(tile, ti, pr[:, 0:E], bpr, h32, bh32)
            k.barrier()

    def lin_fm(self, ps, bps, W, bW, col0, M, inT, bin_, t0, n, nk=8):
        for kc in range(nk):
            self.k.mm(ps[0:M, 0:n], W[:, kc, col0:col0 + M], inT[:, kc, t0:t0 + n], kc == 0, kc == nk - 1,
                      [bW] + bin_, [bps])

    def out_proj(self, b, l, W, bW, mixT, bmix_of_tile, t_end):
        k = self.k
        with ExitStack() as es:
            yb = self.sb(es, "oy", [128, 8, 256], F32)
            byb = k.buf("oy")
            sq = self.sb(es, "osq", [128, 8, 256], F32)
            bsq = k.buf("osq")
            rs = self.sb(es, "ors", [128, 256], F32)
            brs = k.buf("ors")
            xb = [self.sb(es, "ox", [128, 8, 256], F32) for _ in range(2)]
            bxb = k.bufs(2, "ox")
            tmp = self.sb(es, "otmp", [128, 256], F32)
            btmp = k.buf("otmp")
            blks = blocks(0, min(t_end, S), 256) + (blocks(S, t_end, 256) if t_end > S else [])
            for bi, (t0, n) in enumerate(blks):
                v = b if t0 < S else 2
                G = self.modvec(l, 0, 2, v)
                p = bi % 2
                tiles = list(range(t0 // 128, (t0 + n) // 128))
                bin_ = [bb for i in tiles for bb in bmix_of_tile(i)]
                k.dma("sp", xb[p], self.xres[b][:, :, t0:t0 + n], [self.bxres[b][i] for i in tiles], [bxb[p]])
                for m in range(8):
                    ps = self.PS[m // 2][:, (m % 2) * 256:(m % 2) * 256 + 256]
                    self.lin_fm(ps, self.bPS[m // 2], W, bW, m * 128, 128, mixT, bin_, t0, n)
                for q in range(4):
                    eng = "act" if q % 2 == 0 else "dve"
                    src = self.PS[q].rearrange("p (c t) -> p c t", c=2)
                    if eng == "act":
                        k.act(yb[:, 2 * q:2 * q + 2, :], src, AF.Copy, [self.bPS[q]], [byb])
                    else:
                        k.v(lambda e: e.tensor_copy(yb[:, 2 * q:2 * q + 2, :], src), [self.bPS[q]], [byb])
                k.act(sq, yb, AF.Square, [byb], [bsq])
                ps = self.PS[6]
                for c in range(8):
                    k.mm(ps[:, 0:n], self.ones, sq[:, c, :], c == 0, c == 7, [self.bones, bsq], [self.bPS[6]])
                k.act(rs, ps[:, 0:n], AF.Sqrt, [self.bPS[6]], [brs], scale=1.0 / D, bias=RMS_EPS)
                k.v(lambda e: e.reciprocal(rs, rs), [brs], [brs])
                for c in range(8):
                    k.v(lambda e: e.tensor_tensor(tmp, yb[:, c, :], rs, ALU.mult), [byb, brs], [btmp])
                    k.v(lambda e: e.scalar_tensor_tensor(xb[p][:, c, :], tmp, G[:, c:c + 1], xb[p][:, c, :], ALU.mult, ALU.add),
                        [btmp, self.bmod, bxb[p]], [bxb[p]])
                k.dma("sp", self.xres[b][:, :, t0:t0 + n], xb[p], [bxb[p]], [self.bxres[b][i] for i in tiles])
            k.barrier()

    def mixer0(self, b):
        nc, k, I = self.nc, self.k, self.I
        with ExitStack() as esm:
            cvT = self.sb(esm, "cvT", [128, 4, T], BF16)
            bcv = k.bufs(NT, "cvT")
            with ExitStack() as esg:
                with ExitStack() as esh:
                    hT = self.sb(esh, "hT", [128, 8, T], BF16)
                    bh = k.bufs(NT, "hT")
                    self.norm_mod(hT, bh, b, 0, 0, T)
                    if self.stop == "m0_norm":
                        return
                    self.conv_branch(hT, bh, cvT, bcv)
                    if self.stop == "m0_conv":
                        return
                    qT = self.sb(esg, "qT", [128, 2, T], BF16)
                    kT = self.sb(esg, "kT", [128, 2, T], BF16)
                    bqk = k.bufs(NT, "qk")
                    v_tm = self.sb(esg, "v_tm", [128, NT, 512], BF16)
                    sg_tm = self.sb(esg, "sg_tm", [128, NT, 512], BF16)
                    bvg = k.bufs(NT, "vg")
                    lrT = [self.sb(esg, "lrT", [32, T], F32) for _ in range(2)]
                    blr = k.bufs(2, "lrT")
                    self.gla_proj(hT, bh, qT, kT, bqk, v_tm, sg_tm, bvg, lrT, blr)
                    if self.stop == "m0_proj":
                        return
                aoT = self.sb(esm, "aoT", [128, 4, T], BF16)
                bao = k.bufs(NT, "aoT")
                self.gla_scan(qT, kT, bqk, v_tm, sg_tm, bvg, lrT, blr, aoT, bao)
                if self.stop == "m0_scan":
                    return
            with ExitStack() as es:
                W = self.sb(es, "wout", [128, 8, D], BF16)
                bW = k.buf("wout")
                k.dma("pool", W, I["ab_w_out"].rearrange("(c p) n -> p c n", p=128), [], [bW])

                class Mix:
                    def __getitem__(s, idx):
                        p_, kc, tsl = idx
                        return aoT[:, kc, tsl] if kc < 4 else cvT[:, kc - 4, tsl]
                self.out_proj(b, 0, W, bW, Mix(), lambda i: [bao[i], bcv[i]], T)

    def conv_branch(self, hT, bh, cvT, bcv):
        k, I = self.k, self.I
        with ExitStack() as es:
            W = self.sb(es, "wglu", [128, 8, 256], BF16)
            bW = k.buf("wglu")
            cw, bcw = self.load_const(es, "convw", I["convw"], [128, 4, 31])
            cvv, bcvv = self.load_const(es, "convv", I["convv"], [128, 3, 4])
            u = self.sb(es, "cu", [128, T], F32)
            bu = k.buf("cu")
            acc = self.sb(es, "cacc", [128, 4, T], F32)
            bacc = k.bufs(4, "cacc")
            sg = self.sb(es, "csg", [128, 512], F32)
            bsg = k.buf("csg")
            blks = blocks(0, S, 512) + blocks(S, T, 512)
            wg = I["w_glu"].rearrange("(c p) n -> p c n", p=128)
            for cc in range(4):
                k.dma("pool", W[:, :, 0:128], wg[:, :, cc * 128:(cc + 1) * 128], [], [bW])
                k.dma("pool", W[:, :, 128:256], wg[:, :, 512 + cc * 128:512 + (cc + 1) * 128], [], [bW])
                for bi, (t0, n) in enumerate(blks):
                    tiles = list(range(t0 // 128, (t0 + n) // 128))
                    bin_ = [bh[i] for i in tiles]
                    pa, pb = self.PS[2 * (bi % 2)], self.PS[2 * (bi % 2) + 1]
                    bpa, bpb = self.bPS[2 * (bi % 2)], self.bPS[2 * (bi % 2) + 1]
                    self.lin_fm(pa, bpa, W, bW, 0, 128, hT, bin_, t0, n)
                    self.lin_fm(pb, bpb, W, bW, 128, 128, hT, bin_, t0, n)
                    k.act(sg[:, 0:n], pb[:, 0:n], AF.Sigmoid, [bpb], [bsg])
                    k.v(lambda e: e.tensor_tensor(u[:, t0:t0 + n], pa[:, 0:n], sg[:, 0:n], ALU.mult), [bpa, bsg], [bu])
                a = acc[:, cc, :]
                k.v(lambda e: e.tensor_scalar(a, u, cw[:, cc, 15:16], cvv[:, 0, cc:cc + 1], ALU.mult, ALU.add),
                    [bu, bcw, bcvv], [bacc[cc]])
                for (lo, hi) in ((0, S), (S, T)):
                    for j in range(31):
                        d = j - 15
                        if d == 0:
                            continue
                        o0, o1 = lo + max(0, -d), hi - max(0, d)
                        k.v(lambda e: e.scalar_tensor_tensor(a[:, o0:o1], u[:, o0 + d:o1 + d], cw[:, cc, j:j + 1], a[:, o0:o1],
                                                             ALU.mult, ALU.add), [bu, bcw, bacc[cc]], [bacc[cc]])
            sq = self.sb(es, "csq", [128, 4, 256], F32)
            bsq = k.buf("csq")
            mean = self.sb(es, "cmean", [128, 256], F32)
            msq = self.sb(es, "cmsq", [128, 256], F32)
            rstd = self.sb(es, "crstd", [128, 256], F32)
            bst = k.buf("cstat")
            tmp = self.sb(es, "ctmp", [128, 256], F32)
            btmp = k.buf("ctmp")
            for bi, (t0, n) in enumerate(blocks(0, T, 256)):
                tiles = list(range(t0 // 128, (t0 + n) // 128))
                ps1, ps2 = self.PS[4], self.PS[5]
                for cc in range(4):
                    k.mm(ps1[:, 0:n], self.ones, acc[:, cc, t0:t0 + n], cc == 0, cc == 3, [self.bones, bacc[cc]], [self.bPS[4]])
                k.act(sq[:, :, 0:n], acc[:, :, t0:t0 + n], AF.Square, bacc, [bsq])
                for cc in range(4):
                    k.mm(ps2[:, 0:n], self.ones, sq[:, cc, 0:n], cc == 0, cc == 3, [self.bones, bsq], [self.bPS[5]])
                k.act(mean[:, 0:n], ps1[:, 0:n], AF.Copy, [self.bPS[4]], [bst], scale=1.0 / 512)
                k.v(lambda e: e.tensor_tensor(msq[:, 0:n], mean[:, 0:n], mean[:, 0:n], ALU.mult), [bst], [bst])
                k.v(lambda e: e.scalar_tensor_tensor(rstd[:, 0:n], ps2[:, 0:n], 1.0 / 512, msq[:, 0:n], ALU.mult, ALU.subtract),
                    [self.bPS[5], bst], [bst])
                k.act(rstd[:, 0:n], rstd[:, 0:n], AF.Sqrt, [bst], [bst], scale=1.0, bias=LN_EPS)
                k.v(lambda e: e.reciprocal(rstd[:, 0:n], rstd[:, 0:n]), [bst], [bst])
                for cc in range(4):
                    k.v(lambda e: e.tensor_tensor(tmp[:, 0:n], acc[:, cc, t0:t0 + n], mean[:, 0:n], ALU.subtract), [bacc[cc], bst], [btmp])
                    k.v(lambda e: e.tensor_tensor(tmp[:, 0:n], tmp[:, 0:n], rstd[:, 0:n], ALU.mult), [btmp, bst], [btmp])
                    k.act(cvT[:, cc, t0:t0 + n], tmp[:, 0:n], AF.Silu, [btmp, bcvv], [bcv[i] for i in tiles],
                          scale=cvv[:, 1, cc:cc + 1], bias=cvv[:, 2, cc:cc + 1])
            k.barrier()

    def gla_proj(self, hT, bh, qT, kT, bqk, v_tm, sg_tm, bvg, lrT, blr):
        k, I = self.k, self.I
        with ExitStack() as es:
            Wqk = self.sb(es, "wqk", [128, 8, 1024], BF16)
            bWqk = k.buf("wqk")
            k.dma("pool", Wqk, I["w_qk"].rearrange("(c p) n -> p c n", p=128), [], [bWqk])
            Wvg, bWvg = Wqk, bWqk
            Wlr = self.sb(es, "wlr", [128, 8, 32], BF16)
            bWlr = k.buf("wlr")
            k.dma("pool", Wlr, I["w_lr"].rearrange("(c p) n -> p c n", p=128), [], [bWlr])
            rope, brope = self.load_const(es, "rope", I["rope"][0:2].rearrange("a p t -> p a t"), [128, 2, S])
            t1 = self.sb(es, "rt1", [128, 512], F32)
            t2 = self.sb(es, "rt2", [128, 512], F32)
            bt = k.bufs(2, "rt")
            blks = blocks(0, S, 512) + blocks(S, T, 512)
            for hp in range(2):
                for bi, (t0, n) in enumerate(blks):
                    tiles = list(range(t0 // 128, (t0 + n) // 128))
                    bin_ = [bh[i] for i in tiles]
                    for qi, dst in enumerate((qT, kT)):
                        pa, pb = self.PS[2 * qi], self.PS[2 * qi + 1]
                        bpa, bpb = self.bPS[2 * qi], self.bPS[2 * qi + 1]
                        self.lin_fm(pa, bpa, Wqk, bWqk, qi * 256 + hp * 128, 128, hT, bin_, t0, n)
                        if t0 < S:
                            self.lin_fm(pb, bpb, Wqk, bWqk, 512 + qi * 256 + hp * 128, 128, hT, bin_, t0, n)
                            k.v(lambda e: e.tensor_tensor(t1[:, 0:n], pa[:, 0:n], rope[:, 0, t0:t0 + n], ALU.mult), [bpa, brope], [bt[0]])
                            k.v(lambda e: e.tensor_tensor(t2[:, 0:n], pb[:, 0:n], rope[:, 1, t0:t0 + n], ALU.mult), [bpb, brope], [bt[1]])
                            k.v(lambda e: e.tensor_tensor(dst[:, hp, t0:t0 + n], t1[:, 0:n], t2[:, 0:n], ALU.add), bt, [bqk[i] for i in tiles])
                        else:
                            k.act(dst[:, hp, t0:t0 + n], pa[:, 0:n], AF.Copy, [bpa], [bqk[i] for i in tiles])
            k.dma("pool", Wvg, I["w_vg"].rearrange("(c p) n -> p c n", p=128), [], [bWvg])
            for i in range(NT):
                pv, pg = self.PS[2 * (i % 2)], self.PS[2 * (i % 2) + 1]
                bpv, bpg = self.bPS[2 * (i % 2)], self.bPS[2 * (i % 2) + 1]
                for kc in range(8):
                    k.mm(pv, hT[:, kc, i * 128:(i + 1) * 128], Wvg[:, kc, 0:512], kc == 0, kc == 7, [bh[i], bWvg], [bpv])
                for kc in range(8):
                    k.mm(pg, hT[:, kc, i * 128:(i + 1) * 128], Wvg[:, kc, 512:1024], kc == 0, kc == 7, [bh[i], bWvg], [bpg])
                k.v(lambda e: e.tensor_copy(v_tm[:, i, :], pv), [bpv], [bvg[i]])
                k.act(sg_tm[:, i, :], pg, AF.Silu, [bpg], [bvg[i]])
            for d in range(2):
                k.v(lambda e: e.memset(lrT[d], 1.0), [], [blr[d]])
                for bi, (t0, n) in enumerate(blks):
                    tiles = list(range(t0 // 128, (t0 + n) // 128))
                    ps = self.PS[4 + bi % 2]
                    bps = self.bPS[4 + bi % 2]
                    self.lin_fm(ps, bps, Wlr, bWlr, d * 16, 16, hT, [bh[i] for i in tiles], t0, n)
                    k.v(lambda e: e.tensor_copy(lrT[d][0:16, t0:t0 + n], ps[0:16, 0:n]), [bps], [blr[d]])
            k.barrier()

    def gla_scan(self, qT, kT, bqk, v_tm, sg_tm, bvg, lrT, blr, aoT, bao):
        k, I = self.k, self.I
        with ExitStack() as es:
            dwb = [self.load_const(es, "dwb", I["dwb_f" if d == 0 else "dwb_b"], [17, 256]) for d in range(2)]
            tri, btri = self.load_const(es, "tri", I["tri"].rearrange("a p t -> p a t"), [128, 2, 128])
            am, bam = self.load_const(es, "amask", I["amask"].rearrange("a p t -> p a t"), [128, 2, 64])
            gg, bgg = self.load_const(es, "glag", I["gla_g"].to_broadcast([128, 512]), [128, 512])
            o_f = self.sb(es, "o_f", [128, NT, 512], F32)
            bof = k.bufs(NT, "o_f")
            S32 = self.sb(es, "S32", [128, 2, 128], F32)
            Sb = self.sb(es, "Sb", [128, 2, 128], BF16)
            bS = [[k.buf("S") for _ in range(2)] for _ in range(2)]
            ex = self.sb(es, "gex", [128, 256], F32)
            ll = self.sb(es, "gll", [128, 256], F32)
            bll = k.buf("gll")
            NB_ = 2
            bT = [[self.sb(es, "gbT", [128, 128], F32) for _ in range(2)] for _ in range(NB_)]
            E3 = [[self.sb(es, "gE3", [128, 128], F32) for _ in range(2)] for _ in range(NB_)]
            E1 = [[self.sb(es, "gE1", [128, 128], F32) for _ in range(2)] for _ in range(NB_)]
            E2 = [[self.sb(es, "gE2", [128, 128], F32) for _ in range(2)] for _ in range(NB_)]
            E4 = [[self.sb(es, "gE4", [128, 128], F32) for _ in range(2)] for _ in range(NB_)]
            qe = [[self.sb(es, "gqe", [128, 128], BF16) for _ in range(2)] for _ in range(NB_)]
            ke = [[self.sb(es, "gke", [128, 128], BF16) for _ in range(2)] for _ in range(NB_)]
            qi_ = [[self.sb(es, "gqi", [128, 128], BF16) for _ in range(2)] for _ in range(NB_)]
            koT = [[self.sb(es, "gko", [128, 128], BF16) for _ in range(2)] for _ in range(NB_)]
            ko_tm = [[self.sb(es, "gkt", [128, 128], BF16) for _ in range(2)] for _ in range(NB_)]
            bprep = [[k.buf("gprep") for _ in range(2)] for _ in range(NB_)]
            aTm = [self.sb(es, "gaTm", [128, 64], BF16) for _ in range(2)]
            baTm = k.bufs(2, "gaTm")
            osum = self.sb(es, "gosum", [128, 512], F32)
            bosum = k.buf("gosum")
            ssq = self.sb(es, "gssq", [128, 4], F32)
            junk = self.sb(es, "gjunk", [128, 128], F32)
            bssq = k.buf("gssq")
            ao = self.sb(es, "gao", [128, 512], F32)
            aob = self.sb(es, "gaob", [128, 512], BF16)
            bao_t = k.buf("gao")

            step = 0
            for d in range(2):
                for hp in range(2):
                    k.v(lambda e: e.memset(S32[:, hp, :], 0.0), [], bS[hp])
                    k.v(lambda e: e.memset(Sb[:, hp, :], 0.0), [], bS[hp])
                order = [16, 17] + list(range(16)) if d == 0 else [17, 16] + list(range(15, -1, -1))
                for i in order:
                    pp = step % NB_
                    step += 1
                    c0 = i * 128
                    px = self.PS[6]
                    k.mm(px[:, 0:256], lrT[d][0:17, c0:c0 + 128], dwb[d][0], True, True, [blr[d], dwb[d][1]], [self.bPS[6]])
                    k.act(ex, px[:, 0:256], AF.Exp, [self.bPS[6]], [bll], scale=-1.0)
                    k.act(ll, ex, AF.Ln, [bll], [bll], scale=1.0, bias=1.0)
                    if DBG < 2:
                        continue
                    for hp in range(2):
                        bp = bprep[pp][hp]
                        pb_ = self.PS[4 + hp]
                        k.mm(pb_[:, 0:128], ll[:, hp * 128:(hp + 1) * 128], tri[:, d, :], True, True, [bll, btri], [self.bPS[4 + hp]])
                        b_ = bT[pp][hp]
                        k.v(lambda e: e.tensor_copy(b_, pb_[:, 0:128]), [self.bPS[4 + hp]], [bp])
                        k.act(E3[pp][hp], b_, AF.Exp, [bp], [bp])
                        for ch in range(2):
                            cs = ch * 64
                            mid = cs + (31 if d == 0 else 32)
                            last = cs + (63 if d == 0 else 0)
                            k.act(E2[pp][hp][:, cs:cs + 64], b_[:, cs:cs + 64], AF.Exp, [bp], [bp], scale=-1.0, bias=b_[:, mid:mid + 1])
                            k.act(E4[pp][hp][:, cs:cs + 64], b_[:, cs:cs + 64], AF.Exp, [bp], [bp], scale=-1.0, bias=b_[:, last:last + 1])
                        k.v(lambda e: e.reciprocal(E1[pp][hp], E2[pp][hp]), [bp], [bp])
                        qs = qT[:, hp, c0:c0 + 128]
                        ks = kT[:, hp, c0:c0 + 128]
                        k.v(lambda e: e.tensor_tensor(qe[pp][hp], qs, E1[pp][hp], ALU.mult), [bp, bqk[i]], [bp])
                        k.v(lambda e: e.tensor_tensor(ke[pp][hp], ks, E2[pp][hp], ALU.mult), [bp, bqk[i]], [bp])
                        k.v(lambda e: e.tensor_tensor(qi_[pp][hp], qs, E3[pp][hp], ALU.mult), [bp, bqk[i]], [bp], eng="pool")
                        k.v(lambda e: e.tensor_tensor(koT[pp][hp], ks, E4[pp][hp], ALU.mult), [bp, bqk[i]], [bp], eng="pool")
                        if DBG < 3:
                            continue
                        ptr = self.PSB[:, hp * 128:(hp + 1) * 128]
                        k.tr(ptr, koT[pp][hp], self.identb, [bp, self.bidentb], [self.bPS[7]])
                        k.v(lambda e: e.tensor_copy(ko_tm[pp][hp], ptr), [self.bPS[7]], [bp])
                    if DBG < 4:
                        continue
                    for ch in ((0, 1) if d == 0 else (1, 0)):
                        cs = ch * 64
                        last = cs + (63 if d == 0 else 0)
                        for hp in range(2):
                            bp = bprep[pp][hp]
                            for h2 in range(2):
                                hd = hp * 2 + h2
                                pr = h2 * 64
                                pa = self.PS[0]
                                bpa = self.bPS[0]
                                am_ = aTm[hd % 2]
                                a0 = (hd % 2) * 64
                                k.mm(pa[cs:cs + 64, a0:a0 + 64], ke[pp][hp][pr:pr + 64, cs:cs + 64], qe[pp][hp][pr:pr + 64, cs:cs + 64],
                                     True, True, [bp], [bpa])
                                k.v(lambda e: e.tensor_tensor(am_[cs:cs + 64, :], pa[cs:cs + 64, a0:a0 + 64], am[cs:cs + 64, d, :], ALU.mult),
                                    [bpa, bam], [baTm[hd % 2]])
                                if DBG < 5:
                                    continue
                                po = self.PS[2]
                                bpo = self.bPS[2]
                                k.mm(po[cs:cs + 64, hd * 128:(hd + 1) * 128], am_[cs:cs + 64, :], v_tm[cs:cs + 64, i, hd * 128:(hd + 1) * 128],
                                     True, True, [baTm[hd % 2], bvg[i]], [bpo])
                                k.mm(self.PS[1][cs:cs + 64, hd * 128:(hd + 1) * 128], qi_[pp][hp][pr:pr + 64, cs:cs + 64], Sb[pr:pr + 64, hp, :],
                                     True, True, [bp, bS[hp][h2]], [self.bPS[1]])
                                if DBG < 6:
                                    continue
                                pS = self.PS[3]
                                bpS = self.bPS[3]
                                k.mm(pS[pr:pr + 64, hp * 128:(hp + 1) * 128], ko_tm[pp][hp][cs:cs + 64, pr:pr + 64],
                                     v_tm[cs:cs + 64, i, hd * 128:(hd + 1) * 128], True, True, [bp, bvg[i]], [bpS])
                                k.v(lambda e: e.scalar_tensor_tensor(S32[pr:pr + 64, hp, :], S32[pr:pr + 64, hp, :],
                                                                     E3[pp][hp][pr:pr + 64, last:last + 1],
                                                                     pS[pr:pr + 64, hp * 128:(hp + 1) * 128], ALU.mult, ALU.add),
                                    [bp, bpS, bS[hp][h2]], [bS[hp][h2]])
                                k.act(Sb[pr:pr + 64, hp, :], S32[pr:pr + 64, hp, :], AF.Copy, [bS[hp][h2]], [bS[hp][h2]])
                    if DBG < 7:
                        continue
                    po = self.PS[2]
                    bpo = self.bPS[2]
                    if d == 0:
                        k.act(o_f[:, i, :], po, AF.Copy, [bpo], [bof[i]])
                        k.v(lambda e: e.tensor_tensor(o_f[:, i, :], self.PS[1], o_f[:, i, :], ALU.add), [self.bPS[1], bof[i]], [bof[i]])
                    else:
                        k.v(lambda e: e.tensor_tensor(osum, po, o_f[:, i, :], ALU.add), [bpo, bof[i]], [bosum])
                        k.v(lambda e: e.tensor_tensor(osum, self.PS[1], osum, ALU.add), [self.bPS[1], bosum], [bosum])
                        for hd in range(4):
                            k.act(junk, osum[:, hd * 128:(hd + 1) * 128], AF.Square, [bosum], [bssq], accum_out=ssq[:, hd:hd + 1])
                        k.act(ssq, ssq, AF.Sqrt, [bssq], [bssq], scale=1.0 / (64.0 * 128.0), bias=RMS_EPS)
                        k.v(lambda e: e.reciprocal(ssq, ssq), [bssq], [bssq])
                        for hd in range(4):
                            k.v(lambda e: e.tensor_scalar(ao[:, hd * 128:(hd + 1) * 128], osum[:, hd * 128:(hd + 1) * 128],
                                                          ssq[:, hd:hd + 1], 0.125, ALU.mult, ALU.mult), [bosum, bssq], [bao_t])
                        k.v(lambda e: e.tensor_tensor(ao, ao, gg, ALU.mult), [bao_t, bgg], [bao_t])
                        k.v(lambda e: e.tensor_tensor(aob, ao, sg_tm[:, i, :], ALU.mult), [bao_t, bvg[i]], [bao_t])
                        for c in range(4):
                            ptr = self.PSB[:, 512 + c * 128:512 + (c + 1) * 128]
                            k.tr(ptr, aob[:, c * 128:(c + 1) * 128], self.identb, [bao_t, self.bidentb], [self.bPS[7]])
                        k.act(aoT[:, :, c0:c0 + 128], self.PSB[:, 512:1024].rearrange("p (c t) -> p c t", c=4), AF.Copy,
                              [self.bPS[7]], [bao[i]])
            k.barrier()

    def mixer1(self, b):
        k, I = self.k, self.I
        with ExitStack() as esm:
          with ExitStack() as esq:
            with ExitStack() as esh:
                hT = self.sb(esh, "hT", [128, 8, T], BF16)
                bh = k.bufs(NT, "hT")
                self.norm_mod(hT, bh, b, 1, 0, T)
                qT = self.sb(esq, "nqT", [128, 8, S], BF16)
                kT = self.sb(esq, "nkT", [128, 8, T], BF16)
                bq = k.bufs(NT, "nq")
                bk = k.bufs(NT, "nk")
                v_tm = self.sb(esq, "nv", [128, NT, D], BF16)
                bv = k.bufs(NT, "nv")
                v_sh = self.sb(esq, "nvs", [128, 15, D], BF16)
                bvs = k.bufs(15, "nvs")
                with ExitStack() as es:
                    w = self.sb(es, "nw", [128, 8, D], BF16)
                    bW = k.buf("nw")
                    for j in range(3):
                        k.dma("pool", w, I["na_w_in"][:, j * D:(j + 1) * D].rearrange("(c p) n -> p c n", p=128), [], [bW])
                        if j < 2:
                            dst, bd, tend = (qT, bq, S) if j == 0 else (kT, bk, T)
                            blks = blocks(0, min(tend, S), 512) + (blocks(S, tend, 512) if tend > S else [])
                            n_ = 0
                            for (t0, n) in blks:
                                tiles = list(range(t0 // 128, (t0 + n) // 128))
                                for m in range(8):
                                    ps = self.PS[n_ % 4]
                                    bps = self.bPS[n_ % 4]
                                    n_ += 1
                                    self.lin_fm(ps, bps, w, bW, m * 128, 128, hT, [bh[i] for i in tiles], t0, n)
                                    if m % 2 == 0:
                                        k.act(dst[:, m, t0:t0 + n], ps[:, 0:n], AF.Copy, [bps], [bd[i] for i in tiles],
                                              scale=(0.125 if j == 0 else 1.0))
                                    else:
                                        k.v(lambda e: e.tensor_scalar(dst[:, m, t0:t0 + n], ps[:, 0:n], (0.125 if j == 0 else 1.0), None, ALU.mult),
                                            [bps], [bd[i] for i in tiles])
                        else:
                            n_ = 0
                            for (dstv, bdv, ntl, toff) in ((v_tm, bv, NT, 0), (v_sh, bvs, 15, 64)):
                                for i in range(ntl):
                                    c0 = toff + i * 128
                                    rd = [bh[c0 // 128], bh[(c0 + 127) // 128], bW]
                                    for hf in range(2):
                                        ps = self.PS[n_ % 4]
                                        bps = self.bPS[n_ % 4]
                                        n_ += 1
                                        for kc in range(8):
                                            k.mm(ps, hT[:, kc, c0:c0 + 128], w[:, kc, hf * 512:(hf + 1) * 512], kc == 0, kc == 7, rd, [bps])
                                        if hf == 0:
                                            k.act(dstv[:, i, 0:512], ps, AF.Copy, [bps], [bdv[i]])
                                        else:
                                            k.v(lambda e: e.tensor_copy(dstv[:, i, 512:1024], ps), [bps], [bdv[i]])
                    k.barrier()
            atT = self.sb(esm, "natT", [128, 8, S], BF16)
            bat = k.bufs(16, "nat")
            self.na_attention(qT, kT, bq, bk, v_tm, bv, v_sh, bvs, atT, bat)
            esq.close()
          if True:
            with ExitStack() as es:
                W = self.sb(es, "nwout", [128, 8, D], BF16)
                bW = k.buf("nwout")
                k.dma("pool", W, I["na_w_out"].rearrange("(c p) n -> p c n", p=128), [], [bW])
                self.out_proj(b, 1, W, bW, atT, lambda i: [bat[i]], S)

    def na_attention(self, qT, kT, bq, bk, v_tm, bv, v_sh, bvs, atT, bat):
        k, I = self.k, self.I
        with ExitStack() as es:
            Bh = [self.sb(es, "nB", [64, 960], F32) for _ in range(2)]
            bBh = k.bufs(2, "nB")
            NP = 2
            sc = [self.sb(es, "nsc", [64, 768], F32) for _ in range(NP)]
            pe_ = [self.sb(es, "npe", [64, 768], F32) for _ in range(NP)]
            pb = [self.sb(es, "npb", [64, 768], BF16) for _ in range(NP)]
            st = [self.sb(es, "nst", [64, 4], F32) for _ in range(NP)]
            bsm = k.bufs(NP, "nsm")
            pT = [self.sb(es, "npT", [128, 7, 64], BF16) for _ in range(NP)]
            bpT = k.bufs(NP, "npT")
            u = 0
            for h in range(16):
                m, pr = h // 2, (h % 2) * 64
                B = Bh[h % 2]
                k.dma("sp", B, I["na_bias"][h], [], [bBh[h % 2]])
                for r in range(32):
                    p = u % NP
                    u += 1
                    r0 = min(max(r - 4, 0), 24)
                    s = r0 - r + 7
                    w0 = r0 * 64
                    ktiles = sorted(set(range(w0 // 128, (w0 + 511) // 128 + 1)))
                    qtile = r // 2
                    qs = qT[pr:pr + 64, m, r * 64:(r + 1) * 64]
                    ps_w, ps_c = self.PS[2 * p], self.PS[2 * p + 1]
                    bpw, bpc = self.bPS[2 * p], self.bPS[2 * p + 1]
                    k.mm(ps_w[0:64, 0:512], qs, kT[pr:pr + 64, m, w0:w0 + 512], True, True, [bq[qtile]] + [bk[i] for i in ktiles], [bpw])
                    k.mm(ps_c[0:64, 0:256], qs, kT[pr:pr + 64, m, S:T], True, True, [bq[qtile], bk[16], bk[17]], [bpc])
                    bs = bsm[p]
                    k.v(lambda e: e.tensor_tensor(sc[p][:, 0:512], ps_w[0:64, 0:512], B[:, s * 64:s * 64 + 512], ALU.add), [bpw, bBh[h % 2]], [bs])
                    k.act(sc[p][:, 512:768], ps_c[0:64, 0:256], AF.Copy, [bpc], [bs])
                    k.v(lambda e: e.reduce_max(st[p][:, 0:1], sc[p], AX.X), [bs], [bs])
                    k.v(lambda e: e.tensor_scalar(st[p][:, 1:2], st[p][:, 0:1], -1.0, None, ALU.mult), [bs], [bs])
                    k.act(pe_[p], sc[p], AF.Exp, [bs], [bs], scale=1.0, bias=st[p][:, 1:2], accum_out=st[p][:, 2:3])
                    k.v(lambda e: e.reciprocal(st[p][:, 3:4], st[p][:, 2:3]), [bs], [bs])
                    k.v(lambda e: e.tensor_scalar(pb[p], pe_[p], st[p][:, 3:4], None, ALU.mult), [bs], [bs])
                    if w0 % 128 == 0:
                        chunks = [(j * 128, v_tm, bv, w0 // 128 + j) for j in range(4)]
                    else:
                        chunks = [(j * 128, v_sh, bvs, (w0 - 64) // 128 + j) for j in range(4)]
                    chunks += [(512, v_tm, bv, 16), (640, v_tm, bv, 17)]
                    ptb = self.PSB[:, 0:384].rearrange("p (j q) -> p j q", q=64)
                    for j, (col, vt, bvt, ti_) in enumerate(chunks):
                        k.tr(ptb[:, j, :], pb[p][:, col:col + 128], self.identb[0:64, 0:64], [bs, self.bidentb], [self.bPS[7]])
                    k.v(lambda e: e.tensor_copy(pT[p][:, 0:6, :], ptb), [self.bPS[7]], [bpT[p]])
                    po = self.PS[4 + p]
                    bpo = self.bPS[4 + p]
                    for j, (col, vt, bvt, ti_) in enumerate(chunks):
                        k.mm(po[pr:pr + 64, 0:64], vt[:, ti_, h * 64:(h + 1) * 64], pT[p][:, j, :],
                             j == 0, j == 5, [bvt[ti_], bpT[p]], [bpo])
                    k.act(atT[pr:pr + 64, m, r * 64:(r + 1) * 64], po[pr:pr + 64, 0:64], AF.Copy, [bpo], [bat[qtile]])
            k.barrier()

    def moe_layer(self, l, t_end):
        k, I = self.k, self.I
        nbl = self.nbl
        ntl = t_end // 128
        NSH = self.NSH
        with ExitStack() as esr:
            rw, brw = self.load_const(esr, "rw", I["router_w"][l].rearrange("(c p) n -> p c n", p=128), [128, 8, E])
            rb, brb = self.load_const(esr, "rb", I["router_b"][l].to_broadcast([128, E]), [128, E])
            R = {}
            for nm, shp in (("sc", [128, E]), ("bi", [128, E]), ("m8", [128, 8, 8]), ("gs", [128, 8]), ("g8", [128, 8]),
                            ("pen", [128, 8]), ("mk", [128, E]), ("t8", [128, 8]), ("s01", [128, E]), ("gd", [128, E]),
                            ("gsum", [128, 2]), ("slotm", [128, E]), ("s8", [128, 8]), ("junk", [128, E]), ("selacc", [128, E])):
                R[nm] = self.sb(esr, "r" + nm, shp, F32)
            s01b = self.sb(esr, "rs01b", [128, E], BF16)
            saccb = self.sb(esr, "rsaccb", [128, E], BF16)
            bR = k.buf("rout")
            bsacc = k.buf("selacc")
            htm = [self.sb(esr, "htm", [128, D], BF16) for _ in range(2)]
            bhtm = k.bufs(2, "htm")
            k.v(lambda e: e.memset(R["selacc"], 0.0), [], [bsacc])
            k.v(lambda e: e.memset(saccb, 0.0), [], [bsacc])
            cnt = [0]
            for b in range(nbl):
                def route(tile, ti, ps, bps, h32, bh32, b=b):
                    g = b * NT + tile
                    k.act(R["sc"], ps, AF.Sigmoid, [bps], [bR])
                    k.v(lambda e: e.tensor_tensor(R["bi"], R["sc"], rb, ALU.add), [bR, brb], [bR])
                    for gg in range(8):
                        k.v(lambda e: e.max(out=R["m8"][:, gg, :], in_=R["bi"][:, gg * 32:(gg + 1) * 32]), [bR], [bR])
                    k.v(lambda e: e.tensor_tensor(R["gs"], R["m8"][:, :, 0], R["m8"][:, :, 1], ALU.add), [bR], [bR])
                    k.v(lambda e: e.max(out=R["g8"], in_=R["gs"]), [bR], [bR])
                    k.v(lambda e: e.tensor_scalar(R["pen"], R["gs"], R["g8"][:, 3:4], None, ALU.is_ge), [bR], [bR])
                    k.v(lambda e: e.tensor_scalar(R["pen"], R["pen"], -1.0, 1.0e4, ALU.add, ALU.mult), [bR], [bR])
                    for gg in range(8):
                        k.v(lambda e: e.tensor_scalar(R["mk"][:, gg * 32:(gg + 1) * 32], R["bi"][:, gg * 32:(gg + 1) * 32],
                                                      R["pen"][:, gg:gg + 1], None, ALU.add), [bR], [bR])
                    k.v(lambda e: e.max(out=R["t8"], in_=R["mk"]), [bR], [bR])
                    k.v(lambda e: e.tensor_scalar(R["s01"], R["mk"], R["t8"][:, 7:8], None, ALU.is_ge), [bR], [bR])
                    k.v(lambda e: e.tensor_tensor(R["gd"], R["s01"], R["sc"], ALU.mult), [bR], [bR])
                    k.v(lambda e: e.reduce_sum(R["gsum"][:, 0:1], R["gd"], AX.X), [bR], [bR])
                    k.v(lambda e: e.reciprocal(R["gsum"][:, 1:2], R["gsum"][:, 0:1]), [bR], [bR])
                    k.v(lambda e: e.tensor_scalar(R["gd"], R["gd"], R["gsum"][:, 1:2], 2.5, ALU.mult, ALU.mult), [bR], [bR])
                    k.v(lambda e: e.tensor_copy(s01b, R["s01"]), [bR], [bR])
                    pp_ = self.PS[2]
                    k.mm(pp_[:, 0:E], self.onesb, saccb, True, False, [self.bones, bsacc], [self.bPS[2]])
                    k.mm(pp_[:, 0:E], self.usb, s01b, False, True, [self.bones, bR], [self.bPS[2]])
                    k.v(lambda e: e.tensor_tensor(R["slotm"], pp_[:, 0:E], self.ebase1, ALU.add), [self.bPS[2]], [bR])
                    k.v(lambda e: e.tensor_tensor(R["slotm"], R["slotm"], R["s01"], ALU.mult), [bR], [bR])
                    k.v(lambda e: e.tensor_tensor(R["selacc"], R["selacc"], R["s01"], ALU.add), [bR, bsacc], [bsacc])
                    k.v(lambda e: e.tensor_copy(saccb, R["selacc"]), [bsacc], [bsacc])
                    k.v(lambda e: e.max(out=R["s8"], in_=R["slotm"]), [bR], [bR])
                    k.v(lambda e: e.tensor_scalar(self.slotI[:, g, :], R["s8"], -1.0, None, ALU.add), [bR], [self.bslot[g]])
                    if self.dbg_out is not None:
                        k.v(lambda e: e.tensor_copy(self.s8all[:, g, :], R["s8"]), [bR], [self.bslot[g]])
                    for kk in range(8):
                        k.v(lambda e: e.scalar_tensor_tensor(R["junk"], R["slotm"], R["s8"][:, kk:kk + 1], R["gd"], ALU.is_equal, ALU.mult,
                                                             accum_out=self.gate8[:, g, kk:kk + 1]), [bR], [self.bslot[g], bR])
                    p = cnt[0] % 2
                    cnt[0] += 1
                    pa, pb_ = self.PS[0], self.PS[1]
                    for c in range(8):
                        pst = (pa if c < 4 else pb_)[:, (c % 4) * 128:(c % 4 + 1) * 128]
                        k.tr(pst, h32[:, c, ti * 128:(ti + 1) * 128], self.ident, [bh32, self.bident], [self.bPS[c // 4]])
                    k.act(htm[p][:, 0:512], pa, AF.Copy, [self.bPS[0]], [bhtm[p]])
                    k.v(lambda e: e.tensor_copy(htm[p][:, 512:1024], pb_), [self.bPS[1]], [bhtm[p]])
                    for kk in range(8 if MDBG >= 2 else 0):
                        k.idma(self.xs, bass.IndirectOffsetOnAxis(ap=self.slotI[:, g, kk:kk + 1], axis=0), htm[p], None,
                               [bhtm[p], self.bslot[g]], [k.buf("xsw")])
                    k.dma("sp", self.xsh[g * 128:(g + 1) * 128, :], htm[p], [bhtm[p]], [k.buf("xsw")])

                self.norm_mod(None, None, b, l, 1, t_end, router=(rw, brw, route))
            k.barrier()

        if MDBG < 3:
            return
        with ExitStack() as es:
            Wgu = [self.sb(es, "wgu", [128, 8, 512], BF16) for _ in range(2)]
            Wd = [self.sb(es, "wd", [128, 2, D], BF16) for _ in range(2)]
            bWe = k.bufs(2, "we")
            xtm = [self.sb(es, "extm", [128, D], BF16) for _ in range(2)]
            bxtm = k.bufs(2, "extm")
            xTe = [self.sb(es, "exT", [128, 8, 512], BF16) for _ in range(2)]
            bxTe = k.bufs(2, "exT")
            sgl = self.sb(es, "msg", [128, 512], F32)
            bsgl = k.buf("msg")
            hid = [[self.sb(es, "mhid", [128, 512], BF16) for _ in range(2)] for _ in range(2)]
            bhid = k.bufs(2, "mhid")
            ytm = [self.sb(es, "eytm", [128, D], F32) for _ in range(2)]
            bytm = k.bufs(2, "eytm")
            elist = list(self.experts) + [E]
            xq = yq = bq_ = tq = 0
            for ei, e_ in enumerate(elist):
                p = ei % 2
                if e_ == E:
                    sg_, su_, sd_ = I["ws_gate"][l], I["ws_up"][l], I["ws_down"][l]
                    sblocks = []
                    for b in range(nbl):
                        sblocks += [(b * T + t0, n) for (t0, n) in blocks(0, t_end, 512)]
                else:
                    sg_, su_, sd_ = I["w_gate"][l, e_], I["w_up"][l, e_], I["w_down"][l, e_]
                    sblocks = [(e_ * CAP, CAP)]
                k.dma("pool", Wgu[p][:, :, 0:256], sg_.rearrange("(c p) n -> p c n", p=128), [], [bWe[p]])
                k.dma("pool", Wgu[p][:, :, 256:512], su_.rearrange("(c p) n -> p c n", p=128), [], [bWe[p]])
                k.dma("pool", Wd[p], sd_.rearrange("(c p) n -> p c n", p=128), [], [bWe[p]])
                for (r0, n) in sblocks:
                    xp = bq_ % 2
                    bq_ += 1
                    nst = n // 128
                    for st in range(nst):
                        q = xq % 2
                        xq += 1
                        xsrc = self.xsh if e_ == E else self.xs
                        k.dma("sp", xtm[q], xsrc[r0 + st * 128:r0 + (st + 1) * 128, :], [], [bxtm[q]])
                        for c in range(8):
                            k.tr(self.PSB[:, c * 128:(c + 1) * 128], xtm[q][:, c * 128:(c + 1) * 128], self.identb,
                                 [bxtm[q], self.bidentb], [self.bPS[7]])
                        k.act(xTe[xp][:, :, st * 128:(st + 1) * 128], self.PSB.rearrange("p (c t) -> p c t", c=8), AF.Copy,
                              [self.bPS[7]], [bxTe[xp]])
                    for m in range(2):
                        self.lin_fm(self.PS[m], self.bPS[m], Wgu[p], bWe[p], m * 128, 128, xTe[xp], [bxTe[xp]], 0, n)
                        self.lin_fm(self.PS[2 + m], self.bPS[2 + m], Wgu[p], bWe[p], 256 + m * 128, 128, xTe[xp], [bxTe[xp]], 0, n)
                    for m in range(2):
                        k.act(sgl[:, 0:n], self.PS[m][:, 0:n], AF.Silu, [self.bPS[m]], [bsgl])
                        k.v(lambda e: e.tensor_tensor(hid[xp][m][:, 0:n], sgl[:, 0:n], self.PS[2 + m][:, 0:n], ALU.mult),
                            [bsgl, self.bPS[2 + m]], [bhid[xp]])
                    for st in range(nst):
                        q = tq % 2
                        tq += 1
                        for hf in range(2):
                            py = self.PS[4 + yq % 3]
                            bpy = self.bPS[4 + yq % 3]
                            yq += 1
                            for m in range(2):
                                k.mm(py, hid[xp][m][:, st * 128:(st + 1) * 128], Wd[p][:, m, hf * 512:(hf + 1) * 512], m == 0, m == 1,
                                     [bhid[xp], bWe[p]], [bpy])
                            if hf == 0:
                                k.act(ytm[q][:, 0:512], py, AF.Copy, [bpy], [bytm[q]])
                            else:
                                k.v(lambda e: e.tensor_copy(ytm[q][:, 512:1024], py), [bpy], [bytm[q]])
                        rr = slice(r0 + st * 128, r0 + (st + 1) * 128)
                        if e_ == E:
                            k.dma("sp", self.ysh_d[rr, :], ytm[q], [bytm[q]], [k.buf("ysw")])
                        else:
                            for hf in range(2):
                                k.dma("sp", self.ys2[hf][rr, :], ytm[q][:, hf * 512:(hf + 1) * 512], [bytm[q]], [k.buf("ysw")])
            k.barrier()

        if MDBG < 4:
            return
        with ExitStack() as es:
            yg = [self.sb(es, "yg", [128, 8, D], F32) for _ in range(2)]
            byg = k.bufs(2, "yg")
            ysh = [self.sb(es, "ysh", [128, D], F32) for _ in range(2)]
            bysh = k.bufs(2, "ysh")
            acc = self.sb(es, "pacc", [128, D], F32)
            bacc = k.buf("pacc")
            ssq = self.sb(es, "pssq", [128, 2], F32)
            bss = k.buf("pssq")
            junk = self.sb(es, "pjunk", [128, D], F32)
            yn = self.sb(es, "pyn", [128, D], F32)
            byn = k.buf("pyn")
            xb = [self.sb(es, "px", [128, 8, 128], F32) for _ in range(2)]
            bxb = k.bufs(2, "px")
            it = 0
            for b in range(nbl):
                for tile in range(ntl):
                    g = b * NT + tile
                    p = it % 2
                    it += 1
                    v = b if tile < 16 else 2
                    G = self.modvec(l, 1, 2, v)
                    for kk in range(8):
                        for hf in range(2):
                            k.idma(yg[p][:, kk, hf * 512:(hf + 1) * 512], None, self.ys2[hf],
                                   bass.IndirectOffsetOnAxis(ap=self.slotI[:, g, kk:kk + 1], axis=0), [self.bslot[g]], [byg[p]])
                    k.dma("sp", ysh[p], self.ysh_d[g * 128:(g + 1) * 128, :], [], [bysh[p]])
                    k.dma("sp", xb[p], self.xres[b][:, :, tile * 128:(tile + 1) * 128], [self.bxres[b][tile]], [bxb[p]])
                    k.v(lambda e: e.scalar_tensor_tensor(acc, yg[p][:, 0, :], self.gate8[:, g, 0:1], ysh[p], ALU.mult, ALU.add),
                        [byg[p], bysh[p], self.bslot[g]], [bacc])
                    for kk in range(1, 8):
                        k.v(lambda e: e.scalar_tensor_tensor(acc, yg[p][:, kk, :], self.gate8[:, g, kk:kk + 1], acc, ALU.mult, ALU.add),
                            [byg[p], self.bslot[g], bacc], [bacc])
                    k.act(junk, acc, AF.Square, [bacc], [bss], accum_out=ssq[:, 0:1])
                    k.act(ssq[:, 1:2], ssq[:, 0:1], AF.Sqrt, [bss], [bss], scale=1.0 / D, bias=RMS_EPS)
                    k.v(lambda e: e.reciprocal(ssq[:, 1:2], ssq[:, 1:2]), [bss], [bss])
                    k.v(lambda e: e.tensor_scalar(yn, acc, ssq[:, 1:2], None, ALU.mult), [bss, bacc], [byn])
                    pa, pb_ = self.PS[2 * p], self.PS[2 * p + 1]
                    for c in range(8):
                        ps = (pa if c < 4 else pb_)[:, (c % 4) * 128:(c % 4 + 1) * 128]
                        k.tr(ps, yn[:, c * 128:(c + 1) * 128], self.ident, [byn, self.bident], [self.bPS[2 * p + c // 4]])
                    for c in range(8):
                        ps = (pa if c < 4 else pb_)[:, (c % 4) * 128:(c % 4 + 1) * 128]
                        k.v(lambda e: e.scalar_tensor_tensor(xb[p][:, c, :], ps, G[:, c:c + 1], xb[p][:, c, :], ALU.mult, ALU.add),
                            [self.bPS[2 * p + c // 4], self.bmod, bxb[p]], [bxb[p]])
                    k.dma("sp", self.xres[b][:, :, tile * 128:(tile + 1) * 128], xb[p], [bxb[p]], [self.bxres[b][tile]])
            k.barrier()


def host_consts():
    ident = np.eye(128, dtype=np.float32)
    s = np.arange(128)[:, None]
    t = np.arange(128)[None, :]
    same = (s // 64) == (t // 64)
    tri = np.stack([np.where(same & (s <= t), -1.0 / 16, 0.0), np.where(same & (s >= t), -1.0 / 16, 0.0)]).astype(np.float32)
    s6 = np.arange(64)[:, None]
    t6 = np.arange(64)[None, :]
    mf = (s6 <= t6).astype(np.float32)
    mb = (s6 >= t6).astype(np.float32)
    amask = np.stack([np.concatenate([mf, mf], 0), np.concatenate([mb, mb], 0)]).astype(np.float32)
    kk = np.arange(128) % 64
    f = kk % 16
    inv = (10000.0 ** (-(np.arange(16, dtype=np.float32)) / 16)).astype(np.float32)
    tt = np.arange(S)
    rows = (tt // 64).astype(np.float32)
    cols = (tt % 64).astype(np.float32)
    pos = np.where((kk < 32)[:, None], rows[None, :], cols[None, :]).astype(np.float32)
    ang = pos * inv[f][:, None]
    cosT = np.cos(ang).astype(np.float32)
    sinT = np.sin(ang).astype(np.float32)
    sign = np.where((kk % 32) < 16, -1.0, 1.0).astype(np.float32)[:, None]
    rope = np.stack([cosT, sinT * sign, cosT, sinT * sign]).astype(np.float32)
    ustrict = (s < t).astype(np.float32)
    ebase1 = (np.arange(E, dtype=np.float32) * CAP + 1.0)[None, :]
    return ident, tri, amask, rope, ustrict, ebase1


def na_bias_table(rpb):
    qc = np.arange(64)
    cstart = np.clip(qc - 8, 0, 64 - 16)
    kc = np.arange(64)
    ok = (kc[None, :] >= cstart[:, None]) & (kc[None, :] < cstart[:, None] + 16)
    idx = np.clip(kc[None, :] - qc[:, None] + 15, 0, 30)
    g = rpb[:, :, idx]
    g = np.transpose(g, (0, 2, 1, 3))
    out = np.where(ok[None, :, None, :], g, np.float32(NEG)).astype(np.float32)
    return np.ascontiguousarray(out.reshape(16, 64, 960))


def fm(vec):
    return np.ascontiguousarray(np.asarray(vec, np.float32).reshape(8, 128).T)


def prep_shared(inp):
    f32 = lambda a: np.ascontiguousarray(np.asarray(a, dtype=np.float32))
    ident, tri, amask, rope, ustrict, ebase1 = host_consts()
    w_in = f32(inp["ab_w_in"][0])
    q, kk_, v, g, lrf, lrb, ga, gb = np.split(w_in, np.cumsum([256, 256, 512, 512, 16, 16, 512, 512])[:-1], axis=1)
    i64 = np.arange(64)
    partner = np.where((i64 % 32) < 16, i64 + 16, i64 - 16)
    perm = (np.arange(256) // 64) * 64 + partner[np.arange(256) % 64]
    sh = {
        "ada_w": f32(inp["ada_w"]),
        "ada_bT": np.ascontiguousarray(f32(inp["ada_b"]).reshape(2, 48, 128).transpose(0, 2, 1)),
        "gvec": np.ascontiguousarray(np.stack([np.stack([fm(inp[n][l]) for n in ("g_pre_mix", "g_post_mix", "g_pre_ffn", "g_post_ffn")], 1)
                                               for l in range(2)])),
        "w_qk": np.ascontiguousarray(np.concatenate([q, kk_, q[:, perm], kk_[:, perm]], 1)),
        "w_vg": np.ascontiguousarray(np.concatenate([v, g], 1)),
        "w_lr": np.ascontiguousarray(np.concatenate([lrf, lrb], 1)),
        "w_glu": np.ascontiguousarray(np.concatenate([ga, gb], 1)),
        "ab_w_out": f32(inp["ab_w_out"][0]),
        "dwb_f": np.ascontiguousarray(np.concatenate([f32(inp["gla_dw_f"][0]), f32(inp["gla_db_f"][0])[None]], 0)),
        "dwb_b": np.ascontiguousarray(np.concatenate([f32(inp["gla_dw_b"][0]), f32(inp["gla_db_b"][0])[None]], 0)),
        "gla_g": f32(inp["gla_norm_g"][0])[None],
        "convw": np.ascontiguousarray(f32(inp["conv_w"][0]).reshape(31, 4, 128).transpose(2, 1, 0)),
        "convv": np.ascontiguousarray(np.stack([f32(inp[n][0]).reshape(4, 128).T for n in ("conv_b", "conv_ln_g", "conv_ln_b")], 1)),
        "rope": rope, "tri": tri, "amask": amask, "ident": ident, "ustrict": ustrict, "ebase1": ebase1,
        "na_w_in": f32(inp["na_w_in"][0]),
        "na_w_out": f32(inp["na_w_out"][0]),
        "na_bias": na_bias_table(f32(inp["na_rpb"][0])),
        "router_w": f32(inp["moe_router_w"]),
        "router_b": f32(inp["moe_router_b"])[:, None, :],
        "w_gate": f32(inp["moe_w_gate"]), "w_up": f32(inp["moe_w_up"]), "w_down": f32(inp["moe_w_down"]),
        "ws_gate": f32(inp["moe_ws_gate"]), "ws_up": f32(inp["moe_ws_up"]), "ws_down": f32(inp["moe_ws_down"]),
    }
    return sh


def core_inputs(inp, sh, b0, nbl):
    x = np.ascontiguousarray(np.asarray(inp["x"], np.float32)[b0:b0 + nbl])
    ctx = np.ascontiguousarray(np.asarray(inp["ctx"], np.float32)[b0:b0 + nbl])
    c = np.asarray(inp["c"], np.float32)
    cols = [c[b0 + j] if j < nbl else np.zeros(D, np.float32) for j in range(2)] + [np.asarray(inp["c_ctx"], np.float32), np.zeros(D, np.float32)]
    cvec = np.ascontiguousarray(np.stack([fm(v) for v in cols], axis=2))
    m = dict(sh)
    m.update({"x": x, "ctx": ctx, "cvec": cvec})
    return m


_PROG = {}


def kernel(**inputs):
    if "full" not in _PROG:
        _PROG["full"] = Prog()
    prog = _PROG["full"]
    sh = prep_shared(inputs)
    in_maps = [core_inputs(inputs, sh, 2 * c, 2) for c in range(8)]
    res = run_bass_kernel_spmd(prog.nc, in_maps, core_ids=list(range(8)))
    return np.concatenate([r["out"] for r in res.results], axis=0).astype(np.float32)
```

```python
from contextlib import ExitStack
import os
DBG = int(os.environ.get('GLA_DBG', '9'))
MDBG = int(os.environ.get('MOE_DBG', '9'))
import numpy as np
import concourse.bass as bass
import concourse.mybir as mybir
from concourse.bass_utils import run_bass_kernel_spmd

F32 = mybir.dt.float32
BF16 = mybir.dt.bfloat16
AF = mybir.ActivationFunctionType
ALU = mybir.AluOpType
AX = mybir.AxisListType

D = 1024
S = 2048
C = 256
T = S + C
NT = T // 128
NBL = 2
E = 256
RMS_EPS = 1e-6
LN_EPS = 1e-5
NEG = -30000.0
CAP = 384
U32 = mybir.dt.uint32


class Buf:
    __slots__ = ("name", "w", "r", "dsem")

    def __init__(self, name):
        self.name = name
        self.w = {}
        self.r = {}
        self.dsem = None


class KB:
    def __init__(self, nc):
        self.nc = nc
        self.E = {"pe": nc.tensor, "dve": nc.vector, "act": nc.scalar, "pool": nc.gpsimd, "sp": nc.sync}
        self.esem = {e: nc.alloc_semaphore("es_" + e) for e in self.E}
        self.ecnt = {e: 0 for e in self.E}
        self.seen = {e: {} for e in self.E}
        self.dcnt = {}
        self.dpool = []
        self.dnext = 0
        self.nbuf = 0
        self.allbufs = []

    def buf(self, name=None):
        self.nbuf += 1
        b = Buf("%s_%d" % (name or "b", self.nbuf))
        self.allbufs.append(b)
        return b

    def bufs(self, n, name="b"):
        return [self.buf(name) for _ in range(n)]

    def _wait(self, eng, deps):
        for key, (sem, val, src) in deps.items():
            if src == "pe" and eng == "pe":
                continue
            if src == "dma":
                val = self.dcnt[key[2:]]
            if self.seen[eng].get(key, 0) >= val:
                continue
            self.E[eng].wait_ge(sem, val)
            self.seen[eng][key] = val

    @staticmethod
    def _deps(reads, writes):
        deps = {}
        for b in reads:
            for k, t in b.w.items():
                if k not in deps or deps[k][1] < t[1]:
                    deps[k] = t
        for b in writes:
            for d in (b.w, b.r):
                for k, t in d.items():
                    if k not in deps or deps[k][1] < t[1]:
                        deps[k] = t
        return deps

    @staticmethod
    def _mark(tok, key, reads, writes):
        for b in writes:
            b.w = {key: tok}
            b.r = {}
        for b in reads:
            if b in writes:
                continue
            b.r[key] = tok

    def op(self, eng, fn, reads=(), writes=()):
        self._wait(eng, self._deps(reads, writes))
        ins = fn(self.E[eng])
        self.ecnt[eng] += 1
        ins.then_inc(self.esem[eng], 1)
        self._mark((self.esem[eng], self.ecnt[eng], eng), "e_" + eng, reads, writes)
        return ins

    def dma(self, q, out, in_, reads=(), writes=(), **kw):
        self._wait(q, self._deps(reads, writes))
        tgt = writes[0]
        if tgt.dsem is None:
            if len(self.dpool) < 72:
                nm = "ds%d" % len(self.dpool)
                self.dpool.append((self.nc.alloc_semaphore(nm), nm))
                self.dcnt[nm] = 0
            tgt.dsem = self.dpool[self.dnext % len(self.dpool)] if len(self.dpool) == 72 else self.dpool[-1]
            self.dnext += 1
        sem, nm = tgt.dsem
        ins = self.E[q].dma_start(out=out, in_=in_, **kw)
        self.dcnt[nm] += 16
        ins.then_inc(sem, 16)
        self._mark((sem, self.dcnt[nm], "dma"), "d_" + nm, reads, writes)
        return ins

    def idma(self, out, out_off, in_, in_off, reads=(), writes=()):
        self._wait("pool", self._deps(reads, writes))
        tgt = writes[0]
        if tgt.dsem is None:
            if len(self.dpool) < 72:
                nm = "ds%d" % len(self.dpool)
                self.dpool.append((self.nc.alloc_semaphore(nm), nm))
                self.dcnt[nm] = 0
            tgt.dsem = self.dpool[self.dnext % len(self.dpool)] if len(self.dpool) == 72 else self.dpool[-1]
            self.dnext += 1
        sem, nm = tgt.dsem
        ins = self.nc.gpsimd.indirect_dma_start(out=out, out_offset=out_off, in_=in_, in_offset=in_off)
        self.dcnt[nm] += 16
        ins.then_inc(sem, 16)
        self._mark((sem, self.dcnt[nm], "dma"), "d_" + nm, reads, writes)
        return ins

    def barrier(self):
        deps = {}
        for e in self.E:
            if self.ecnt[e]:
                deps["e_" + e] = (self.esem[e], self.ecnt[e], "x")
        for (sem, nm) in self.dpool:
            deps["d_" + nm] = (sem, self.dcnt[nm], "dma")
        self.allbufs = []
        for e in self.E:
            self._wait(e, {k: v for k, v in deps.items() if k != "e_" + e})

    def mm(self, out, lhsT, rhs, start, stop, reads, writes):
        return self.op("pe", lambda e: e.matmul(out, lhsT, rhs, start=start, stop=stop), reads, writes)

    def tr(self, out, in_, ident, reads, writes):
        return self.op("pe", lambda e: e.transpose(out, in_, ident), reads, writes)

    def act(self, out, in_, func, reads, writes, **kw):
        return self.op("act", lambda e: e.activation(out, in_, func, **kw), reads, writes)

    def v(self, fn, reads, writes, eng="dve"):
        return self.op(eng, fn, reads, writes)


def blocks(t0, t1, n):
    out = []
    while t0 < t1:
        m = min(n, t1 - t0)
        out.append((t0, m))
        t0 += m
    return out


class Prog:
    def __init__(self, stop="end", experts=None, nbl=NBL):
        self.stop = stop
        self.experts = list(range(E)) if experts is None else experts
        self.nbl = nbl
        nc = self.nc = bass.Bass("TRN2", target_bir_lowering=False)
        self.k = KB(nc)
        self.es = ExitStack()
        self.build()

    def din(self, name, shape, dt=F32):
        return self.nc.dram_tensor(name, list(shape), dt, kind="ExternalInput").ap()

    def sb(self, es, name, shape, dt):
        if not hasattr(self, "arena"):
            self.AW = 52800
            self.arena = self.nc.alloc_sbuf_tensor("arena", [128, self.AW], F32).ap()
            self.free_list = [(0, self.AW)]
        esz = 2 if dt == BF16 else 4
        nfree = int(np.prod(shape[1:]))
        words = (nfree * esz + 3) // 4
        words = (words + 7) // 8 * 8
        for idx, (o, n) in enumerate(self.free_list):
            if n >= words:
                break
        else:
            raise RuntimeError("SBUF arena exhausted allocating %s %s; free=%s" % (name, shape, self.free_list))
        if n == words:
            self.free_list.pop(idx)
        else:
            self.free_list[idx] = (o + words, n - words)
        P = shape[0]
        ap = self.arena[0:P, o:o + words]
        if dt != F32:
            ap = ap.bitcast(dt)
        ap = ap[:, 0:nfree]
        if len(shape) == 3:
            ap = ap.rearrange("p (a b) -> p a b", a=shape[1])
        elif len(shape) == 4:
            ap = ap.rearrange("p (a b c) -> p a b c", a=shape[1], b=shape[2])
        elif len(shape) == 5:
            ap = ap.rearrange("p (a b c d) -> p a b c d", a=shape[1], b=shape[2], c=shape[3])

        def _free():
            fl = self.free_list
            fl.append((o, words))
            fl.sort()
            merged = []
            for (a, n_) in fl:
                if merged and merged[-1][0] + merged[-1][1] == a:
                    merged[-1] = (merged[-1][0], merged[-1][1] + n_)
                else:
                    merged.append((a, n_))
            self.free_list[:] = merged
        es.callback(_free)
        return ap

    def load_const(self, es, name, src, shape, dt=F32, q="sp"):
        t = self.sb(es, name, shape, dt)
        b = self.k.buf(name)
        self.k.dma(q, t, src, [], [b])
        return t, b

    def build(self):
        nc, k = self.nc, self.k
        nbl = self.nbl
        I = self.I = {}
        I["x"] = self.din("x", [nbl, S, D])
        I["ctx"] = self.din("ctx", [nbl, C, D])
        I["cvec"] = self.din("cvec", [128, 8, 4])
        I["ada_w"] = self.din("ada_w", [2, D, 6 * D])
        I["ada_bT"] = self.din("ada_bT", [2, 128, 48])
        I["gvec"] = self.din("gvec", [2, 128, 4, 8])
        I["w_qk"] = self.din("w_qk", [D, 1024])
        I["w_vg"] = self.din("w_vg", [D, 1024])
        I["w_lr"] = self.din("w_lr", [D, 32])
        I["w_glu"] = self.din("w_glu", [D, 1024])
        I["ab_w_out"] = self.din("ab_w_out", [D, D])
        I["dwb_f"] = self.din("dwb_f", [17, 256])
        I["dwb_b"] = self.din("dwb_b", [17, 256])
        I["gla_g"] = self.din("gla_g", [1, 512])
        I["convw"] = self.din("convw", [128, 4, 31])
        I["convv"] = self.din("convv", [128, 3, 4])
        I["rope"] = self.din("rope", [4, 128, S])
        I["tri"] = self.din("tri", [2, 128, 128])
        I["amask"] = self.din("amask", [2, 128, 64])
        I["ident"] = self.din("ident", [128, 128])
        I["ustrict"] = self.din("ustrict", [128, 128])
        I["ebase1"] = self.din("ebase1", [1, E])
        I["na_w_in"] = self.din("na_w_in", [D, 3 * D])
        I["na_w_out"] = self.din("na_w_out", [D, D])
        I["na_bias"] = self.din("na_bias", [16, 64, 960])
        I["router_w"] = self.din("router_w", [2, D, E])
        I["router_b"] = self.din("router_b", [2, 1, E])
        ne = self.ne_decl = (max(self.experts) + 1) if self.experts else 1
        I["w_gate"] = self.din("w_gate", [2, ne, D, 256])
        I["w_up"] = self.din("w_up", [2, ne, D, 256])
        I["w_down"] = self.din("w_down", [2, ne, 256, D])
        I["ws_gate"] = self.din("ws_gate", [2, D, 256])
        I["ws_up"] = self.din("ws_up", [2, D, 256])
        I["ws_down"] = self.din("ws_down", [2, 256, D])
        self.out = nc.dram_tensor("out", [nbl, S, D], F32, kind="ExternalOutput").ap()
        self.xres = [nc.dram_tensor("xres%d" % b, [128, 8, T], F32).ap() for b in range(nbl)]
        self.bxres = [[k.buf("xres") for _ in range(NT)] for b in range(nbl)]
        self.bout = k.buf("out")
        self.NSH = E * CAP
        self.xs = nc.dram_tensor("xs_scr", [E * CAP, D], BF16).ap()
        self.xsh = nc.dram_tensor("xsh_scr", [nbl * NT * 128, D], BF16).ap()
        self.ys2 = [nc.dram_tensor("ys_scr%d" % hf, [E * CAP, 512], F32).ap() for hf in range(2)]
        self.ysh_d = nc.dram_tensor("ysh_scr", [nbl * NT * 128, D], F32).ap()

        self.PS = [nc.alloc_psum_tensor("ps%d" % i, [128, 512], F32).ap() for i in range(8)]
        self.bPS = k.bufs(8, "ps")
        self.PSB = self.PS[7].bitcast(BF16)

        es = self.es
        self.ident, self.bident = self.load_const(es, "ident", I["ident"], [128, 128])
        self.identb = self.sb(es, "identb", [128, 128], BF16)
        self.bidentb = k.buf("identb")
        k.v(lambda e: e.tensor_copy(self.identb, self.ident), [self.bident], [self.bidentb])
        self.ones = self.sb(es, "ones", [128, 128], F32)
        self.bones = k.buf("ones")
        k.v(lambda e: e.memset(self.ones, 1.0), [], [self.bones])

        self.onesb = self.sb(es, "onesb", [128, 128], BF16)
        k.v(lambda e: e.memset(self.onesb, 1.0), [], [self.bones])
        us32, bus = self.load_const(es, "us32", I["ustrict"], [128, 128])
        self.usb = self.sb(es, "usb", [128, 128], BF16)
        k.v(lambda e: e.tensor_copy(self.usb, us32), [bus], [self.bones])
        self.ebase1, _ = self.load_const(es, "ebase1", I["ebase1"].to_broadcast([128, E]), [128, E])
        self.slotI = self.sb(es, "slotI", [128, nbl * NT, 8], U32)
        self.gate8 = self.sb(es, "gate8", [128, nbl * NT, 8], F32)
        self.bslot = k.bufs(nbl * NT, "slot")
        self.dbg_out = None
        if os.environ.get("SLOT_DBG"):
            self.dbg_out = nc.dram_tensor("dbg", [128, nbl * NT * 8], F32, kind="ExternalOutput").ap()
            self.s8all = self.sb(es, "s8all", [128, nbl * NT, 8], F32)
            k.v(lambda e: e.memset(self.s8all, 0.0), [], [self.bones])

        self.compute_mods()
        for b in range(nbl):
            self.load_x(b)
        if self.stop != "load":
            for l in range(2):
                for b in range(nbl):
                    (self.mixer0 if l == 0 else self.mixer1)(b)
                if self.stop in ("mix%d" % l, "m0_norm", "m0_conv", "m0_proj", "m0_scan"):
                    break
                self.moe_layer(l, T if l == 0 else S)
                if self.stop == "moe%d" % l:
                    break
        if self.dbg_out is not None:
            k.barrier()
            k.dma("sp", self.dbg_out, self.s8all.rearrange("p a b -> p (a b)"), [], [self.bout])
        for b in range(nbl):
            self.dump_x(b)
        deps = dict(self.bout.w)
        k._wait("sp", deps)
        es.close()

    def compute_mods(self):
        nc, k, I = self.nc, self.k, self.I
        self.mod = []
        self.bmod = k.buf("mod")
        self.modv = self.sb(self.es, "modv", [128, 2, 6, 3, 8], F32)
        with ExitStack() as es:
            cv, bcv = self.load_const(es, "cvec", I["cvec"], [128, 8, 4])
            sc = self.sb(es, "silc", [128, 8, 4], F32)
            bsc = k.buf("silc")
            k.act(sc, cv, AF.Silu, [bcv], [bsc])
            wbuf = [self.sb(es, "adaw", [128, 8, 512], F32) for _ in range(2)]
            bw = k.bufs(2, "adaw")
            gv, bgv = self.load_const(es, "gvec", I["gvec"].rearrange("l p a c -> p l a c"), [128, 2, 4, 8])
            abt, babt = self.load_const(es, "adab", I["ada_bT"].rearrange("l p j -> p l j"), [128, 2, 48])
            modraw = self.sb(es, "modraw", [128, 2, 3, 48], F32)
            bmr = k.buf("modraw")
            ps = self.PS[0]
            bps = self.bPS[0]
            i = 0
            for l in range(2):
                for ch in range(12):
                    w = wbuf[i % 2]
                    k.dma("sp", w, I["ada_w"][l, :, ch * 512:(ch + 1) * 512].rearrange("(c p) n -> p c n", p=128),
                          [], [bw[i % 2]])
                    for jj in range(4):
                        j = ch * 4 + jj
                        for kc in range(8):
                            k.mm(ps[:, j * 4:(j + 1) * 4], w[:, kc, jj * 128:(jj + 1) * 128], sc[:, kc, :],
                                 kc == 0, kc == 7, [bw[i % 2], bsc], [bps])
                    i += 1
                for v in range(3):
                    k.v(lambda e: e.tensor_tensor(modraw[:, l, v, :],
                                                  ps[:, 0:192].rearrange("p (j v) -> p j v", v=4)[:, :, v],
                                                  abt[:, l, :], ALU.add), [bps, babt], [bmr])
            mv = self.modv
            for l in range(2):
                for v in range(3):
                    for s_ in range(2):
                        sh = modraw[:, l, v, (3 * s_ + 0) * 8:(3 * s_ + 0) * 8 + 8]
                        scl = modraw[:, l, v, (3 * s_ + 1) * 8:(3 * s_ + 1) * 8 + 8]
                        gt = modraw[:, l, v, (3 * s_ + 2) * 8:(3 * s_ + 2) * 8 + 8]
                        gpre = gv[:, l, 2 * s_ + 0, :]
                        gpost = gv[:, l, 2 * s_ + 1, :]
                        A = mv[:, l, 3 * s_ + 0, v, :]
                        k.v(lambda e: e.scalar_tensor_tensor(A, scl, 1.0, gpre, ALU.add, ALU.mult), [bmr, bgv], [self.bmod])
                        k.v(lambda e: e.tensor_copy(mv[:, l, 3 * s_ + 1, v, :], sh), [bmr], [self.bmod])
                        k.v(lambda e: e.tensor_tensor(mv[:, l, 3 * s_ + 2, v, :], gt, gpost, ALU.mult), [bmr, bgv], [self.bmod])
            k.barrier()

    def modvec(self, l, s_, kind, v):
        return self.modv[:, l, 3 * s_ + kind, v, :]

    def load_x(self, b):
        k, I = self.k, self.I
        with ExitStack() as es:
            xin = [self.sb(es, "xin", [128, D], F32) for _ in range(2)]
            bxin = k.bufs(2, "xin")
            xt = [self.sb(es, "xt", [128, 8, 128], F32) for _ in range(2)]
            bxt = k.bufs(2, "xt")
            for i in range(NT):
                src = I["x"][b, i * 128:(i + 1) * 128, :] if i < 16 else I["ctx"][b, (i - 16) * 128:(i - 15) * 128, :]
                p = i % 2
                k.dma("sp", xin[p], src, [], [bxin[p]])
                pa, pb = self.PS[2 * p], self.PS[2 * p + 1]
                for c in range(8):
                    ps = (pa if c < 4 else pb)[:, (c % 4) * 128:(c % 4 + 1) * 128]
                    k.tr(ps, xin[p][:, c * 128:(c + 1) * 128], self.ident, [bxin[p], self.bident],
                         [self.bPS[2 * p + (c // 4)]])
                k.act(xt[p][:, 0:4, :], pa.rearrange("p (c t) -> p c t", c=4), AF.Copy, [self.bPS[2 * p]], [bxt[p]])
                k.v(lambda e: e.tensor_copy(xt[p][:, 4:8, :], pb.rearrange("p (c t) -> p c t", c=4)),
                    [self.bPS[2 * p + 1]], [bxt[p]])
                k.dma("sp", self.xres[b][:, :, i * 128:(i + 1) * 128], xt[p], [bxt[p]], [self.bxres[b][i]])
            k.barrier()

    def dump_x(self, b):
        k = self.k
        with ExitStack() as es:
            xt = [self.sb(es, "dxt", [128, 8, 128], F32) for _ in range(2)]
            bxt = k.bufs(2, "dxt")
            xo = [self.sb(es, "dxo", [128, D], F32) for _ in range(2)]
            bxo = k.bufs(2, "dxo")
            for i in range(16):
                p = i % 2
                k.dma("sp", xt[p], self.xres[b][:, :, i * 128:(i + 1) * 128], [self.bxres[b][i]], [bxt[p]])
                pa, pb = self.PS[2 * p], self.PS[2 * p + 1]
                for c in range(8):
                    ps = (pa if c < 4 else pb)[:, (c % 4) * 128:(c % 4 + 1) * 128]
                    k.tr(ps, xt[p][:, c, :], self.ident, [bxt[p], self.bident], [self.bPS[2 * p + (c // 4)]])
                k.act(xo[p][:, 0:512], pa, AF.Copy, [self.bPS[2 * p]], [bxo[p]])
                k.v(lambda e: e.tensor_copy(xo[p][:, 512:1024], pb), [self.bPS[2 * p + 1]], [bxo[p]])
                k.dma("sp", self.out[b, i * 128:(i + 1) * 128, :], xo[p], [bxo[p]], [self.bout])
            k.barrier()

    def norm_mod(self, hT, bh, b, l, s_, t_end, router=None):
        k = self.k
        NBK = 256
        with ExitStack() as es:
            xb = [self.sb(es, "nx", [128, 8, NBK], F32) for _ in range(2)]
            bxb = k.bufs(2, "nx")
            sq = self.sb(es, "nsq", [128, 8, NBK], F32)
            bsq = k.buf("nsq")
            rs = self.sb(es, "nrs", [128, NBK], F32)
            brs = k.buf("nrs")
            tmp = self.sb(es, "ntmp", [128, NBK], F32)
            btmp = k.buf("ntmp")
            h32 = self.sb(es, "nh32", [128, 8, NBK], F32) if router else None
            bh32 = k.buf("nh32")
            blks = blocks(0, min(t_end, S), NBK) + (blocks(S, t_end, NBK) if t_end > S else [])
            for bi, (t0, n) in enumerate(blks):
                v = b if t0 < S else 2
                A = self.modvec(l, s_, 0, v)
                sh = self.modvec(l, s_, 1, v)
                p = bi % 2
                tiles = list(range(t0 // 128, (t0 + n) // 128))
                k.dma("sp", xb[p][:, :, 0:n], self.xres[b][:, :, t0:t0 + n], [self.bxres[b][i] for i in tiles], [bxb[p]])
                k.act(sq[:, :, 0:n], xb[p][:, :, 0:n], AF.Square, [bxb[p]], [bsq])
                ps = self.PS[6]
                for c in range(8):
                    k.mm(ps[:, 0:n], self.ones, sq[:, c, 0:n], c == 0, c == 7, [self.bones, bsq], [self.bPS[6]])
                k.act(rs[:, 0:n], ps[:, 0:n], AF.Sqrt, [self.bPS[6]], [brs], scale=1.0 / D, bias=RMS_EPS)
                k.v(lambda e: e.reciprocal(rs[:, 0:n], rs[:, 0:n]), [brs], [brs])
                for c in range(8):
                    k.v(lambda e: e.tensor_tensor(tmp[:, 0:n], xb[p][:, c, 0:n], rs[:, 0:n], ALU.mult), [bxb[p], brs], [btmp])
                    if router:
                        k.act(h32[:, c, 0:n], tmp[:, 0:n], AF.Identity, [btmp, self.bmod], [bh32],
                              scale=A[:, c:c + 1], bias=sh[:, c:c + 1])
                    else:
                        k.act(hT[:, c, t0:t0 + n], tmp[:, 0:n], AF.Identity, [btmp, self.bmod], [bh[i] for i in tiles],
                              scale=A[:, c:c + 1], bias=sh[:, c:c + 1])
                if router:
                    rw, brw, cb = router
                    for ti, tile in enumerate(tiles):
                        pr = self.PS[4 + ti % 2]
                        bpr = self.bPS[4 + ti % 2]
                        for c in range(8):
                            k.mm(pr[:, 0:E], h32[:, c, ti * 128:(ti + 1) * 128], rw[:, c, :], c == 0, c == 7, [bh32, brw], [bpr])
                        cb(tile, ti, pr[:, 0:E], bpr, h32, bh32)
            k.barrier()

    def lin_fm(self, ps, bps, W, bW, col0, M, inT, bin_, t0, n, nk=8):
        for kc in range(nk):
            self.k.mm(ps[0:M, 0:n], W[:, kc, col0:col0 + M], inT[:, kc, t0:t0 + n], kc == 0, kc == nk - 1,
                      [bW] + bin_, [bps])

    def out_proj(self, b, l, W, bW, mixT, bmix_of_tile, t_end):
        k = self.k
        with ExitStack() as es:
            yb = self.sb(es, "oy", [128, 8, 256], F32)
            byb = k.buf("oy")
            sq = self.sb(es, "osq", [128, 8, 256], F32)
            bsq = k.buf("osq")
            rs = self.sb(es, "ors", [128, 256], F32)
            brs = k.buf("ors")
            xb = [self.sb(es, "ox", [128, 8, 256], F32) for _ in range(2)]
            bxb = k.bufs(2, "ox")
            tmp = self.sb(es, "otmp", [128, 256], F32)
            btmp = k.buf("otmp")
            blks = blocks(0, min(t_end, S), 256) + (blocks(S, t_end, 256) if t_end > S else [])
            for bi, (t0, n) in enumerate(blks):
                v = b if t0 < S else 2
                G = self.modvec(l, 0, 2, v)
                p = bi % 2
                tiles = list(range(t0 // 128, (t0 + n) // 128))
                bin_ = [bb for i in tiles for bb in bmix_of_tile(i)]
                k.dma("sp", xb[p], self.xres[b][:, :, t0:t0 + n], [self.bxres[b][i] for i in tiles], [bxb[p]])
                for m in range(8):
                    ps = self.PS[m // 2][:, (m % 2) * 256:(m % 2) * 256 + 256]
                    self.lin_fm(ps, self.bPS[m // 2], W, bW, m * 128, 128, mixT, bin_, t0, n)
                for q in range(4):
                    eng = "act" if q % 2 == 0 else "dve"
                    src = self.PS[q].rearrange("p (c t) -> p c t", c=2)
                    if eng == "act":
                        k.act(yb[:, 2 * q:2 * q + 2, :], src, AF.Copy, [self.bPS[q]], [byb])
                    else:
                        k.v(lambda e: e.tensor_copy(yb[:, 2 * q:2 * q + 2, :], src), [self.bPS[q]], [byb])
                k.act(sq, yb, AF.Square, [byb], [bsq])
                ps = self.PS[6]
                for c in range(8):
                    k.mm(ps[:, 0:n], self.ones, sq[:, c, :], c == 0, c == 7, [self.bones, bsq], [self.bPS[6]])
                k.act(rs, ps[:, 0:n], AF.Sqrt, [self.bPS[6]], [brs], scale=1.0 / D, bias=RMS_EPS)
                k.v(lambda e: e.reciprocal(rs, rs), [brs], [brs])
                for c in range(8):
                    k.v(lambda e: e.tensor_tensor(tmp, yb[:, c, :], rs, ALU.mult), [byb, brs], [btmp])
                    k.v(lambda e: e.scalar_tensor_tensor(xb[p][:, c, :], tmp, G[:, c:c + 1], xb[p][:, c, :], ALU.mult, ALU.add),
                        [btmp, self.bmod, bxb[p]], [bxb[p]])
                k.dma("sp", self.xres[b][:, :, t0:t0 + n], xb[p], [bxb[p]], [self.bxres[b][i] for i in tiles])
            k.barrier()

    def mixer0(self, b):
        nc, k, I = self.nc, self.k, self.I
        with ExitStack() as esm:
            cvT = self.sb(esm, "cvT", [128, 4, T], BF16)
            bcv = k.bufs(NT, "cvT")
            with ExitStack() as esg:
                with ExitStack() as esh:
                    hT = self.sb(esh, "hT", [128, 8, T], BF16)
                    bh = k.bufs(NT, "hT")
                    self.norm_mod(hT, bh, b, 0, 0, T)
                    if self.stop == "m0_norm":
                        return
                    self.conv_branch(hT, bh, cvT, bcv)
                    if self.stop == "m0_conv":
                        return
                    qT = self.sb(esg, "qT", [128, 2, T], BF16)
                    kT = self.sb(esg, "kT", [128, 2, T], BF16)
                    bqk = k.bufs(NT, "qk")
                    v_tm = self.sb(esg, "v_tm", [128, NT, 512], BF16)
                    sg_tm = self.sb(esg, "sg_tm", [128, NT, 512], BF16)
                    bvg = k.bufs(NT, "vg")
                    lrT = [self.sb(esg, "lrT", [32, T], F32) for _ in range(2)]
                    blr = k.bufs(2, "lrT")
                    self.gla_proj(hT, bh, qT, kT, bqk, v_tm, sg_tm, bvg, lrT, blr)
                    if self.stop == "m0_proj":
                        return
                aoT = self.sb(esm, "aoT", [128, 4, T], BF16)
                bao = k.bufs(NT, "aoT")
                self.gla_scan(qT, kT, bqk, v_tm, sg_tm, bvg, lrT, blr, aoT, bao)
                if self.stop == "m0_scan":
                    return
            with ExitStack() as es:
                W = self.sb(es, "wout", [128, 8, D], BF16)
                bW = k.buf("wout")
                k.dma("pool", W, I["ab_w_out"].rearrange("(c p) n -> p c n", p=128), [], [bW])

                class Mix:
                    def __getitem__(s, idx):
                        p_, kc, tsl = idx
                        return aoT[:, kc, tsl] if kc < 4 else cvT[:, kc - 4, tsl]
                self.out_proj(b, 0, W, bW, Mix(), lambda i: [bao[i], bcv[i]], T)

    def conv_branch(self, hT, bh, cvT, bcv):
        k, I = self.k, self.I
        with ExitStack() as es:
            W = self.sb(es, "wglu", [128, 8, 256], BF16)
            bW = k.buf("wglu")
            cw, bcw = self.load_const(es, "convw", I["convw"], [128, 4, 31])
            cvv, bcvv = self.load_const(es, "convv", I["convv"], [128, 3, 4])
            u = self.sb(es, "cu", [128, T], F32)
            bu = k.buf("cu")
            acc = self.sb(es, "cacc", [128, 4, T], F32)
            bacc = k.bufs(4, "cacc")
            sg = self.sb(es, "csg", [128, 512], F32)
            bsg = k.buf("csg")
            blks = blocks(0, S, 512) + blocks(S, T, 512)
            wg = I["w_glu"].rearrange("(c p) n -> p c n", p=128)
            for cc in range(4):
                k.dma("pool", W[:, :, 0:128], wg[:, :, cc * 128:(cc + 1) * 128], [], [bW])
                k.dma("pool", W[:, :, 128:256], wg[:, :, 512 + cc * 128:512 + (cc + 1) * 128], [], [bW])
                for bi, (t0, n) in enumerate(blks):
                    tiles = list(range(t0 // 128, (t0 + n) // 128))
                    bin_ = [bh[i] for i in tiles]
                    pa, pb = self.PS[2 * (bi % 2)], self.PS[2 * (bi % 2) + 1]
                    bpa, bpb = self.bPS[2 * (bi % 2)], self.bPS[2 * (bi % 2) + 1]
                    self.lin_fm(pa, bpa, W, bW, 0, 128, hT, bin_, t0, n)
                    self.lin_fm(pb, bpb, W, bW, 128, 128, hT, bin_, t0, n)
                    k.act(sg[:, 0:n], pb[:, 0:n], AF.Sigmoid, [bpb], [bsg])
                    k.v(lambda e: e.tensor_tensor(u[:, t0:t0 + n], pa[:, 0:n], sg[:, 0:n], ALU.mult), [bpa, bsg], [bu])
                a = acc[:, cc, :]
                k.v(lambda e: e.tensor_scalar(a, u, cw[:, cc, 15:16], cvv[:, 0, cc:cc + 1], ALU.mult, ALU.add),
                    [bu, bcw, bcvv], [bacc[cc]])
                for (lo, hi) in ((0, S), (S, T)):
                    for j in range(31):
                        d = j - 15
                        if d == 0:
                            continue
                        o0, o1 = lo + max(0, -d), hi - max(0, d)
                        k.v(lambda e: e.scalar_tensor_tensor(a[:, o0:o1], u[:, o0 + d:o1 + d], cw[:, cc, j:j + 1], a[:, o0:o1],
                                                             ALU.mult, ALU.add), [bu, bcw, bacc[cc]], [bacc[cc]])
            sq = self.sb(es, "csq", [128, 4, 256], F32)
            bsq = k.buf("csq")
            mean = self.sb(es, "cmean", [128, 256], F32)
            msq = self.sb(es, "cmsq", [128, 256], F32)
            rstd = self.sb(es, "crstd", [128, 256], F32)
            bst = k.buf("cstat")
            tmp = self.sb(es, "ctmp", [128, 256], F32)
            btmp = k.buf("ctmp")
            for bi, (t0, n) in enumerate(blocks(0, T, 256)):
                tiles = list(range(t0 // 128, (t0 + n) // 128))
                ps1, ps2 = self.PS[4], self.PS[5]
                for cc in range(4):
                    k.mm(ps1[:, 0:n], self.ones, acc[:, cc, t0:t0 + n], cc == 0, cc == 3, [self.bones, bacc[cc]], [self.bPS[4]])
                k.act(sq[:, :, 0:n], acc[:, :, t0:t0 + n], AF.Square, bacc, [bsq])
                for cc in range(4):
                    k.mm(ps2[:, 0:n], self.ones, sq[:, cc, 0:n], cc == 0, cc == 3, [self.bones, bsq], [self.bPS[5]])
                k.act(mean[:, 0:n], ps1[:, 0:n], AF.Copy, [self.bPS[4]], [bst], scale=1.0 / 512)
                k.v(lambda e: e.tensor_tensor(msq[:, 0:n], mean[:, 0:n], mean[:, 0:n], ALU.mult), [bst], [bst])
                k.v(lambda e: e.scalar_tensor_tensor(rstd[:, 0:n], ps2[:, 0:n], 1.0 / 512, msq[:, 0:n], ALU.mult, ALU.subtract),
                    [self.bPS[5], bst], [bst])
                k.act(rstd[:, 0:n], rstd[:, 0:n], AF.Sqrt, [bst], [bst], scale=1.0, bias=LN_EPS)
                k.v(lambda e: e.reciprocal(rstd[:, 0:n], rstd[:, 0:n]), [bst], [bst])
                for cc in range(4):
                    k.v(lambda e: e.tensor_tensor(tmp[:, 0:n], acc[:, cc, t0:t0 + n], mean[:, 0:n], ALU.subtract), [bacc[cc], bst], [btmp])
                    k.v(lambda e: e.tensor_tensor(tmp[:, 0:n], tmp[:, 0:n], rstd[:, 0:n], ALU.mult), [btmp, bst], [btmp])
                    k.act(cvT[:, cc, t0:t0 + n], tmp[:, 0:n], AF.Silu, [btmp, bcvv], [bcv[i] for i in tiles],
                          scale=cvv[:, 1, cc:cc + 1], bias=cvv[:, 2, cc:cc + 1])
            k.barrier()

    def gla_proj(self, hT, bh, qT, kT, bqk, v_tm, sg_tm, bvg, lrT, blr):
        k, I = self.k, self.I
        with ExitStack() as es:
            Wqk = self.sb(es, "wqk", [128, 8, 1024], BF16)
            bWqk = k.buf("wqk")
            k.dma("pool", Wqk, I["w_qk"].rearrange("(c p) n -> p c n", p=128), [], [bWqk])
            Wvg, bWvg = Wqk, bWqk
            Wlr = self.sb(es, "wlr", [128, 8, 32], BF16)
            bWlr = k.buf("wlr")
            k.dma("pool", Wlr, I["w_lr"].rearrange("(c p) n -> p c n", p=128), [], [bWlr])
            rope, brope = self.load_const(es, "rope", I["rope"][0:2].rearrange("a p t -> p a t"), [128, 2, S])
            t1 = self.sb(es, "rt1", [128, 512], F32)
            t2 = self.sb(es, "rt2", [128, 512], F32)
            bt = k.bufs(2, "rt")
            blks = blocks(0, S, 512) + blocks(S, T, 512)
            for hp in range(2):
                for bi, (t0, n) in enumerate(blks):
                    tiles = list(range(t0 // 128, (t0 + n) // 128))
                    bin_ = [bh[i] for i in tiles]
                    for qi, dst in enumerate((qT, kT)):
                        pa, pb = self.PS[2 * qi], self.PS[2 * qi + 1]
                        bpa, bpb = self.bPS[2 * qi], self.bPS[2 * qi + 1]
                        self.lin_fm(pa, bpa, Wqk, bWqk, qi * 256 + hp * 128, 128, hT, bin_, t0, n)
                        if t0 < S:
                            self.lin_fm(pb, bpb, Wqk, bWqk, 512 + qi * 256 + hp * 128, 128, hT, bin_, t0, n)
                            k.v(lambda e: e.tensor_tensor(t1[:, 0:n], pa[:, 0:n], rope[:, 0, t0:t0 + n], ALU.mult), [bpa, brope], [bt[0]])
                            k.v(lambda e: e.tensor_tensor(t2[:, 0:n], pb[:, 0:n], rope[:, 1, t0:t0 + n], ALU.mult), [bpb, brope], [bt[1]])
                            k.v(lambda e: e.tensor_tensor(dst[:, hp, t0:t0 + n], t1[:, 0:n], t2[:, 0:n], ALU.add), bt, [bqk[i] for i in tiles])
                        else:
                            k.act(dst[:, hp, t0:t0 + n], pa[:, 0:n], AF.Copy, [bpa], [bqk[i] for i in tiles])
            k.dma("pool", Wvg, I["w_vg"].rearrange("(c p) n -> p c n", p=128), [], [bWvg])
            for i in range(NT):
                pv, pg = self.PS[2 * (i % 2)], self.PS[2 * (i % 2) + 1]
                bpv, bpg = self.bPS[2 * (i % 2)], self.bPS[2 * (i % 2) + 1]
                for kc in range(8):
                    k.mm(pv, hT[:, kc, i * 128:(i + 1) * 128], Wvg[:, kc, 0:512], kc == 0, kc == 7, [bh[i], bWvg], [bpv])
                for kc in range(8):
                    k.mm(pg, hT[:, kc, i * 128:(i + 1) * 128], Wvg[:, kc, 512:1024], kc == 0, kc == 7, [bh[i], bWvg], [bpg])
                k.v(lambda e: e.tensor_copy(v_tm[:, i, :], pv), [bpv], [bvg[i]])
                k.act(sg_tm[:, i, :], pg, AF.Silu, [bpg], [bvg[i]])
            for d in range(2):
                k.v(lambda e: e.memset(lrT[d], 1.0), [], [blr[d]])
                for bi, (t0, n) in enumerate(blks):
                    tiles = list(range(t0 // 128, (t0 + n) // 128))
                    ps = self.PS[4 + bi % 2]
                    bps = self.bPS[4 + bi % 2]
                    self.lin_fm(ps, bps, Wlr, bWlr, d * 16, 16, hT, [bh[i] for i in tiles], t0, n)
                    k.v(lambda e: e.tensor_copy(lrT[d][0:16, t0:t0 + n], ps[0:16, 0:n]), [bps], [blr[d]])
            k.barrier()

    def gla_scan(self, qT, kT, bqk, v_tm, sg_tm, bvg, lrT, blr, aoT, bao):
        k, I = self.k, self.I
        with ExitStack() as es:
            dwb = [self.load_const(es, "dwb", I["dwb_f" if d == 0 else "dwb_b"], [17, 256]) for d in range(2)]
            tri, btri = self.load_const(es, "tri", I["tri"].rearrange("a p t -> p a t"), [128, 2, 128])
            am, bam = self.load_const(es, "amask", I["amask"].rearrange("a p t -> p a t"), [128, 2, 64])
            gg, bgg = self.load_const(es, "glag", I["gla_g"].to_broadcast([128, 512]), [128, 512])
            o_f = self.sb(es, "o_f", [128, NT, 512], F32)
            bof = k.bufs(NT, "o_f")
            S32 = self.sb(es, "S32", [128, 2, 128], F32)
            Sb = self.sb(es, "Sb", [128, 2, 128], BF16)
            bS = [[k.buf("S") for _ in range(2)] for _ in range(2)]
            ex = self.sb(es, "gex", [128, 256], F32)
            ll = self.sb(es, "gll", [128, 256], F32)
            bll = k.buf("gll")
            NB_ = 2
            bT = [[self.sb(es, "gbT", [128, 128], F32) for _ in range(2)] for _ in range(NB_)]
            E3 = [[self.sb(es, "gE3", [128, 128], F32) for _ in range(2)] for _ in range(NB_)]
            E1 = [[self.sb(es, "gE1", [128, 128], F32) for _ in range(2)] for _ in range(NB_)]
            E2 = [[self.sb(es, "gE2", [128, 128], F32) for _ in range(2)] for _ in range(NB_)]
            E4 = [[self.sb(es, "gE4", [128, 128], F32) for _ in range(2)] for _ in range(NB_)]
            qe = [[self.sb(es, "gqe", [128, 128], BF16) for _ in range(2)] for _ in range(NB_)]
            ke = [[self.sb(es, "gke", [128, 128], BF16) for _ in range(2)] for _ in range(NB_)]
            qi_ = [[self.sb(es, "gqi", [128, 128], BF16) for _ in range(2)] for _ in range(NB_)]
            koT = [[self.sb(es, "gko", [128, 128], BF16) for _ in range(2)] for _ in range(NB_)]
            ko_tm = [[self.sb(es, "gkt", [128, 128], BF16) for _ in range(2)] for _ in range(NB_)]
            bprep = [[k.buf("gprep") for _ in range(2)] for _ in range(NB_)]
            aTm = [self.sb(es, "gaTm", [128, 64], BF16) for _ in range(2)]
            baTm = k.bufs(2, "gaTm")
            osum = self.sb(es, "gosum", [128, 512], F32)
            bosum = k.buf("gosum")
            ssq = self.sb(es, "gssq", [128, 4], F32)
            junk = self.sb(es, "gjunk", [128, 128], F32)
            bssq = k.buf("gssq")
            ao = self.sb(es, "gao", [128, 512], F32)
            aob = self.sb(es, "gaob", [128, 512], BF16)
            bao_t = k.buf("gao")

            step = 0
            for d in range(2):
                for hp in range(2):
                    k.v(lambda e: e.memset(S32[:, hp, :], 0.0), [], bS[hp])
                    k.v(lambda e: e.memset(Sb[:, hp, :], 0.0), [], bS[hp])
                order = [16, 17] + list(range(16)) if d == 0 else [17, 16] + list(range(15, -1, -1))
                for i in order:
                    pp = step % NB_
                    step += 1
                    c0 = i * 128
                    px = self.PS[6]
                    k.mm(px[:, 0:256], lrT[d][0:17, c0:c0 + 128], dwb[d][0], True, True, [blr[d], dwb[d][1]], [self.bPS[6]])
                    k.act(ex, px[:, 0:256], AF.Exp, [self.bPS[6]], [bll], scale=-1.0)
                    k.act(ll, ex, AF.Ln, [bll], [bll], scale=1.0, bias=1.0)
                    if DBG < 2:
                        continue
                    for hp in range(2):
                        bp = bprep[pp][hp]
                        pb_ = self.PS[4 + hp]
                        k.mm(pb_[:, 0:128], ll[:, hp * 128:(hp + 1) * 128], tri[:, d, :], True, True, [bll, btri], [self.bPS[4 + hp]])
                        b_ = bT[pp][hp]
                        k.v(lambda e: e.tensor_copy(b_, pb_[:, 0:128]), [self.bPS[4 + hp]], [bp])
                        k.act(E3[pp][hp], b_, AF.Exp, [bp], [bp])
                        for ch in range(2):
                            cs = ch * 64
                            mid = cs + (31 if d == 0 else 32)
                            last = cs + (63 if d == 0 else 0)
                            k.act(E2[pp][hp][:, cs:cs + 64], b_[:, cs:cs + 64], AF.Exp, [bp], [bp], scale=-1.0, bias=b_[:, mid:mid + 1])
                            k.act(E4[pp][hp][:, cs:cs + 64], b_[:, cs:cs + 64], AF.Exp, [bp], [bp], scale=-1.0, bias=b_[:, last:last + 1])
                        k.v(lambda e: e.reciprocal(E1[pp][hp], E2[pp][hp]), [bp], [bp])
                        qs = qT[:, hp, c0:c0 + 128]
                        ks = kT[:, hp, c0:c0 + 128]
                        k.v(lambda e: e.tensor_tensor(qe[pp][hp], qs, E1[pp][hp], ALU.mult), [bp, bqk[i]], [bp])
                        k.v(lambda e: e.tensor_tensor(ke[pp][hp], ks, E2[pp][hp], ALU.mult), [bp, bqk[i]], [bp])
                        k.v(lambda e: e.tensor_tensor(qi_[pp][hp], qs, E3[pp][hp], ALU.mult), [bp, bqk[i]], [bp], eng="pool")
                        k.v(lambda e: e.tensor_tensor(koT[pp][hp], ks, E4[pp][hp], ALU.mult), [bp, bqk[i]], [bp], eng="pool")
                        if DBG < 3:
                            continue
                        ptr = self.PSB[:, hp * 128:(hp + 1) * 128]
                        k.tr(ptr, koT[pp][hp], self.identb, [bp, self.bidentb], [self.bPS[7]])
                        k.v(lambda e: e.tensor_copy(ko_tm[pp][hp], ptr), [self.bPS[7]], [bp])
                    if DBG < 4:
                        continue
                    for ch in ((0, 1) if d == 0 else (1, 0)):
                        cs = ch * 64
                        last = cs + (63 if d == 0 else 0)
                        for hp in range(2):
                            bp = bprep[pp][hp]
                            for h2 in range(2):
                                hd = hp * 2 + h2
                                pr = h2 * 64
                                pa = self.PS[0]
                                bpa = self.bPS[0]
                                am_ = aTm[hd % 2]
                                a0 = (hd % 2) * 64
                                k.mm(pa[cs:cs + 64, a0:a0 + 64], ke[pp][hp][pr:pr + 64, cs:cs + 64], qe[pp][hp][pr:pr + 64, cs:cs + 64],
                                     True, True, [bp], [bpa])
                                k.v(lambda e: e.tensor_tensor(am_[cs:cs + 64, :], pa[cs:cs + 64, a0:a0 + 64], am[cs:cs + 64, d, :], ALU.mult),
                                    [bpa, bam], [baTm[hd % 2]])
                                if DBG < 5:
                                    continue
                                po = self.PS[2]
                                bpo = self.bPS[2]
                                k.mm(po[cs:cs + 64, hd * 128:(hd + 1) * 128], am_[cs:cs + 64, :], v_tm[cs:cs + 64, i, hd * 128:(hd + 1) * 128],
                                     True, True, [baTm[hd % 2], bvg[i]], [bpo])
                                k.mm(self.PS[1][cs:cs + 64, hd * 128:(hd + 1) * 128], qi_[pp][hp][pr:pr + 64, cs:cs + 64], Sb[pr:pr + 64, hp, :],
                                     True, True, [bp, bS[hp][h2]], [self.bPS[1]])
                                if DBG < 6:
                                    continue
                                pS = self.PS[3]
                                bpS = self.bPS[3]
                                k.mm(pS[pr:pr + 64, hp * 128:(hp + 1) * 128], ko_tm[pp][hp][cs:cs + 64, pr:pr + 64],
                                     v_tm[cs:cs + 64, i, hd * 128:(hd + 1) * 128], True, True, [bp, bvg[i]], [bpS])
                                k.v(lambda e: e.scalar_tensor_tensor(S32[pr:pr + 64, hp, :], S32[pr:pr + 64, hp, :],
                                                                     E3[pp][hp][pr:pr + 64, last:last + 1],
                                                                     pS[pr:pr + 64, hp * 128:(hp + 1) * 128], ALU.mult, ALU.add),
                                    [bp, bpS, bS[hp][h2]], [bS[hp][h2]])
                                k.act(Sb[pr:pr + 64, hp, :], S32[pr:pr + 64, hp, :], AF.Copy, [bS[hp][h2]], [bS[hp][h2]])
                    if DBG < 7:
                        continue
                    po = self.PS[2]
                    bpo = self.bPS[2]
                    if d == 0:
                        k.act(o_f[:, i, :], po, AF.Copy, [bpo], [bof[i]])
                        k.v(lambda e: e.tensor_tensor(o_f[:, i, :], self.PS[1], o_f[:, i, :], ALU.add), [self.bPS[1], bof[i]], [bof[i]])
                    else:
                        k.v(lambda e: e.tensor_tensor(osum, po, o_f[:, i, :], ALU.add), [bpo, bof[i]], [bosum])
                        k.v(lambda e: e.tensor_tensor(osum, self.PS[1], osum, ALU.add), [self.bPS[1], bosum], [bosum])
                        for hd in range(4):
                            k.act(junk, osum[:, hd * 128:(hd + 1) * 128], AF.Square, [bosum], [bssq], accum_out=ssq[:, hd:hd + 1])
                        k.act(ssq, ssq, AF.Sqrt, [bssq], [bssq], scale=1.0 / (64.0 * 128.0), bias=RMS_EPS)
                        k.v(lambda e: e.reciprocal(ssq, ssq), [bssq], [bssq])
                        for hd in range(4):
                            k.v(lambda e: e.tensor_scalar(ao[:, hd * 128:(hd + 1) * 128], osum[:, hd * 128:(hd + 1) * 128],
                                                          ssq[:, hd:hd + 1], 0.125, ALU.mult, ALU.mult), [bosum, bssq], [bao_t])
                        k.v(lambda e: e.tensor_tensor(ao, ao, gg, ALU.mult), [bao_t, bgg], [bao_t])
                        k.v(lambda e: e.tensor_tensor(aob, ao, sg_tm[:, i, :], ALU.mult), [bao_t, bvg[i]], [bao_t])
                        for c in range(4):
                            ptr = self.PSB[:, 512 + c * 128:512 + (c + 1) * 128]
                            k.tr(ptr, aob[:, c * 128:(c + 1) * 128], self.identb, [bao_t, self.bidentb], [self.bPS[7]])
                        k.act(aoT[:, :, c0:c0 + 128], self.PSB[:, 512:1024].rearrange("p (c t) -> p c t", c=4), AF.Copy,
                              [self.bPS[7]], [bao[i]])
            k.barrier()

    def mixer1(self, b):
        k, I = self.k, self.I
        with ExitStack() as esm:
          with ExitStack() as esq:
            with ExitStack() as esh:
                hT = self.sb(esh, "hT", [128, 8, T], BF16)
                bh = k.bufs(NT, "hT")
                self.norm_mod(hT, bh, b, 1, 0, T)
                qT = self.sb(esq, "nqT", [128, 8, S], BF16)
                kT = self.sb(esq, "nkT", [128, 8, T], BF16)
                bq = k.bufs(NT, "nq")
                bk = k.bufs(NT, "nk")
                v_tm = self.sb(esq, "nv", [128, NT, D], BF16)
                bv = k.bufs(NT, "nv")
                v_sh = self.sb(esq, "nvs", [128, 15, D], BF16)
                bvs = k.bufs(15, "nvs")
                with ExitStack() as es:
                    w = self.sb(es, "nw", [128, 8, D], BF16)
                    bW = k.buf("nw")
                    for j in range(3):
                        k.dma("pool", w, I["na_w_in"][:, j * D:(j + 1) * D].rearrange("(c p) n -> p c n", p=128), [], [bW])
                        if j < 2:
                            dst, bd, tend = (qT, bq, S) if j == 0 else (kT, bk, T)
                            blks = blocks(0, min(tend, S), 512) + (blocks(S, tend, 512) if tend > S else [])
                            n_ = 0
                            for (t0, n) in blks:
                                tiles = list(range(t0 // 128, (t0 + n) // 128))
                                for m in range(8):
                                    ps = self.PS[n_ % 4]
                                    bps = self.bPS[n_ % 4]
                                    n_ += 1
                                    self.lin_fm(ps, bps, w, bW, m * 128, 128, hT, [bh[i] for i in tiles], t0, n)
                                    if m % 2 == 0:
                                        k.act(dst[:, m, t0:t0 + n], ps[:, 0:n], AF.Copy, [bps], [bd[i] for i in tiles],
                                              scale=(0.125 if j == 0 else 1.0))
                                    else:
                                        k.v(lambda e: e.tensor_scalar(dst[:, m, t0:t0 + n], ps[:, 0:n], (0.125 if j == 0 else 1.0), None, ALU.mult),
                                            [bps], [bd[i] for i in tiles])
                        else:
                            n_ = 0
                            for (dstv, bdv, ntl, toff) in ((v_tm, bv, NT, 0), (v_sh, bvs, 15, 64)):
                                for i in range(ntl):
                                    c0 = toff + i * 128
                                    rd = [bh[c0 // 128], bh[(c0 + 127) // 128], bW]
                                    for hf in range(2):
                                        ps = self.PS[n_ % 4]
                                        bps = self.bPS[n_ % 4]
                                        n_ += 1
                                        for kc in range(8):
                                            k.mm(ps, hT[:, kc, c0:c0 + 128], w[:, kc, hf * 512:(hf + 1) * 512], kc == 0, kc == 7, rd, [bps])
                                        if hf == 0:
                                            k.act(dstv[:, i, 0:512], ps, AF.Copy, [bps], [bdv[i]])
                                        else:
                                            k.v(lambda e: e.tensor_copy(dstv[:, i, 512:1024], ps), [bps], [bdv[i]])
                    k.barrier()
            atT = self.sb(esm, "natT", [128, 8, S], BF16)
            bat = k.bufs(16, "nat")
            self.na_attention(qT, kT, bq, bk, v_tm, bv, v_sh, bvs, atT, bat)
            esq.close()
          if True:
            with ExitStack() as es:
                W = self.sb(es, "nwout", [128, 8, D], BF16)
                bW = k.buf("nwout")
                k.dma("pool", W, I["na_w_out"].rearrange("(c p) n -> p c n", p=128), [], [bW])
                self.out_proj(b, 1, W, bW, atT, lambda i: [bat[i]], S)

    def na_attention(self, qT, kT, bq, bk, v_tm, bv, v_sh, bvs, atT, bat):
        k, I = self.k, self.I
        with ExitStack() as es:
            Bh = [self.sb(es, "nB", [64, 960], F32) for _ in range(2)]
            bBh = k.bufs(2, "nB")
            NP = 2
            sc = [self.sb(es, "nsc", [64, 768], F32) for _ in range(NP)]
            pe_ = [self.sb(es, "npe", [64, 768], F32) for _ in range(NP)]
            pb = [self.sb(es, "npb", [64, 768], BF16) for _ in range(NP)]
            st = [self.sb(es, "nst", [64, 4], F32) for _ in range(NP)]
            bsm = k.bufs(NP, "nsm")
            pT = [self.sb(es, "npT", [128, 7, 64], BF16) for _ in range(NP)]
            bpT = k.bufs(NP, "npT")
            u = 0
            for h in range(16):
                m, pr = h // 2, (h % 2) * 64
                B = Bh[h % 2]
                k.dma("sp", B, I["na_bias"][h], [], [bBh[h % 2]])
                for r in range(32):
                    p = u % NP
                    u += 1
                    r0 = min(max(r - 4, 0), 24)
                    s = r0 - r + 7
                    w0 = r0 * 64
                    ktiles = sorted(set(range(w0 // 128, (w0 + 511) // 128 + 1)))
                    qtile = r // 2
                    qs = qT[pr:pr + 64, m, r * 64:(r + 1) * 64]
                    ps_w, ps_c = self.PS[2 * p], self.PS[2 * p + 1]
                    bpw, bpc = self.bPS[2 * p], self.bPS[2 * p + 1]
                    k.mm(ps_w[0:64, 0:512], qs, kT[pr:pr + 64, m, w0:w0 + 512], True, True, [bq[qtile]] + [bk[i] for i in ktiles], [bpw])
                    k.mm(ps_c[0:64, 0:256], qs, kT[pr:pr + 64, m, S:T], True, True, [bq[qtile], bk[16], bk[17]], [bpc])
                    bs = bsm[p]
                    k.v(lambda e: e.tensor_tensor(sc[p][:, 0:512], ps_w[0:64, 0:512], B[:, s * 64:s * 64 + 512], ALU.add), [bpw, bBh[h % 2]], [bs])
                    k.act(sc[p][:, 512:768], ps_c[0:64, 0:256], AF.Copy, [bpc], [bs])
                    k.v(lambda e: e.reduce_max(st[p][:, 0:1], sc[p], AX.X), [bs], [bs])
                    k.v(lambda e: e.tensor_scalar(st[p][:, 1:2], st[p][:, 0:1], -1.0, None, ALU.mult), [bs], [bs])
                    k.act(pe_[p], sc[p], AF.Exp, [bs], [bs], scale=1.0, bias=st[p][:, 1:2], accum_out=st[p][:, 2:3])
                    k.v(lambda e: e.reciprocal(st[p][:, 3:4], st[p][:, 2:3]), [bs], [bs])
                    k.v(lambda e: e.tensor_scalar(pb[p], pe_[p], st[p][:, 3:4], None, ALU.mult), [bs], [bs])
                    if w0 % 128 == 0:
                        chunks = [(j * 128, v_tm, bv, w0 // 128 + j) for j in range(4)]
                    else:
                        chunks = [(j * 128, v_sh, bvs, (w0 - 64) // 128 + j) for j in range(4)]
                    chunks += [(512, v_tm, bv, 16), (640, v_tm, bv, 17)]
                    ptb = self.PSB[:, 0:384].rearrange("p (j q) -> p j q", q=64)
                    for j, (col, vt, bvt, ti_) in enumerate(chunks):
                        k.tr(ptb[:, j, :], pb[p][:, col:col + 128], self.identb[0:64, 0:64], [bs, self.bidentb], [self.bPS[7]])
                    k.v(lambda e: e.tensor_copy(pT[p][:, 0:6, :], ptb), [self.bPS[7]], [bpT[p]])
                    po = self.PS[4 + p]
                    bpo = self.bPS[4 + p]
                    for j, (col, vt, bvt, ti_) in enumerate(chunks):
                        k.mm(po[pr:pr + 64, 0:64], vt[:, ti_, h * 64:(h + 1) * 64], pT[p][:, j, :],
                             j == 0, j == 5, [bvt[ti_], bpT[p]], [bpo])
                    k.act(atT[pr:pr + 64, m, r * 64:(r + 1) * 64], po[pr:pr + 64, 0:64], AF.Copy, [bpo], [bat[qtile]])
            k.barrier()

    def moe_layer(self, l, t_end):
        k, I = self.k, self.I
        nbl = self.nbl
        ntl = t_end // 128
        NSH = self.NSH
        with ExitStack() as esr:
            rw, brw = self.load_const(esr, "rw", I["router_w"][l].rearrange("(c p) n -> p c n", p=128), [128, 8, E])
            rb, brb = self.load_const(esr, "rb", I["router_b"][l].to_broadcast([128, E]), [128, E])
            R = {}
            for nm, shp in (("sc", [128, E]), ("bi", [128, E]), ("m8", [128, 8, 8]), ("gs", [128, 8]), ("g8", [128, 8]),
                            ("pen", [128, 8]), ("mk", [128, E]), ("t8", [128, 8]), ("s01", [128, E]), ("gd", [128, E]),
                            ("gsum", [128, 2]), ("slotm", [128, E]), ("s8", [128, 8]), ("junk", [128, E]), ("selacc", [128, E])):
                R[nm] = self.sb(esr, "r" + nm, shp, F32)
            s01b = self.sb(esr, "rs01b", [128, E], BF16)
            saccb = self.sb(esr, "rsaccb", [128, E], BF16)
            bR = k.buf("rout")
            bsacc = k.buf("selacc")
            htm = [self.sb(esr, "htm", [128, D], BF16) for _ in range(2)]
            bhtm = k.bufs(2, "htm")
            k.v(lambda e: e.memset(R["selacc"], 0.0), [], [bsacc])
            k.v(lambda e: e.memset(saccb, 0.0), [], [bsacc])
            cnt = [0]
            for b in range(nbl):
                def route(tile, ti, ps, bps, h32, bh32, b=b):
                    g = b * NT + tile
                    k.act(R["sc"], ps, AF.Sigmoid, [bps], [bR])
                    k.v(lambda e: e.tensor_tensor(R["bi"], R["sc"], rb, ALU.add), [bR, brb], [bR])
                    for gg in range(8):
                        k.v(lambda e: e.max(out=R["m8"][:, gg, :], in_=R["bi"][:, gg * 32:(gg + 1) * 32]), [bR], [bR])
                    k.v(lambda e: e.tensor_tensor(R["gs"], R["m8"][:, :, 0], R["m8"][:, :, 1], ALU.add), [bR], [bR])
                    k.v(lambda e: e.max(out=R["g8"], in_=R["gs"]), [bR], [bR])
                    k.v(lambda e: e.tensor_scalar(R["pen"], R["gs"], R["g8"][:, 3:4], None, ALU.is_ge), [bR], [bR])
                    k.v(lambda e: e.tensor_scalar(R["pen"], R["pen"], -1.0, 1.0e4, ALU.add, ALU.mult), [bR], [bR])
                    for gg in range(8):
                        k.v(lambda e: e.tensor_scalar(R["mk"][:, gg * 32:(gg + 1) * 32], R["bi"][:, gg * 32:(gg + 1) * 32],
                                                      R["pen"][:, gg:gg + 1], None, ALU.add), [bR], [bR])
                    k.v(lambda e: e.max(out=R["t8"], in_=R["mk"]), [bR], [bR])
                    k.v(lambda e: e.tensor_scalar(R["s01"], R["mk"], R["t8"][:, 7:8], None, ALU.is_ge), [bR], [bR])
                    k.v(lambda e: e.tensor_tensor(R["gd"], R["s01"], R["sc"], ALU.mult), [bR], [bR])
                    k.v(lambda e: e.reduce_sum(R["gsum"][:, 0:1], R["gd"], AX.X), [bR], [bR])
                    k.v(lambda e: e.reciprocal(R["gsum"][:, 1:2], R["gsum"][:, 0:1]), [bR], [bR])
                    k.v(lambda e: e.tensor_scalar(R["gd"], R["gd"], R["gsum"][:, 1:2], 2.5, ALU.mult, ALU.mult), [bR], [bR])
                    k.v(lambda e: e.tensor_copy(s01b, R["s01"]), [bR], [bR])
                    pp_ = self.PS[2]
                    k.mm(pp_[:, 0:E], self.onesb, saccb, True, False, [self.bones, bsacc], [self.bPS[2]])
                    k.mm(pp_[:, 0:E], self.usb, s01b, False, True, [self.bones, bR], [self.bPS[2]])
                    k.v(lambda e: e.tensor_tensor(R["slotm"], pp_[:, 0:E], self.ebase1, ALU.add), [self.bPS[2]], [bR])
                    k.v(lambda e: e.tensor_tensor(R["slotm"], R["slotm"], R["s01"], ALU.mult), [bR], [bR])
                    k.v(lambda e: e.tensor_tensor(R["selacc"], R["selacc"], R["s01"], ALU.add), [bR, bsacc], [bsacc])
                    k.v(lambda e: e.tensor_copy(saccb, R["selacc"]), [bsacc], [bsacc])
                    k.v(lambda e: e.max(out=R["s8"], in_=R["slotm"]), [bR], [bR])
                    k.v(lambda e: e.tensor_scalar(self.slotI[:, g, :], R["s8"], -1.0, None, ALU.add), [bR], [self.bslot[g]])
                    if self.dbg_out is not None:
                        k.v(lambda e: e.tensor_copy(self.s8all[:, g, :], R["s8"]), [bR], [self.bslot[g]])
                    for kk in range(8):
                        k.v(lambda e: e.scalar_tensor_tensor(R["junk"], R["slotm"], R["s8"][:, kk:kk + 1], R["gd"], ALU.is_equal, ALU.mult,
                                                             accum_out=self.gate8[:, g, kk:kk + 1]), [bR], [self.bslot[g], bR])
                    p = cnt[0] % 2
                    cnt[0] += 1
                    pa, pb_ = self.PS[0], self.PS[1]
                    for c in range(8):
                        pst = (pa if c < 4 else pb_)[:, (c % 4) * 128:(c % 4 + 1) * 128]
                        k.tr(pst, h32[:, c, ti * 128:(ti + 1) * 128], self.ident, [bh32, self.bident], [self.bPS[c // 4]])
                    k.act(htm[p][:, 0:512], pa, AF.Copy, [self.bPS[0]], [bhtm[p]])
                    k.v(lambda e: e.tensor_copy(htm[p][:, 512:1024], pb_), [self.bPS[1]], [bhtm[p]])
                    for kk in range(8 if MDBG >= 2 else 0):
                        k.idma(self.xs, bass.IndirectOffsetOnAxis(ap=self.slotI[:, g, kk:kk + 1], axis=0), htm[p], None,
                               [bhtm[p], self.bslot[g]], [k.buf("xsw")])
                    k.dma("sp", self.xsh[g * 128:(g + 1) * 128, :], htm[p], [bhtm[p]], [k.buf("xsw")])

                self.norm_mod(None, None, b, l, 1, t_end, router=(rw, brw, route))
            k.barrier()

        if MDBG < 3:
            return
        with ExitStack() as es:
            Wgu = [self.sb(es, "wgu", [128, 8, 512], BF16) for _ in range(2)]
            Wd = [self.sb(es, "wd", [128, 2, D], BF16) for _ in range(2)]
            bWe = k.bufs(2, "we")
            xtm = [self.sb(es, "extm", [128, D], BF16) for _ in range(2)]
            bxtm = k.bufs(2, "extm")
            xTe = [self.sb(es, "exT", [128, 8, 512], BF16) for _ in range(2)]
            bxTe = k.bufs(2, "exT")
            sgl = self.sb(es, "msg", [128, 512], F32)
            bsgl = k.buf("msg")
            hid = [[self.sb(es, "mhid", [128, 512], BF16) for _ in range(2)] for _ in range(2)]
            bhid = k.bufs(2, "mhid")
            ytm = [self.sb(es, "eytm", [128, D], F32) for _ in range(2)]
            bytm = k.bufs(2, "eytm")
            W32 = [[self.sb(es, "wg32", [128, 8, 256], F32), self.sb(es, "wu32", [128, 8, 256], F32),
                    self.sb(es, "wd32", [128, 2, D], F32)] for _ in range(2)]
            bW32 = k.bufs(2, "w32")
            elist = list(self.experts) + [E]

            def wsrc(e_):
                if e_ == E:
                    return I["ws_gate"][l], I["ws_up"][l], I["ws_down"][l]
                return I["w_gate"][l, e_], I["w_up"][l, e_], I["w_down"][l, e_]

            def stage(ei):
                p_ = ei % 2
                sg_, su_, sd_ = wsrc(elist[ei])
                k.dma("sp", W32[p_][0], sg_.rearrange("(c p) n -> p c n", p=128), [], [bW32[p_]])
                k.dma("sp", W32[p_][1], su_.rearrange("(c p) n -> p c n", p=128), [], [bW32[p_]])
                k.dma("sp", W32[p_][2], sd_.rearrange("(c p) n -> p c n", p=128), [], [bW32[p_]])

            def cast(ei):
                p_ = ei % 2
                k.act(Wgu[p_][:, :, 0:256], W32[p_][0], AF.Copy, [bW32[p_]], [bWe[p_]])
                k.v(lambda e: e.tensor_copy(Wgu[p_][:, :, 256:512], W32[p_][1]), [bW32[p_]], [bWe[p_]])
                k.act(Wd[p_][:, 0, :], W32[p_][2][:, 0, :], AF.Copy, [bW32[p_]], [bWe[p_]])
                k.v(lambda e: e.tensor_copy(Wd[p_][:, 1, :], W32[p_][2][:, 1, :]), [bW32[p_]], [bWe[p_]])

            stage(0)
            cast(0)
            if len(elist) > 1:
                stage(1)
            items = []
            for ei, e_ in enumerate(elist):
                if e_ == E:
                    sblocks = []
                    for b in range(nbl):
                        sblocks += [(b * T + t0, n) for (t0, n) in blocks(0, t_end, 512)]
                else:
                    sblocks = [(e_ * CAP, CAP)]
                for bi_, (r0, n) in enumerate(sblocks):
                    items.append((ei, e_, r0, n, bi_ == len(sblocks) - 1))
            cnt = {"xq": 0, "yq": 0, "tq": 0}

            def prefetch_x(j):
                ei, e_, r0, n, _ = items[j]
                xp = j % 2
                xsrc = self.xsh if e_ == E else self.xs
                for st in range(n // 128):
                    q = cnt["xq"] % 2
                    cnt["xq"] += 1
                    k.dma("pool", xtm[q], xsrc[r0 + st * 128:r0 + (st + 1) * 128, :], [], [bxtm[q]])
                    for c in range(8):
                        k.tr(self.PSB[:, c * 128:(c + 1) * 128], xtm[q][:, c * 128:(c + 1) * 128], self.identb,
                             [bxtm[q], self.bidentb], [self.bPS[7]])
                    k.act(xTe[xp][:, :, st * 128:(st + 1) * 128], self.PSB.rearrange("p (c t) -> p c t", c=8), AF.Copy,
                          [self.bPS[7]], [bxTe[xp]])

            prefetch_x(0)
            for j, (ei, e_, r0, n, last) in enumerate(items):
                p = ei % 2
                xp = j % 2
                nst = n // 128
                for m in range(2):
                    self.lin_fm(self.PS[m], self.bPS[m], Wgu[p], bWe[p], m * 128, 128, xTe[xp], [bxTe[xp]], 0, n)
                    self.lin_fm(self.PS[2 + m], self.bPS[2 + m], Wgu[p], bWe[p], 256 + m * 128, 128, xTe[xp], [bxTe[xp]], 0, n)
                for m in range(2):
                    k.act(sgl[:, 0:n], self.PS[m][:, 0:n], AF.Silu, [self.bPS[m]], [bsgl])
                    k.v(lambda e: e.tensor_tensor(hid[xp][m][:, 0:n], sgl[:, 0:n], self.PS[2 + m][:, 0:n], ALU.mult),
                        [bsgl, self.bPS[2 + m]], [bhid[xp]])
                if last:
                    if ei + 1 < len(elist):
                        cast(ei + 1)
                    if ei + 2 < len(elist):
                        stage(ei + 2)
                if j + 1 < len(items):
                    prefetch_x(j + 1)
                for st in range(nst):
                    q = cnt["tq"] % 2
                    cnt["tq"] += 1
                    for hf in range(2):
                        py = self.PS[4 + cnt["yq"] % 3]
                        bpy = self.bPS[4 + cnt["yq"] % 3]
                        cnt["yq"] += 1
                        for m in range(2):
                            k.mm(py, hid[xp][m][:, st * 128:(st + 1) * 128], Wd[p][:, m, hf * 512:(hf + 1) * 512], m == 0, m == 1,
                                 [bhid[xp], bWe[p]], [bpy])
                        if hf == 0:
                            k.act(ytm[q][:, 0:512], py, AF.Copy, [bpy], [bytm[q]])
                        else:
                            k.v(lambda e: e.tensor_copy(ytm[q][:, 512:1024], py), [bpy], [bytm[q]])
                    rr = slice(r0 + st * 128, r0 + (st + 1) * 128)
                    if e_ == E:
                        k.dma("sp", self.ysh_d[rr, :], ytm[q], [bytm[q]], [k.buf("ysw")])
                    else:
                        for hf in range(2):
                            k.dma("sp", self.ys2[hf][rr, :], ytm[q][:, hf * 512:(hf + 1) * 512], [bytm[q]], [k.buf("ysw")])
            k.barrier()

        if MDBG < 4:
            return
        with ExitStack() as es:
            yg = [self.sb(es, "yg", [128, 8, D], F32) for _ in range(2)]
            byg = k.bufs(2, "yg")
            ysh = [self.sb(es, "ysh", [128, D], F32) for _ in range(2)]
            bysh = k.bufs(2, "ysh")
            acc = self.sb(es, "pacc", [128, D], F32)
            bacc = k.buf("pacc")
            ssq = self.sb(es, "pssq", [128, 2], F32)
            bss = k.buf("pssq")
            junk = self.sb(es, "pjunk", [128, D], F32)
            yn = self.sb(es, "pyn", [128, D], F32)
            byn = k.buf("pyn")
            xb = [self.sb(es, "px", [128, 8, 128], F32) for _ in range(2)]
            bxb = k.bufs(2, "px")
            it = 0
            for b in range(nbl):
                for tile in range(ntl):
                    g = b * NT + tile
                    p = it % 2
                    it += 1
                    v = b if tile < 16 else 2
                    G = self.modvec(l, 1, 2, v)
                    for kk in range(8):
                        for hf in range(2):
                            k.idma(yg[p][:, kk, hf * 512:(hf + 1) * 512], None, self.ys2[hf],
                                   bass.IndirectOffsetOnAxis(ap=self.slotI[:, g, kk:kk + 1], axis=0), [self.bslot[g]], [byg[p]])
                    k.dma("sp", ysh[p], self.ysh_d[g * 128:(g + 1) * 128, :], [], [bysh[p]])
                    k.dma("sp", xb[p], self.xres[b][:, :, tile * 128:(tile + 1) * 128], [self.bxres[b][tile]], [bxb[p]])
                    k.v(lambda e: e.scalar_tensor_tensor(acc, yg[p][:, 0, :], self.gate8[:, g, 0:1], ysh[p], ALU.mult, ALU.add),
                        [byg[p], bysh[p], self.bslot[g]], [bacc])
                    for kk in range(1, 8):
                        k.v(lambda e: e.scalar_tensor_tensor(acc, yg[p][:, kk, :], self.gate8[:, g, kk:kk + 1], acc, ALU.mult, ALU.add),
                            [byg[p], self.bslot[g], bacc], [bacc])
                    k.act(junk, acc, AF.Square, [bacc], [bss], accum_out=ssq[:, 0:1])
                    k.act(ssq[:, 1:2], ssq[:, 0:1], AF.Sqrt, [bss], [bss], scale=1.0 / D, bias=RMS_EPS)
                    k.v(lambda e: e.reciprocal(ssq[:, 1:2], ssq[:, 1:2]), [bss], [bss])
                    k.v(lambda e: e.tensor_scalar(yn, acc, ssq[:, 1:2], None, ALU.mult), [bss, bacc], [byn])
                    pa, pb_ = self.PS[2 * p], self.PS[2 * p + 1]
                    for c in range(8):
                        ps = (pa if c < 4 else pb_)[:, (c % 4) * 128:(c % 4 + 1) * 128]
                        k.tr(ps, yn[:, c * 128:(c + 1) * 128], self.ident, [byn, self.bident], [self.bPS[2 * p + c // 4]])
                    for c in range(8):
                        ps = (pa if c < 4 else pb_)[:, (c % 4) * 128:(c % 4 + 1) * 128]
                        k.v(lambda e: e.scalar_tensor_tensor(xb[p][:, c, :], ps, G[:, c:c + 1], xb[p][:, c, :], ALU.mult, ALU.add),
                            [self.bPS[2 * p + c // 4], self.bmod, bxb[p]], [bxb[p]])
                    k.dma("sp", self.xres[b][:, :, tile * 128:(tile + 1) * 128], xb[p], [bxb[p]], [self.bxres[b][tile]])
            k.barrier()


def host_consts():
    ident = np.eye(128, dtype=np.float32)
    s = np.arange(128)[:, None]
    t = np.arange(128)[None, :]
    same = (s // 64) == (t // 64)
    tri = np.stack([np.where(same & (s <= t), -1.0 / 16, 0.0), np.where(same & (s >= t), -1.0 / 16, 0.0)]).astype(np.float32)
    s6 = np.arange(64)[:, None]
    t6 = np.arange(64)[None, :]
    mf = (s6 <= t6).astype(np.float32)
    mb = (s6 >= t6).astype(np.float32)
    amask = np.stack([np.concatenate([mf, mf], 0), np.concatenate([mb, mb], 0)]).astype(np.float32)
    kk = np.arange(128) % 64
    f = kk % 16
    inv = (10000.0 ** (-(np.arange(16, dtype=np.float32)) / 16)).astype(np.float32)
    tt = np.arange(S)
    rows = (tt // 64).astype(np.float32)
    cols = (tt % 64).astype(np.float32)
    pos = np.where((kk < 32)[:, None], rows[None, :], cols[None, :]).astype(np.float32)
    ang = pos * inv[f][:, None]
    cosT = np.cos(ang).astype(np.float32)
    sinT = np.sin(ang).astype(np.float32)
    sign = np.where((kk % 32) < 16, -1.0, 1.0).astype(np.float32)[:, None]
    rope = np.stack([cosT, sinT * sign, cosT, sinT * sign]).astype(np.float32)
    ustrict = (s < t).astype(np.float32)
    ebase1 = (np.arange(E, dtype=np.float32) * CAP + 1.0)[None, :]
    return ident, tri, amask, rope, ustrict, ebase1


def na_bias_table(rpb):
    qc = np.arange(64)
    cstart = np.clip(qc - 8, 0, 64 - 16)
    kc = np.arange(64)
    ok = (kc[None, :] >= cstart[:, None]) & (kc[None, :] < cstart[:, None] + 16)
    idx = np.clip(kc[None, :] - qc[:, None] + 15, 0, 30)
    g = rpb[:, :, idx]
    g = np.transpose(g, (0, 2, 1, 3))
    out = np.where(ok[None, :, None, :], g, np.float32(NEG)).astype(np.float32)
    return np.ascontiguousarray(out.reshape(16, 64, 960))


def fm(vec):
    return np.ascontiguousarray(np.asarray(vec, np.float32).reshape(8, 128).T)


def prep_shared(inp):
    f32 = lambda a: np.ascontiguousarray(np.asarray(a, dtype=np.float32))
    ident, tri, amask, rope, ustrict, ebase1 = host_consts()
    w_in = f32(inp["ab_w_in"][0])
    q, kk_, v, g, lrf, lrb, ga, gb = np.split(w_in, np.cumsum([256, 256, 512, 512, 16, 16, 512, 512])[:-1], axis=1)
    i64 = np.arange(64)
    partner = np.where((i64 % 32) < 16, i64 + 16, i64 - 16)
    perm = (np.arange(256) // 64) * 64 + partner[np.arange(256) % 64]
    sh = {
        "ada_w": f32(inp["ada_w"]),
        "ada_bT": np.ascontiguousarray(f32(inp["ada_b"]).reshape(2, 48, 128).transpose(0, 2, 1)),
        "gvec": np.ascontiguousarray(np.stack([np.stack([fm(inp[n][l]) for n in ("g_pre_mix", "g_post_mix", "g_pre_ffn", "g_post_ffn")], 1)
                                               for l in range(2)])),
        "w_qk": np.ascontiguousarray(np.concatenate([q, kk_, q[:, perm], kk_[:, perm]], 1)),
        "w_vg": np.ascontiguousarray(np.concatenate([v, g], 1)),
        "w_lr": np.ascontiguousarray(np.concatenate([lrf, lrb], 1)),
        "w_glu": np.ascontiguousarray(np.concatenate([ga, gb], 1)),
        "ab_w_out": f32(inp["ab_w_out"][0]),
        "dwb_f": np.ascontiguousarray(np.concatenate([f32(inp["gla_dw_f"][0]), f32(inp["gla_db_f"][0])[None]], 0)),
        "dwb_b": np.ascontiguousarray(np.concatenate([f32(inp["gla_dw_b"][0]), f32(inp["gla_db_b"][0])[None]], 0)),
        "gla_g": f32(inp["gla_norm_g"][0])[None],
        "convw": np.ascontiguousarray(f32(inp["conv_w"][0]).reshape(31, 4, 128).transpose(2, 1, 0)),
        "convv": np.ascontiguousarray(np.stack([f32(inp[n][0]).reshape(4, 128).T for n in ("conv_b", "conv_ln_g", "conv_ln_b")], 1)),
        "rope": rope, "tri": tri, "amask": amask, "ident": ident, "ustrict": ustrict, "ebase1": ebase1,
        "na_w_in": f32(inp["na_w_in"][0]),
        "na_w_out": f32(inp["na_w_out"][0]),
        "na_bias": na_bias_table(f32(inp["na_rpb"][0])),
        "router_w": f32(inp["moe_router_w"]),
        "router_b": f32(inp["moe_router_b"])[:, None, :],
        "w_gate": f32(inp["moe_w_gate"]), "w_up": f32(inp["moe_w_up"]), "w_down": f32(inp["moe_w_down"]),
        "ws_gate": f32(inp["moe_ws_gate"]), "ws_up": f32(inp["moe_ws_up"]), "ws_down": f32(inp["moe_ws_down"]),
    }
    return sh


def core_inputs(inp, sh, b0, nbl):
    x = np.ascontiguousarray(np.asarray(inp["x"], np.float32)[b0:b0 + nbl])
    ctx = np.ascontiguousarray(np.asarray(inp["ctx"], np.float32)[b0:b0 + nbl])
    c = np.asarray(inp["c"], np.float32)
    cols = [c[b0 + j] if j < nbl else np.zeros(D, np.float32) for j in range(2)] + [np.asarray(inp["c_ctx"], np.float32), np.zeros(D, np.float32)]
    cvec = np.ascontiguousarray(np.stack([fm(v) for v in cols], axis=2))
    m = dict(sh)
    m.update({"x": x, "ctx": ctx, "cvec": cvec})
    return m


_PROG = {}


def kernel(**inputs):
    if "full" not in _PROG:
        _PROG["full"] = Prog()
    prog = _PROG["full"]
    sh = prep_shared(inputs)
    in_maps = [core_inputs(inputs, sh, 2 * c, 2) for c in range(8)]
    res = run_bass_kernel_spmd(prog.nc, in_maps, core_ids=list(range(8)))
    return np.concatenate([r["out"] for r in res.results], axis=0).astype(np.float32)
```

```python
from contextlib import ExitStack
import os
DBG = int(os.environ.get('GLA_DBG', '9'))
MDBG = int(os.environ.get('MOE_DBG', '9'))
import numpy as np
import concourse.bass as bass
import concourse.mybir as mybir
from concourse.bass_utils import run_bass_kernel_spmd

F32 = mybir.dt.float32
BF16 = mybir.dt.bfloat16
AF = mybir.ActivationFunctionType
ALU = mybir.AluOpType
AX = mybir.AxisListType

D = 1024
S = 2048
C = 256
T = S + C
NT = T // 128
NBL = 2
E = 256
RMS_EPS = 1e-6
LN_EPS = 1e-5
NEG = -30000.0
CAP = 384
U32 = mybir.dt.uint32


class Buf:
    __slots__ = ("name", "w", "r", "dsem")

    def __init__(self, name):
        self.name = name
        self.w = {}
        self.r = {}
        self.dsem = None


class KB:
    def __init__(self, nc):
        self.nc = nc
        self.E = {"pe": nc.tensor, "dve": nc.vector, "act": nc.scalar, "pool": nc.gpsimd, "sp": nc.sync}
        self.esem = {e: nc.alloc_semaphore("es_" + e) for e in self.E}
        self.ecnt = {e: 0 for e in self.E}
        self.seen = {e: {} for e in self.E}
        self.dcnt = {}
        self.dpool = []
        self.dnext = 0
        self.nbuf = 0
        self.allbufs = []

    def buf(self, name=None):
        self.nbuf += 1
        b = Buf("%s_%d" % (name or "b", self.nbuf))
        self.allbufs.append(b)
        return b

    def bufs(self, n, name="b"):
        return [self.buf(name) for _ in range(n)]

    def _wait(self, eng, deps):
        for key, (sem, val, src) in deps.items():
            if src == "pe" and eng == "pe":
                continue
            if src == "dma":
                val = self.dcnt[key[2:]]
            if self.seen[eng].get(key, 0) >= val:
                continue
            self.E[eng].wait_ge(sem, val)
            self.seen[eng][key] = val

    @staticmethod
    def _deps(reads, writes):
        deps = {}
        for b in reads:
            for k, t in b.w.items():
                if k not in deps or deps[k][1] < t[1]:
                    deps[k] = t
        for b in writes:
            for d in (b.w, b.r):
                for k, t in d.items():
                    if k not in deps or deps[k][1] < t[1]:
                        deps[k] = t
        return deps

    @staticmethod
    def _mark(tok, key, reads, writes):
        for b in writes:
            b.w = {key: tok}
            b.r = {}
        for b in reads:
            if b in writes:
                continue
            b.r[key] = tok

    def op(self, eng, fn, reads=(), writes=()):
        self._wait(eng, self._deps(reads, writes))
        ins = fn(self.E[eng])
        self.ecnt[eng] += 1
        ins.then_inc(self.esem[eng], 1)
        self._mark((self.esem[eng], self.ecnt[eng], eng), "e_" + eng, reads, writes)
        return ins

    def dma(self, q, out, in_, reads=(), writes=(), **kw):
        self._wait(q, self._deps(reads, writes))
        tgt = writes[0]
        if tgt.dsem is None:
            if len(self.dpool) < 72:
                nm = "ds%d" % len(self.dpool)
                self.dpool.append((self.nc.alloc_semaphore(nm), nm))
                self.dcnt[nm] = 0
            tgt.dsem = self.dpool[self.dnext % len(self.dpool)] if len(self.dpool) == 72 else self.dpool[-1]
            self.dnext += 1
        sem, nm = tgt.dsem
        ins = self.E[q].dma_start(out=out, in_=in_, **kw)
        self.dcnt[nm] += 16
        ins.then_inc(sem, 16)
        self._mark((sem, self.dcnt[nm], "dma"), "d_" + nm, reads, writes)
        return ins

    def idma(self, out, out_off, in_, in_off, reads=(), writes=()):
        self._wait("pool", self._deps(reads, writes))
        tgt = writes[0]
        if tgt.dsem is None:
            if len(self.dpool) < 72:
                nm = "ds%d" % len(self.dpool)
                self.dpool.append((self.nc.alloc_semaphore(nm), nm))
                self.dcnt[nm] = 0
            tgt.dsem = self.dpool[self.dnext % len(self.dpool)] if len(self.dpool) == 72 else self.dpool[-1]
            self.dnext += 1
        sem, nm = tgt.dsem
        ins = self.nc.gpsimd.indirect_dma_start(out=out, out_offset=out_off, in_=in_, in_offset=in_off)
        self.dcnt[nm] += 16
        ins.then_inc(sem, 16)
        self._mark((sem, self.dcnt[nm], "dma"), "d_" + nm, reads, writes)
        return ins

    def barrier(self):
        deps = {}
        for e in self.E:
            if self.ecnt[e]:
                deps["e_" + e] = (self.esem[e], self.ecnt[e], "x")
        for (sem, nm) in self.dpool:
            deps["d_" + nm] = (sem, self.dcnt[nm], "dma")
        self.allbufs = []
        for e in self.E:
            self._wait(e, {k: v for k, v in deps.items() if k != "e_" + e})

    def mm(self, out, lhsT, rhs, start, stop, reads, writes):
        return self.op("pe", lambda e: e.matmul(out, lhsT, rhs, start=start, stop=stop), reads, writes)

    def tr(self, out, in_, ident, reads, writes):
        return self.op("pe", lambda e: e.transpose(out, in_, ident), reads, writes)

    def act(self, out, in_, func, reads, writes, **kw):
        return self.op("act", lambda e: e.activation(out, in_, func, **kw), reads, writes)

    def v(self, fn, reads, writes, eng="dve"):
        return self.op(eng, fn, reads, writes)


def blocks(t0, t1, n):
    out = []
    while t0 < t1:
        m = min(n, t1 - t0)
        out.append((t0, m))
        t0 += m
    return out


class Prog:
    def __init__(self, stop="end", experts=None, nbl=NBL):
        self.stop = stop
        self.experts = list(range(E)) if experts is None else experts
        self.nbl = nbl
        nc = self.nc = bass.Bass("TRN2", target_bir_lowering=False)
        self.k = KB(nc)
        self.es = ExitStack()
        self.build()

    def din(self, name, shape, dt=F32):
        return self.nc.dram_tensor(name, list(shape), dt, kind="ExternalInput").ap()

    def sb(self, es, name, shape, dt):
        if not hasattr(self, "arena"):
            self.AW = 52800
            self.arena = self.nc.alloc_sbuf_tensor("arena", [128, self.AW], F32).ap()
            self.free_list = [(0, self.AW)]
        esz = 2 if dt == BF16 else 4
        nfree = int(np.prod(shape[1:]))
        words = (nfree * esz + 3) // 4
        words = (words + 7) // 8 * 8
        for idx, (o, n) in enumerate(self.free_list):
            if n >= words:
                break
        else:
            raise RuntimeError("SBUF arena exhausted allocating %s %s; free=%s" % (name, shape, self.free_list))
        if n == words:
            self.free_list.pop(idx)
        else:
            self.free_list[idx] = (o + words, n - words)
        P = shape[0]
        ap = self.arena[0:P, o:o + words]
        if dt != F32:
            ap = ap.bitcast(dt)
        ap = ap[:, 0:nfree]
        if len(shape) == 3:
            ap = ap.rearrange("p (a b) -> p a b", a=shape[1])
        elif len(shape) == 4:
            ap = ap.rearrange("p (a b c) -> p a b c", a=shape[1], b=shape[2])
        elif len(shape) == 5:
            ap = ap.rearrange("p (a b c d) -> p a b c d", a=shape[1], b=shape[2], c=shape[3])

        def _free():
            fl = self.free_list
            fl.append((o, words))
            fl.sort()
            merged = []
            for (a, n_) in fl:
                if merged and merged[-1][0] + merged[-1][1] == a:
                    merged[-1] = (merged[-1][0], merged[-1][1] + n_)
                else:
                    merged.append((a, n_))
            self.free_list[:] = merged
        es.callback(_free)
        return ap

    def load_const(self, es, name, src, shape, dt=F32, q="sp"):
        t = self.sb(es, name, shape, dt)
        b = self.k.buf(name)
        self.k.dma(q, t, src, [], [b])
        return t, b

    def build(self):
        nc, k = self.nc, self.k
        nbl = self.nbl
        I = self.I = {}
        I["x"] = self.din("x", [nbl, S, D])
        I["ctx"] = self.din("ctx", [nbl, C, D])
        I["cvec"] = self.din("cvec", [128, 8, 4])
        I["ada_w"] = self.din("ada_w", [2, D, 6 * D])
        I["ada_bT"] = self.din("ada_bT", [2, 128, 48])
        I["gvec"] = self.din("gvec", [2, 128, 4, 8])
        I["w_qk"] = self.din("w_qk", [D, 1024])
        I["w_vg"] = self.din("w_vg", [D, 1024])
        I["w_lr"] = self.din("w_lr", [D, 32])
        I["w_glu"] = self.din("w_glu", [D, 1024])
        I["ab_w_out"] = self.din("ab_w_out", [D, D])
        I["dwb_f"] = self.din("dwb_f", [17, 256])
        I["dwb_b"] = self.din("dwb_b", [17, 256])
        I["gla_g"] = self.din("gla_g", [1, 512])
        I["convw"] = self.din("convw", [128, 4, 31])
        I["convv"] = self.din("convv", [128, 3, 4])
        I["rope"] = self.din("rope", [4, 128, S])
        I["tri"] = self.din("tri", [2, 128, 128])
        I["amask"] = self.din("amask", [2, 128, 64])
        I["ident"] = self.din("ident", [128, 128])
        I["ustrict"] = self.din("ustrict", [128, 128])
        I["ebase1"] = self.din("ebase1", [1, E])
        I["na_w_in"] = self.din("na_w_in", [D, 3 * D])
        I["na_w_out"] = self.din("na_w_out", [D, D])
        I["na_bias"] = self.din("na_bias", [16, 64, 960])
        I["router_w"] = self.din("router_w", [2, D, E])
        I["router_b"] = self.din("router_b", [2, 1, E])
        ne = self.ne_decl = (max(self.experts) + 1) if self.experts else 1
        I["w_gate"] = self.din("w_gate", [2, ne, D, 256])
        I["w_up"] = self.din("w_up", [2, ne, D, 256])
        I["w_down"] = self.din("w_down", [2, ne, 256, D])
        I["ws_gate"] = self.din("ws_gate", [2, D, 256])
        I["ws_up"] = self.din("ws_up", [2, D, 256])
        I["ws_down"] = self.din("ws_down", [2, 256, D])
        self.out = nc.dram_tensor("out", [nbl, S, D], F32, kind="ExternalOutput").ap()
        self.xres = [nc.dram_tensor("xres%d" % b, [128, 8, T], F32).ap() for b in range(nbl)]
        self.bxres = [[k.buf("xres") for _ in range(NT)] for b in range(nbl)]
        self.bout = k.buf("out")
        self.NSH = E * CAP
        self.xs = nc.dram_tensor("xs_scr", [E * CAP, D], BF16).ap()
        self.xsh = nc.dram_tensor("xsh_scr", [nbl * NT * 128, D], BF16).ap()
        self.ys2 = [nc.dram_tensor("ys_scr%d" % hf, [E * CAP, 512], F32).ap() for hf in range(2)]
        self.ysh_d = nc.dram_tensor("ysh_scr", [nbl * NT * 128, D], F32).ap()

        self.PS = [nc.alloc_psum_tensor("ps%d" % i, [128, 512], F32).ap() for i in range(8)]
        self.bPS = k.bufs(8, "ps")
        self.PSB = self.PS[7].bitcast(BF16)

        es = self.es
        self.ident, self.bident = self.load_const(es, "ident", I["ident"], [128, 128])
        self.identb = self.sb(es, "identb", [128, 128], BF16)
        self.bidentb = k.buf("identb")
        k.v(lambda e: e.tensor_copy(self.identb, self.ident), [self.bident], [self.bidentb])
        self.ones = self.sb(es, "ones", [128, 128], F32)
        self.bones = k.buf("ones")
        k.v(lambda e: e.memset(self.ones, 1.0), [], [self.bones])

        self.onesb = self.sb(es, "onesb", [128, 128], BF16)
        k.v(lambda e: e.memset(self.onesb, 1.0), [], [self.bones])
        us32, bus = self.load_const(es, "us32", I["ustrict"], [128, 128])
        self.usb = self.sb(es, "usb", [128, 128], BF16)
        k.v(lambda e: e.tensor_copy(self.usb, us32), [bus], [self.bones])
        self.ebase1, _ = self.load_const(es, "ebase1", I["ebase1"].to_broadcast([128, E]), [128, E])
        self.slotI = self.sb(es, "slotI", [128, nbl * NT, 8], U32)
        self.gate8 = self.sb(es, "gate8", [128, nbl * NT, 8], F32)
        self.bslot = k.bufs(nbl * NT, "slot")
        self.dbg_out = None
        if os.environ.get("SLOT_DBG"):
            self.dbg_out = nc.dram_tensor("dbg", [128, nbl * NT * 8], F32, kind="ExternalOutput").ap()
            self.s8all = self.sb(es, "s8all", [128, nbl * NT, 8], F32)
            k.v(lambda e: e.memset(self.s8all, 0.0), [], [self.bones])

        self.compute_mods()
        for b in range(nbl):
            self.load_x(b)
        if self.stop != "load":
            for l in range(2):
                for b in range(nbl):
                    (self.mixer0 if l == 0 else self.mixer1)(b)
                if self.stop in ("mix%d" % l, "m0_norm", "m0_conv", "m0_proj", "m0_scan"):
                    break
                self.moe_layer(l, T if l == 0 else S)
                if self.stop == "moe%d" % l:
                    break
        if self.dbg_out is not None:
            k.barrier()
            k.dma("sp", self.dbg_out, self.s8all.rearrange("p a b -> p (a b)"), [], [self.bout])
        for b in range(nbl):
            self.dump_x(b)
        deps = dict(self.bout.w)
        k._wait("sp", deps)
        es.close()

    def compute_mods(self):
        nc, k, I = self.nc, self.k, self.I
        self.mod = []
        self.bmod = k.buf("mod")
        self.modv = self.sb(self.es, "modv", [128, 2, 6, 3, 8], F32)
        with ExitStack() as es:
            cv, bcv = self.load_const(es, "cvec", I["cvec"], [128, 8, 4])
            sc = self.sb(es, "silc", [128, 8, 4], F32)
            bsc = k.buf("silc")
            k.act(sc, cv, AF.Silu, [bcv], [bsc])
            wbuf = [self.sb(es, "adaw", [128, 8, 512], F32) for _ in range(2)]
            bw = k.bufs(2, "adaw")
            gv, bgv = self.load_const(es, "gvec", I["gvec"].rearrange("l p a c -> p l a c"), [128, 2, 4, 8])
            abt, babt = self.load_const(es, "adab", I["ada_bT"].rearrange("l p j -> p l j"), [128, 2, 48])
            modraw = self.sb(es, "modraw", [128, 2, 3, 48], F32)
            bmr = k.buf("modraw")
            ps = self.PS[0]
            bps = self.bPS[0]
            i = 0
            for l in range(2):
                for ch in range(12):
                    w = wbuf[i % 2]
                    k.dma("sp", w, I["ada_w"][l, :, ch * 512:(ch + 1) * 512].rearrange("(c p) n -> p c n", p=128),
                          [], [bw[i % 2]])
                    for jj in range(4):
                        j = ch * 4 + jj
                        for kc in range(8):
                            k.mm(ps[:, j * 4:(j + 1) * 4], w[:, kc, jj * 128:(jj + 1) * 128], sc[:, kc, :],
                                 kc == 0, kc == 7, [bw[i % 2], bsc], [bps])
                    i += 1
                for v in range(3):
                    k.v(lambda e: e.tensor_tensor(modraw[:, l, v, :],
                                                  ps[:, 0:192].rearrange("p (j v) -> p j v", v=4)[:, :, v],
                                                  abt[:, l, :], ALU.add), [bps, babt], [bmr])
            mv = self.modv
            for l in range(2):
                for v in range(3):
                    for s_ in range(2):
                        sh = modraw[:, l, v, (3 * s_ + 0) * 8:(3 * s_ + 0) * 8 + 8]
                        scl = modraw[:, l, v, (3 * s_ + 1) * 8:(3 * s_ + 1) * 8 + 8]
                        gt = modraw[:, l, v, (3 * s_ + 2) * 8:(3 * s_ + 2) * 8 + 8]
                        gpre = gv[:, l, 2 * s_ + 0, :]
                        gpost = gv[:, l, 2 * s_ + 1, :]
                        A = mv[:, l, 3 * s_ + 0, v, :]
                        k.v(lambda e: e.scalar_tensor_tensor(A, scl, 1.0, gpre, ALU.add, ALU.mult), [bmr, bgv], [self.bmod])
                        k.v(lambda e: e.tensor_copy(mv[:, l, 3 * s_ + 1, v, :], sh), [bmr], [self.bmod])
                        k.v(lambda e: e.tensor_tensor(mv[:, l, 3 * s_ + 2, v, :], gt, gpost, ALU.mult), [bmr, bgv], [self.bmod])
            k.barrier()

    def modvec(self, l, s_, kind, v):
        return self.modv[:, l, 3 * s_ + kind, v, :]

    def load_x(self, b):
        k, I = self.k, self.I
        with ExitStack() as es:
            xin = [self.sb(es, "xin", [128, D], F32) for _ in range(2)]
            bxin = k.bufs(2, "xin")
            xt = [self.sb(es, "xt", [128, 8, 128], F32) for _ in range(2)]
            bxt = k.bufs(2, "xt")
            for i in range(NT):
                src = I["x"][b, i * 128:(i + 1) * 128, :] if i < 16 else I["ctx"][b, (i - 16) * 128:(i - 15) * 128, :]
                p = i % 2
                k.dma("sp", xin[p], src, [], [bxin[p]])
                pa, pb = self.PS[2 * p], self.PS[2 * p + 1]
                for c in range(8):
                    ps = (pa if c < 4 else pb)[:, (c % 4) * 128:(c % 4 + 1) * 128]
                    k.tr(ps, xin[p][:, c * 128:(c + 1) * 128], self.ident, [bxin[p], self.bident],
                         [self.bPS[2 * p + (c // 4)]])
                k.act(xt[p][:, 0:4, :], pa.rearrange("p (c t) -> p c t", c=4), AF.Copy, [self.bPS[2 * p]], [bxt[p]])
                k.v(lambda e: e.tensor_copy(xt[p][:, 4:8, :], pb.rearrange("p (c t) -> p c t", c=4)),
                    [self.bPS[2 * p + 1]], [bxt[p]])
                k.dma("sp", self.xres[b][:, :, i * 128:(i + 1) * 128], xt[p], [bxt[p]], [self.bxres[b][i]])
            k.barrier()

    def dump_x(self, b):
        k = self.k
        with ExitStack() as es:
            xt = [self.sb(es, "dxt", [128, 8, 128], F32) for _ in range(2)]
            bxt = k.bufs(2, "dxt")
            xo = [self.sb(es, "dxo", [128, D], F32) for _ in range(2)]
            bxo = k.bufs(2, "dxo")
            for i in range(16):
                p = i % 2
                k.dma("sp", xt[p], self.xres[b][:, :, i * 128:(i + 1) * 128], [self.bxres[b][i]], [bxt[p]])
                pa, pb = self.PS[2 * p], self.PS[2 * p + 1]
                for c in range(8):
                    ps = (pa if c < 4 else pb)[:, (c % 4) * 128:(c % 4 + 1) * 128]
                    k.tr(ps, xt[p][:, c, :], self.ident, [bxt[p], self.bident], [self.bPS[2 * p + (c // 4)]])
                k.act(xo[p][:, 0:512], pa, AF.Copy, [self.bPS[2 * p]], [bxo[p]])
                k.v(lambda e: e.tensor_copy(xo[p][:, 512:1024], pb), [self.bPS[2 * p + 1]], [bxo[p]])
                k.dma("sp", self.out[b, i * 128:(i + 1) * 128, :], xo[p], [bxo[p]], [self.bout])
            k.barrier()

    def norm_mod(self, hT, bh, b, l, s_, t_end, router=None):
        k = self.k
        NBK = 256
        with ExitStack() as es:
            xb = [self.sb(es, "nx", [128, 8, NBK], F32) for _ in range(2)]
            bxb = k.bufs(2, "nx")
            sq = self.sb(es, "nsq", [128, 8, NBK], F32)
            bsq = k.buf("nsq")
            rs = self.sb(es, "nrs", [128, NBK], F32)
            brs = k.buf("nrs")
            tmp = self.sb(es, "ntmp", [128, NBK], F32)
            btmp = k.buf("ntmp")
            h32 = self.sb(es, "nh32", [128, 8, NBK], F32) if router else None
            bh32 = k.buf("nh32")
            blks = blocks(0, min(t_end, S), NBK) + (blocks(S, t_end, NBK) if t_end > S else [])
            for bi, (t0, n) in enumerate(blks):
                v = b if t0 < S else 2
                A = self.modvec(l, s_, 0, v)
                sh = self.modvec(l, s_, 1, v)
                p = bi % 2
                tiles = list(range(t0 // 128, (t0 + n) // 128))
                k.dma("sp", xb[p][:, :, 0:n], self.xres[b][:, :, t0:t0 + n], [self.bxres[b][i] for i in tiles], [bxb[p]])
                k.act(sq[:, :, 0:n], xb[p][:, :, 0:n], AF.Square, [bxb[p]], [bsq])
                ps = self.PS[6]
                for c in range(8):
                    k.mm(ps[:, 0:n], self.ones, sq[:, c, 0:n], c == 0, c == 7, [self.bones, bsq], [self.bPS[6]])
                k.act(rs[:, 0:n], ps[:, 0:n], AF.Sqrt, [self.bPS[6]], [brs], scale=1.0 / D, bias=RMS_EPS)
                k.v(lambda e: e.reciprocal(rs[:, 0:n], rs[:, 0:n]), [brs], [brs])
                for c in range(8):
                    k.v(lambda e: e.tensor_tensor(tmp[:, 0:n], xb[p][:, c, 0:n], rs[:, 0:n], ALU.mult), [bxb[p], brs], [btmp])
                    if router:
                        k.act(h32[:, c, 0:n], tmp[:, 0:n], AF.Identity, [btmp, self.bmod], [bh32],
                              scale=A[:, c:c + 1], bias=sh[:, c:c + 1])
                    else:
                        k.act(hT[:, c, t0:t0 + n], tmp[:, 0:n], AF.Identity, [btmp, self.bmod], [bh[i] for i in tiles],
                              scale=A[:, c:c + 1], bias=sh[:, c:c + 1])
                if router:
                    rw, brw, cb = router
                    for ti, tile in enumerate(tiles):
                        pr = self.PS[4 + ti % 2]
                        bpr = self.bPS[4 + ti % 2]
                        for c in range(8):
                            k.mm(pr[:, 0:E], h32[:, c, ti * 128:(ti + 1) * 128], rw[:, c, :], c == 0, c == 7, [bh32, brw], [bpr])
                        cb(tile, ti, pr[:, 0:E], bpr, h32, bh32)
            k.barrier()

    def lin_fm(self, ps, bps, W, bW, col0, M, inT, bin_, t0, n, nk=8):
        for kc in range(nk):
            self.k.mm(ps[0:M, 0:n], W[:, kc, col0:col0 + M], inT[:, kc, t0:t0 + n], kc == 0, kc == nk - 1,
                      [bW] + bin_, [bps])

    def out_proj(self, b, l, W, bW, mixT, bmix_of_tile, t_end):
        k = self.k
        with ExitStack() as es:
            yb = self.sb(es, "oy", [128, 8, 256], F32)
            byb = k.buf("oy")
            sq = self.sb(es, "osq", [128, 8, 256], F32)
            bsq = k.buf("osq")
            rs = self.sb(es, "ors", [128, 256], F32)
            brs = k.buf("ors")
            xb = [self.sb(es, "ox", [128, 8, 256], F32) for _ in range(2)]
            bxb = k.bufs(2, "ox")
            tmp = self.sb(es, "otmp", [128, 256], F32)
            btmp = k.buf("otmp")
            blks = blocks(0, min(t_end, S), 256) + (blocks(S, t_end, 256) if t_end > S else [])
            for bi, (t0, n) in enumerate(blks):
                v = b if t0 < S else 2
                G = self.modvec(l, 0, 2, v)
                p = bi % 2
                tiles = list(range(t0 // 128, (t0 + n) // 128))
                bin_ = [bb for i in tiles for bb in bmix_of_tile(i)]
                k.dma("sp", xb[p], self.xres[b][:, :, t0:t0 + n], [self.bxres[b][i] for i in tiles], [bxb[p]])
                for m in range(8):
                    ps = self.PS[m // 2][:, (m % 2) * 256:(m % 2) * 256 + 256]
                    self.lin_fm(ps, self.bPS[m // 2], W, bW, m * 128, 128, mixT, bin_, t0, n)
                for q in range(4):
                    eng = "act" if q % 2 == 0 else "dve"
                    src = self.PS[q].rearrange("p (c t) -> p c t", c=2)
                    if eng == "act":
                        k.act(yb[:, 2 * q:2 * q + 2, :], src, AF.Copy, [self.bPS[q]], [byb])
                    else:
                        k.v(lambda e: e.tensor_copy(yb[:, 2 * q:2 * q + 2, :], src), [self.bPS[q]], [byb])
                k.act(sq, yb, AF.Square, [byb], [bsq])
                ps = self.PS[6]
                for c in range(8):
                    k.mm(ps[:, 0:n], self.ones, sq[:, c, :], c == 0, c == 7, [self.bones, bsq], [self.bPS[6]])
                k.act(rs, ps[:, 0:n], AF.Sqrt, [self.bPS[6]], [brs], scale=1.0 / D, bias=RMS_EPS)
                k.v(lambda e: e.reciprocal(rs, rs), [brs], [brs])
                for c in range(8):
                    k.v(lambda e: e.tensor_tensor(tmp, yb[:, c, :], rs, ALU.mult), [byb, brs], [btmp])
                    k.v(lambda e: e.scalar_tensor_tensor(xb[p][:, c, :], tmp, G[:, c:c + 1], xb[p][:, c, :], ALU.mult, ALU.add),
                        [btmp, self.bmod, bxb[p]], [bxb[p]])
                k.dma("sp", self.xres[b][:, :, t0:t0 + n], xb[p], [bxb[p]], [self.bxres[b][i] for i in tiles])
            k.barrier()

    def mixer0(self, b):
        nc, k, I = self.nc, self.k, self.I
        with ExitStack() as esm:
            cvT = self.sb(esm, "cvT", [128, 4, T], BF16)
            bcv = k.bufs(NT, "cvT")
            with ExitStack() as esg:
                with ExitStack() as esh:
                    hT = self.sb(esh, "hT", [128, 8, T], BF16)
                    bh = k.bufs(NT, "hT")
                    self.norm_mod(hT, bh, b, 0, 0, T)
                    if self.stop == "m0_norm":
                        return
                    self.conv_branch(hT, bh, cvT, bcv)
                    if self.stop == "m0_conv":
                        return
                    qT = self.sb(esg, "qT", [128, 2, T], BF16)
                    kT = self.sb(esg, "kT", [128, 2, T], BF16)
                    bqk = k.bufs(NT, "qk")
                    v_tm = self.sb(esg, "v_tm", [128, NT, 512], BF16)
                    sg_tm = self.sb(esg, "sg_tm", [128, NT, 512], BF16)
                    bvg = k.bufs(NT, "vg")
                    lrT = [self.sb(esg, "lrT", [32, T], F32) for _ in range(2)]
                    blr = k.bufs(2, "lrT")
                    self.gla_proj(hT, bh, qT, kT, bqk, v_tm, sg_tm, bvg, lrT, blr)
                    if self.stop == "m0_proj":
                        return
                aoT = self.sb(esm, "aoT", [128, 4, T], BF16)
                bao = k.bufs(NT, "aoT")
                self.gla_scan(qT, kT, bqk, v_tm, sg_tm, bvg, lrT, blr, aoT, bao)
                if self.stop == "m0_scan":
                    return
            with ExitStack() as es:
                W = self.sb(es, "wout", [128, 8, D], BF16)
                bW = k.buf("wout")
                k.dma("pool", W, I["ab_w_out"].rearrange("(c p) n -> p c n", p=128), [], [bW])

                class Mix:
                    def __getitem__(s, idx):
                        p_, kc, tsl = idx
                        return aoT[:, kc, tsl] if kc < 4 else cvT[:, kc - 4, tsl]
                self.out_proj(b, 0, W, bW, Mix(), lambda i: [bao[i], bcv[i]], T)

    def conv_branch(self, hT, bh, cvT, bcv):
        k, I = self.k, self.I
        with ExitStack() as es:
            W = self.sb(es, "wglu", [128, 8, 256], BF16)
            bW = k.buf("wglu")
            cw, bcw = self.load_const(es, "convw", I["convw"], [128, 4, 31])
            cvv, bcvv = self.load_const(es, "convv", I["convv"], [128, 3, 4])
            u = self.sb(es, "cu", [128, T], F32)
            bu = k.buf("cu")
            acc = self.sb(es, "cacc", [128, 4, T], F32)
            bacc = k.bufs(4, "cacc")
            sg = self.sb(es, "csg", [128, 512], F32)
            bsg = k.buf("csg")
            blks = blocks(0, S, 512) + blocks(S, T, 512)
            wg = I["w_glu"].rearrange("(c p) n -> p c n", p=128)
            for cc in range(4):
                k.dma("pool", W[:, :, 0:128], wg[:, :, cc * 128:(cc + 1) * 128], [], [bW])
                k.dma("pool", W[:, :, 128:256], wg[:, :, 512 + cc * 128:512 + (cc + 1) * 128], [], [bW])
                for bi, (t0, n) in enumerate(blks):
                    tiles = list(range(t0 // 128, (t0 + n) // 128))
                    bin_ = [bh[i] for i in tiles]
                    pa, pb = self.PS[2 * (bi % 2)], self.PS[2 * (bi % 2) + 1]
                    bpa, bpb = self.bPS[2 * (bi % 2)], self.bPS[2 * (bi % 2) + 1]
                    self.lin_fm(pa, bpa, W, bW, 0, 128, hT, bin_, t0, n)
                    self.lin_fm(pb, bpb, W, bW, 128, 128, hT, bin_, t0, n)
                    k.act(sg[:, 0:n], pb[:, 0:n], AF.Sigmoid, [bpb], [bsg])
                    k.v(lambda e: e.tensor_tensor(u[:, t0:t0 + n], pa[:, 0:n], sg[:, 0:n], ALU.mult), [bpa, bsg], [bu])
                a = acc[:, cc, :]
                k.v(lambda e: e.tensor_scalar(a, u, cw[:, cc, 15:16], cvv[:, 0, cc:cc + 1], ALU.mult, ALU.add),
                    [bu, bcw, bcvv], [bacc[cc]])
                for (lo, hi) in ((0, S), (S, T)):
                    for j in range(31):
                        d = j - 15
                        if d == 0:
                            continue
                        o0, o1 = lo + max(0, -d), hi - max(0, d)
                        k.v(lambda e: e.scalar_tensor_tensor(a[:, o0:o1], u[:, o0 + d:o1 + d], cw[:, cc, j:j + 1], a[:, o0:o1],
                                                             ALU.mult, ALU.add), [bu, bcw, bacc[cc]], [bacc[cc]])
            sq = self.sb(es, "csq", [128, 4, 256], F32)
            bsq = k.buf("csq")
            mean = self.sb(es, "cmean", [128, 256], F32)
            msq = self.sb(es, "cmsq", [128, 256], F32)
            rstd = self.sb(es, "crstd", [128, 256], F32)
            bst = k.buf("cstat")
            tmp = self.sb(es, "ctmp", [128, 256], F32)
            btmp = k.buf("ctmp")
            for bi, (t0, n) in enumerate(blocks(0, T, 256)):
                tiles = list(range(t0 // 128, (t0 + n) // 128))
                ps1, ps2 = self.PS[4], self.PS[5]
                for cc in range(4):
                    k.mm(ps1[:, 0:n], self.ones, acc[:, cc, t0:t0 + n], cc == 0, cc == 3, [self.bones, bacc[cc]], [self.bPS[4]])
                k.act(sq[:, :, 0:n], acc[:, :, t0:t0 + n], AF.Square, bacc, [bsq])
                for cc in range(4):
                    k.mm(ps2[:, 0:n], self.ones, sq[:, cc, 0:n], cc == 0, cc == 3, [self.bones, bsq], [self.bPS[5]])
                k.act(mean[:, 0:n], ps1[:, 0:n], AF.Copy, [self.bPS[4]], [bst], scale=1.0 / 512)
                k.v(lambda e: e.tensor_tensor(msq[:, 0:n], mean[:, 0:n], mean[:, 0:n], ALU.mult), [bst], [bst])
                k.v(lambda e: e.scalar_tensor_tensor(rstd[:, 0:n], ps2[:, 0:n], 1.0 / 512, msq[:, 0:n], ALU.mult, ALU.subtract),
                    [self.bPS[5], bst], [bst])
                k.act(rstd[:, 0:n], rstd[:, 0:n], AF.Sqrt, [bst], [bst], scale=1.0, bias=LN_EPS)
                k.v(lambda e: e.reciprocal(rstd[:, 0:n], rstd[:, 0:n]), [bst], [bst])
                for cc in range(4):
                    k.v(lambda e: e.tensor_tensor(tmp[:, 0:n], acc[:, cc, t0:t0 + n], mean[:, 0:n], ALU.subtract), [bacc[cc], bst], [btmp])
                    k.v(lambda e: e.tensor_tensor(tmp[:, 0:n], tmp[:, 0:n], rstd[:, 0:n], ALU.mult), [btmp, bst], [btmp])
                    k.act(cvT[:, cc, t0:t0 + n], tmp[:, 0:n], AF.Silu, [btmp, bcvv], [bcv[i] for i in tiles],
                          scale=cvv[:, 1, cc:cc + 1], bias=cvv[:, 2, cc:cc + 1])
            k.barrier()

    def gla_proj(self, hT, bh, qT, kT, bqk, v_tm, sg_tm, bvg, lrT, blr):
        k, I = self.k, self.I
        with ExitStack() as es:
            Wqk = self.sb(es, "wqk", [128, 8, 1024], BF16)
            bWqk = k.buf("wqk")
            k.dma("pool", Wqk, I["w_qk"].rearrange("(c p) n -> p c n", p=128), [], [bWqk])
            Wvg, bWvg = Wqk, bWqk
            Wlr = self.sb(es, "wlr", [128, 8, 32], BF16)
            bWlr = k.buf("wlr")
            k.dma("pool", Wlr, I["w_lr"].rearrange("(c p) n -> p c n", p=128), [], [bWlr])
            rope, brope = self.load_const(es, "rope", I["rope"][0:2].rearrange("a p t -> p a t"), [128, 2, S])
            t1 = self.sb(es, "rt1", [128, 512], F32)
            t2 = self.sb(es, "rt2", [128, 512], F32)
            bt = k.bufs(2, "rt")
            blks = blocks(0, S, 512) + blocks(S, T, 512)
            for hp in range(2):
                for bi, (t0, n) in enumerate(blks):
                    tiles = list(range(t0 // 128, (t0 + n) // 128))
                    bin_ = [bh[i] for i in tiles]
                    for qi, dst in enumerate((qT, kT)):
                        pa, pb = self.PS[2 * qi], self.PS[2 * qi + 1]
                        bpa, bpb = self.bPS[2 * qi], self.bPS[2 * qi + 1]
                        self.lin_fm(pa, bpa, Wqk, bWqk, qi * 256 + hp * 128, 128, hT, bin_, t0, n)
                        if t0 < S:
                            self.lin_fm(pb, bpb, Wqk, bWqk, 512 + qi * 256 + hp * 128, 128, hT, bin_, t0, n)
                            k.v(lambda e: e.tensor_tensor(t1[:, 0:n], pa[:, 0:n], rope[:, 0, t0:t0 + n], ALU.mult), [bpa, brope], [bt[0]])
                            k.v(lambda e: e.tensor_tensor(t2[:, 0:n], pb[:, 0:n], rope[:, 1, t0:t0 + n], ALU.mult), [bpb, brope], [bt[1]])
                            k.v(lambda e: e.tensor_tensor(dst[:, hp, t0:t0 + n], t1[:, 0:n], t2[:, 0:n], ALU.add), bt, [bqk[i] for i in tiles])
                        else:
                            k.act(dst[:, hp, t0:t0 + n], pa[:, 0:n], AF.Copy, [bpa], [bqk[i] for i in tiles])
            k.dma("pool", Wvg, I["w_vg"].rearrange("(c p) n -> p c n", p=128), [], [bWvg])
            for i in range(NT):
                pv, pg = self.PS[2 * (i % 2)], self.PS[2 * (i % 2) + 1]
                bpv, bpg = self.bPS[2 * (i % 2)], self.bPS[2 * (i % 2) + 1]
                for kc in range(8):
                    k.mm(pv, hT[:, kc, i * 128:(i + 1) * 128], Wvg[:, kc, 0:512], kc == 0, kc == 7, [bh[i], bWvg], [bpv])
                for kc in range(8):
                    k.mm(pg, hT[:, kc, i * 128:(i + 1) * 128], Wvg[:, kc, 512:1024], kc == 0, kc == 7, [bh[i], bWvg], [bpg])
                k.v(lambda e: e.tensor_copy(v_tm[:, i, :], pv), [bpv], [bvg[i]])
                k.act(sg_tm[:, i, :], pg, AF.Silu, [bpg], [bvg[i]])
            for d in range(2):
                k.v(lambda e: e.memset(lrT[d], 1.0), [], [blr[d]])
                for bi, (t0, n) in enumerate(blks):
                    tiles = list(range(t0 // 128, (t0 + n) // 128))
                    ps = self.PS[4 + bi % 2]
                    bps = self.bPS[4 + bi % 2]
                    self.lin_fm(ps, bps, Wlr, bWlr, d * 16, 16, hT, [bh[i] for i in tiles], t0, n)
                    k.v(lambda e: e.tensor_copy(lrT[d][0:16, t0:t0 + n], ps[0:16, 0:n]), [bps], [blr[d]])
            k.barrier()

    def gla_scan(self, qT, kT, bqk, v_tm, sg_tm, bvg, lrT, blr, aoT, bao):
        k, I = self.k, self.I
        with ExitStack() as es:
            dwb = [self.load_const(es, "dwb", I["dwb_f" if d == 0 else "dwb_b"], [17, 256]) for d in range(2)]
            tri, btri = self.load_const(es, "tri", I["tri"].rearrange("a p t -> p a t"), [128, 2, 128])
            am, bam = self.load_const(es, "amask", I["amask"].rearrange("a p t -> p a t"), [128, 2, 64])
            gg, bgg = self.load_const(es, "glag", I["gla_g"].to_broadcast([128, 512]), [128, 512])
            o_f = self.sb(es, "o_f", [128, NT, 512], F32)
            bof = k.bufs(NT, "o_f")
            S32 = self.sb(es, "S32", [128, 2, 128], F32)
            Sb = self.sb(es, "Sb", [128, 2, 128], BF16)
            bS = [[k.buf("S") for _ in range(2)] for _ in range(2)]
            ex = self.sb(es, "gex", [128, 256], F32)
            ll = self.sb(es, "gll", [128, 256], F32)
            bll = k.buf("gll")
            NB_ = 2
            bT = [[self.sb(es, "gbT", [128, 128], F32) for _ in range(2)] for _ in range(NB_)]
            E3 = [[self.sb(es, "gE3", [128, 128], F32) for _ in range(2)] for _ in range(NB_)]
            E1 = [[self.sb(es, "gE1", [128, 128], F32) for _ in range(2)] for _ in range(NB_)]
            E2 = [[self.sb(es, "gE2", [128, 128], F32) for _ in range(2)] for _ in range(NB_)]
            E4 = [[self.sb(es, "gE4", [128, 128], F32) for _ in range(2)] for _ in range(NB_)]
            qe = [[self.sb(es, "gqe", [128, 128], BF16) for _ in range(2)] for _ in range(NB_)]
            ke = [[self.sb(es, "gke", [128, 128], BF16) for _ in range(2)] for _ in range(NB_)]
            qi_ = [[self.sb(es, "gqi", [128, 128], BF16) for _ in range(2)] for _ in range(NB_)]
            koT = [[self.sb(es, "gko", [128, 128], BF16) for _ in range(2)] for _ in range(NB_)]
            ko_tm = [[self.sb(es, "gkt", [128, 128], BF16) for _ in range(2)] for _ in range(NB_)]
            bprep = [[k.buf("gprep") for _ in range(2)] for _ in range(NB_)]
            aTm = [self.sb(es, "gaTm", [128, 64], BF16) for _ in range(2)]
            baTm = k.bufs(2, "gaTm")
            osum = self.sb(es, "gosum", [128, 512], F32)
            bosum = k.buf("gosum")
            ssq = self.sb(es, "gssq", [128, 4], F32)
            junk = self.sb(es, "gjunk", [128, 128], F32)
            bssq = k.buf("gssq")
            ao = self.sb(es, "gao", [128, 512], F32)
            aob = self.sb(es, "gaob", [128, 512], BF16)
            bao_t = k.buf("gao")

            step = 0
            for d in range(2):
                for hp in range(2):
                    k.v(lambda e: e.memset(S32[:, hp, :], 0.0), [], bS[hp])
                    k.v(lambda e: e.memset(Sb[:, hp, :], 0.0), [], bS[hp])
                order = [16, 17] + list(range(16)) if d == 0 else [17, 16] + list(range(15, -1, -1))
                for i in order:
                    pp = step % NB_
                    step += 1
                    c0 = i * 128
                    px = self.PS[6]
                    k.mm(px[:, 0:256], lrT[d][0:17, c0:c0 + 128], dwb[d][0], True, True, [blr[d], dwb[d][1]], [self.bPS[6]])
                    k.act(ex, px[:, 0:256], AF.Exp, [self.bPS[6]], [bll], scale=-1.0)
                    k.act(ll, ex, AF.Ln, [bll], [bll], scale=1.0, bias=1.0)
                    if DBG < 2:
                        continue
                    for hp in range(2):
                        bp = bprep[pp][hp]
                        pb_ = self.PS[4 + hp]
                        k.mm(pb_[:, 0:128], ll[:, hp * 128:(hp + 1) * 128], tri[:, d, :], True, True, [bll, btri], [self.bPS[4 + hp]])
                        b_ = bT[pp][hp]
                        k.v(lambda e: e.tensor_copy(b_, pb_[:, 0:128]), [self.bPS[4 + hp]], [bp])
                        k.act(E3[pp][hp], b_, AF.Exp, [bp], [bp])
                        for ch in range(2):
                            cs = ch * 64
                            mid = cs + (31 if d == 0 else 32)
                            last = cs + (63 if d == 0 else 0)
                            k.act(E2[pp][hp][:, cs:cs + 64], b_[:, cs:cs + 64], AF.Exp, [bp], [bp], scale=-1.0, bias=b_[:, mid:mid + 1])
                            k.act(E4[pp][hp][:, cs:cs + 64], b_[:, cs:cs + 64], AF.Exp, [bp], [bp], scale=-1.0, bias=b_[:, last:last + 1])
                        k.v(lambda e: e.reciprocal(E1[pp][hp], E2[pp][hp]), [bp], [bp])
                        qs = qT[:, hp, c0:c0 + 128]
                        ks = kT[:, hp, c0:c0 + 128]
                        k.v(lambda e: e.tensor_tensor(qe[pp][hp], qs, E1[pp][hp], ALU.mult), [bp, bqk[i]], [bp])
                        k.v(lambda e: e.tensor_tensor(ke[pp][hp], ks, E2[pp][hp], ALU.mult), [bp, bqk[i]], [bp])
                        k.v(lambda e: e.tensor_tensor(qi_[pp][hp], qs, E3[pp][hp], ALU.mult), [bp, bqk[i]], [bp], eng="pool")
                        k.v(lambda e: e.tensor_tensor(koT[pp][hp], ks, E4[pp][hp], ALU.mult), [bp, bqk[i]], [bp], eng="pool")
                        if DBG < 3:
                            continue
                        ptr = self.PSB[:, hp * 128:(hp + 1) * 128]
                        k.tr(ptr, koT[pp][hp], self.identb, [bp, self.bidentb], [self.bPS[7]])
                        k.v(lambda e: e.tensor_copy(ko_tm[pp][hp], ptr), [self.bPS[7]], [bp])
                    if DBG < 4:
                        continue
                    for ch in ((0, 1) if d == 0 else (1, 0)):
                        cs = ch * 64
                        last = cs + (63 if d == 0 else 0)
                        for hp in range(2):
                            bp = bprep[pp][hp]
                            for h2 in range(2):
                                hd = hp * 2 + h2
                                pr = h2 * 64
                                pa = self.PS[0]
                                bpa = self.bPS[0]
                                am_ = aTm[hd % 2]
                                a0 = (hd % 2) * 64
                                k.mm(pa[cs:cs + 64, a0:a0 + 64], ke[pp][hp][pr:pr + 64, cs:cs + 64], qe[pp][hp][pr:pr + 64, cs:cs + 64],
                                     True, True, [bp], [bpa])
                                k.v(lambda e: e.tensor_tensor(am_[cs:cs + 64, :], pa[cs:cs + 64, a0:a0 + 64], am[cs:cs + 64, d, :], ALU.mult),
                                    [bpa, bam], [baTm[hd % 2]])
                                if DBG < 5:
                                    continue
                                po = self.PS[2]
                                bpo = self.bPS[2]
                                k.mm(po[cs:cs + 64, hd * 128:(hd + 1) * 128], am_[cs:cs + 64, :], v_tm[cs:cs + 64, i, hd * 128:(hd + 1) * 128],
                                     True, True, [baTm[hd % 2], bvg[i]], [bpo])
                                k.mm(self.PS[1][cs:cs + 64, hd * 128:(hd + 1) * 128], qi_[pp][hp][pr:pr + 64, cs:cs + 64], Sb[pr:pr + 64, hp, :],
                                     True, True, [bp, bS[hp][h2]], [self.bPS[1]])
                                if DBG < 6:
                                    continue
                                pS = self.PS[3]
                                bpS = self.bPS[3]
                                k.mm(pS[pr:pr + 64, hp * 128:(hp + 1) * 128], ko_tm[pp][hp][cs:cs + 64, pr:pr + 64],
                                     v_tm[cs:cs + 64, i, hd * 128:(hd + 1) * 128], True, True, [bp, bvg[i]], [bpS])
                                k.v(lambda e: e.scalar_tensor_tensor(S32[pr:pr + 64, hp, :], S32[pr:pr + 64, hp, :],
                                                                     E3[pp][hp][pr:pr + 64, last:last + 1],
                                                                     pS[pr:pr + 64, hp * 128:(hp + 1) * 128], ALU.mult, ALU.add),
                                    [bp, bpS, bS[hp][h2]], [bS[hp][h2]])
                                k.act(Sb[pr:pr + 64, hp, :], S32[pr:pr + 64, hp, :], AF.Copy, [bS[hp][h2]], [bS[hp][h2]])
                    if DBG < 7:
                        continue
                    po = self.PS[2]
                    bpo = self.bPS[2]
                    if d == 0:
                        k.act(o_f[:, i, :], po, AF.Copy, [bpo], [bof[i]])
                        k.v(lambda e: e.tensor_tensor(o_f[:, i, :], self.PS[1], o_f[:, i, :], ALU.add), [self.bPS[1], bof[i]], [bof[i]])
                    else:
                        k.v(lambda e: e.tensor_tensor(osum, po, o_f[:, i, :], ALU.add), [bpo, bof[i]], [bosum])
                        k.v(lambda e: e.tensor_tensor(osum, self.PS[1], osum, ALU.add), [self.bPS[1], bosum], [bosum])
                        for hd in range(4):
                            k.act(junk, osum[:, hd * 128:(hd + 1) * 128], AF.Square, [bosum], [bssq], accum_out=ssq[:, hd:hd + 1])
                        k.act(ssq, ssq, AF.Sqrt, [bssq], [bssq], scale=1.0 / (64.0 * 128.0), bias=RMS_EPS)
                        k.v(lambda e: e.reciprocal(ssq, ssq), [bssq], [bssq])
                        for hd in range(4):
                            k.v(lambda e: e.tensor_scalar(ao[:, hd * 128:(hd + 1) * 128], osum[:, hd * 128:(hd + 1) * 128],
                                                          ssq[:, hd:hd + 1], 0.125, ALU.mult, ALU.mult), [bosum, bssq], [bao_t])
                        k.v(lambda e: e.tensor_tensor(ao, ao, gg, ALU.mult), [bao_t, bgg], [bao_t])
                        k.v(lambda e: e.tensor_tensor(aob, ao, sg_tm[:, i, :], ALU.mult), [bao_t, bvg[i]], [bao_t])
                        for c in range(4):
                            ptr = self.PSB[:, 512 + c * 128:512 + (c + 1) * 128]
                            k.tr(ptr, aob[:, c * 128:(c + 1) * 128], self.identb, [bao_t, self.bidentb], [self.bPS[7]])
                        k.act(aoT[:, :, c0:c0 + 128], self.PSB[:, 512:1024].rearrange("p (c t) -> p c t", c=4), AF.Copy,
                              [self.bPS[7]], [bao[i]])
            k.barrier()

    def mixer1(self, b):
        k, I = self.k, self.I
        with ExitStack() as esm:
          with ExitStack() as esq:
            with ExitStack() as esh:
                hT = self.sb(esh, "hT", [128, 8, T], BF16)
                bh = k.bufs(NT, "hT")
                self.norm_mod(hT, bh, b, 1, 0, T)
                qT = self.sb(esq, "nqT", [128, 8, S], BF16)
                kT = self.sb(esq, "nkT", [128, 8, T], BF16)
                bq = k.bufs(NT, "nq")
                bk = k.bufs(NT, "nk")
                v_tm = self.sb(esq, "nv", [128, NT, D], BF16)
                bv = k.bufs(NT, "nv")
                v_sh = self.sb(esq, "nvs", [128, 15, D], BF16)
                bvs = k.bufs(15, "nvs")
                with ExitStack() as es:
                    w = self.sb(es, "nw", [128, 8, D], BF16)
                    bW = k.buf("nw")
                    for j in range(3):
                        k.dma("pool", w, I["na_w_in"][:, j * D:(j + 1) * D].rearrange("(c p) n -> p c n", p=128), [], [bW])
                        if j < 2:
                            dst, bd, tend = (qT, bq, S) if j == 0 else (kT, bk, T)
                            blks = blocks(0, min(tend, S), 512) + (blocks(S, tend, 512) if tend > S else [])
                            n_ = 0
                            for (t0, n) in blks:
                                tiles = list(range(t0 // 128, (t0 + n) // 128))
                                for m in range(8):
                                    ps = self.PS[n_ % 4]
                                    bps = self.bPS[n_ % 4]
                                    n_ += 1
                                    self.lin_fm(ps, bps, w, bW, m * 128, 128, hT, [bh[i] for i in tiles], t0, n)
                                    if m % 2 == 0:
                                        k.act(dst[:, m, t0:t0 + n], ps[:, 0:n], AF.Copy, [bps], [bd[i] for i in tiles],
                                              scale=(0.125 if j == 0 else 1.0))
                                    else:
                                        k.v(lambda e: e.tensor_scalar(dst[:, m, t0:t0 + n], ps[:, 0:n], (0.125 if j == 0 else 1.0), None, ALU.mult),
                                            [bps], [bd[i] for i in tiles])
                        else:
                            n_ = 0
                            for (dstv, bdv, ntl, toff) in ((v_tm, bv, NT, 0), (v_sh, bvs, 15, 64)):
                                for i in range(ntl):
                                    c0 = toff + i * 128
                                    rd = [bh[c0 // 128], bh[(c0 + 127) // 128], bW]
                                    for hf in range(2):
                                        ps = self.PS[n_ % 4]
                                        bps = self.bPS[n_ % 4]
                                        n_ += 1
                                        for kc in range(8):
                                            k.mm(ps, hT[:, kc, c0:c0 + 128], w[:, kc, hf * 512:(hf + 1) * 512], kc == 0, kc == 7, rd, [bps])
                                        if hf == 0:
                                            k.act(dstv[:, i, 0:512], ps, AF.Copy, [bps], [bdv[i]])
                                        else:
                                            k.v(lambda e: e.tensor_copy(dstv[:, i, 512:1024], ps), [bps], [bdv[i]])
                    k.barrier()
            atT = self.sb(esm, "natT", [128, 8, S], BF16)
            bat = k.bufs(16, "nat")
            self.na_attention(qT, kT, bq, bk, v_tm, bv, v_sh, bvs, atT, bat)
            esq.close()
          if True:
            with ExitStack() as es:
                W = self.sb(es, "nwout", [128, 8, D], BF16)
                bW = k.buf("nwout")
                k.dma("pool", W, I["na_w_out"].rearrange("(c p) n -> p c n", p=128), [], [bW])
                self.out_proj(b, 1, W, bW, atT, lambda i: [bat[i]], S)

    def na_attention(self, qT, kT, bq, bk, v_tm, bv, v_sh, bvs, atT, bat):
        k, I = self.k, self.I
        with ExitStack() as es:
            Bh = [self.sb(es, "nB", [128, 960], F32) for _ in range(2)]
            bBh = k.bufs(2, "nB")
            NP = 2
            sc2 = [self.sb(es, "nsc", [128, 768], F32) for _ in range(2)]
            pe2 = [self.sb(es, "npe", [128, 768], F32) for _ in range(2)]
            pb2 = [self.sb(es, "npb", [128, 768], BF16) for _ in range(2)]
            st2 = [self.sb(es, "nst", [128, 4], F32) for _ in range(2)]
            bsm = k.bufs(NP, "nsm")
            pT = [self.sb(es, "npT", [128, 6, 64], BF16) for _ in range(NP)]
            bpT = k.bufs(NP, "npT")
            bpsw = k.bufs(NP, "npsw")
            bpsc = k.bufs(NP, "npsc")
            bpo_ = k.bufs(NP, "npo")
            bptb4 = k.bufs(NP, "nptb")
            PSB6 = self.PS[6].bitcast(BF16)
            units = [(h, r) for h in range(16) for r in range(32)]
            for g0 in range(0, len(units), NP):
                grp = units[g0:g0 + NP]
                h = grp[0][0]
                m, pr = h // 2, (h % 2) * 64
                B = Bh[h % 2]
                if grp[0][1] == 0:
                    k.dma("sp", B[0:64, :], I["na_bias"][h], [], [bBh[h % 2]])
                    k.dma("sp", B[64:128, :], I["na_bias"][h], [], [bBh[h % 2]])
                U = []
                for p, (h_, r) in enumerate(grp):
                    pair, hb = p, 0
                    r0 = min(max(r - 4, 0), 24)
                    w0 = r0 * 64
                    if w0 % 128 == 0:
                        chunks = [(j * 128, v_tm, bv, w0 // 128 + j) for j in range(4)]
                    else:
                        chunks = [(j * 128, v_sh, bvs, (w0 - 64) // 128 + j) for j in range(4)]
                    chunks += [(512, v_tm, bv, 16), (640, v_tm, bv, 17)]
                    psb_src = self.PSB if p == 0 else PSB6
                    U.append(dict(p=p, r=r, rows=slice(hb, hb + 64), hb=hb, s=r0 - r + 7, w0=w0, qtile=r // 2,
                                  ktiles=sorted(set(range(w0 // 128, (w0 + 511) // 128 + 1))),
                                  sc=sc2[pair], pe=pe2[pair], pb=pb2[pair], st=st2[pair],
                                  ps_w=self.PS[2 * pair], ps_c=self.PS[2 * pair + 1], chunks=chunks,
                                  ptb=psb_src[:, 0:384].rearrange("p (j q) -> p j q", q=64),
                                  po=self.PS[4 + pair][pr:pr + 64, 0:64],
                                  qs=qT[pr:pr + 64, m, r * 64:(r + 1) * 64]))
                for x in U:
                    p, rows = x["p"], x["rows"]
                    k.mm(x["ps_w"][rows, 0:512], x["qs"], kT[pr:pr + 64, m, x["w0"]:x["w0"] + 512], True, True,
                         [bq[x["qtile"]]] + [bk[i] for i in x["ktiles"]], [bpsw[p]])
                    k.mm(x["ps_c"][rows, 0:256], x["qs"], kT[pr:pr + 64, m, S:T], True, True, [bq[x["qtile"]], bk[16], bk[17]], [bpsc[p]])
                for x in U:
                    p, rows, s_ = x["p"], x["rows"], x["s"]
                    k.v(lambda e: e.tensor_tensor(x["sc"][rows, 0:512], x["ps_w"][rows, 0:512], B[rows, s_ * 64:s_ * 64 + 512], ALU.add),
                        [bpsw[p], bBh[h % 2]], [bsm[p]])
                    k.act(x["sc"][rows, 512:768], x["ps_c"][rows, 0:256], AF.Copy, [bpsc[p]], [bsm[p]])
                for x in U:
                    p, rows = x["p"], x["rows"]
                    k.v(lambda e: e.reduce_max(x["st"][rows, 0:1], x["sc"][rows, :], AX.X), [bsm[p]], [bsm[p]])
                for x in U:
                    p, rows = x["p"], x["rows"]
                    k.v(lambda e: e.tensor_scalar(x["st"][rows, 1:2], x["st"][rows, 0:1], -1.0, None, ALU.mult), [bsm[p]], [bsm[p]])
                for x in U:
                    p, rows = x["p"], x["rows"]
                    k.act(x["pe"][rows, :], x["sc"][rows, :], AF.Exp, [bsm[p]], [bsm[p]], scale=1.0, bias=x["st"][rows, 1:2],
                          accum_out=x["st"][rows, 2:3])
                for x in U:
                    p, rows = x["p"], x["rows"]
                    k.v(lambda e: e.reciprocal(x["st"][rows, 3:4], x["st"][rows, 2:3]), [bsm[p]], [bsm[p]])
                for x in U:
                    p, rows = x["p"], x["rows"]
                    k.v(lambda e: e.tensor_scalar(x["pb"][rows, :], x["pe"][rows, :], x["st"][rows, 3:4], None, ALU.mult), [bsm[p]], [bsm[p]])
                for x in U:
                    p, rows, hb = x["p"], x["rows"], x["hb"]
                    for j, (col, vt, bvt, ti_) in enumerate(x["chunks"]):
                        k.tr(x["ptb"][:, j, :], x["pb"][rows, col:col + 128], self.identb[rows, hb:hb + 64], [bsm[p], self.bidentb],
                             [bptb4[p]])
                for x in U:
                    p = x["p"]
                    k.v(lambda e: e.tensor_copy(pT[p], x["ptb"]), [bptb4[p]], [bpT[p]])
                for x in U:
                    p = x["p"]
                    for j, (col, vt, bvt, ti_) in enumerate(x["chunks"]):
                        k.mm(x["po"], vt[:, ti_, h * 64:(h + 1) * 64], pT[p][:, j, :], j == 0, j == 5, [bvt[ti_], bpT[p]], [bpo_[p]])
                for x in U:
                    p, r = x["p"], x["r"]
                    k.act(atT[pr:pr + 64, m, r * 64:(r + 1) * 64], x["po"], AF.Copy, [bpo_[p]], [bat[x["qtile"]]])
            k.barrier()

    def moe_layer(self, l, t_end):
        k, I = self.k, self.I
        nbl = self.nbl
        ntl = t_end // 128
        NSH = self.NSH
        with ExitStack() as esr:
            rw, brw = self.load_const(esr, "rw", I["router_w"][l].rearrange("(c p) n -> p c n", p=128), [128, 8, E])
            rb, brb = self.load_const(esr, "rb", I["router_b"][l].to_broadcast([128, E]), [128, E])
            R = {}
            for nm, shp in (("sc", [128, E]), ("bi", [128, E]), ("m8", [128, 8, 8]), ("gs", [128, 8]), ("g8", [128, 8]),
                            ("pen", [128, 8]), ("mk", [128, E]), ("t8", [128, 8]), ("s01", [128, E]), ("gd", [128, E]),
                            ("gsum", [128, 2]), ("slotm", [128, E]), ("s8", [128, 8]), ("junk", [128, E]), ("selacc", [128, E])):
                R[nm] = self.sb(esr, "r" + nm, shp, F32)
            s01b = self.sb(esr, "rs01b", [128, E], BF16)
            saccb = self.sb(esr, "rsaccb", [128, E], BF16)
            bR = k.buf("rout")
            bsacc = k.buf("selacc")
            htm = [self.sb(esr, "htm", [128, D], BF16) for _ in range(2)]
            bhtm = k.bufs(2, "htm")
            k.v(lambda e: e.memset(R["selacc"], 0.0), [], [bsacc])
            k.v(lambda e: e.memset(saccb, 0.0), [], [bsacc])
            cnt = [0]
            for b in range(nbl):
                def route(tile, ti, ps, bps, h32, bh32, b=b):
                    g = b * NT + tile
                    k.act(R["sc"], ps, AF.Sigmoid, [bps], [bR])
                    k.v(lambda e: e.tensor_tensor(R["bi"], R["sc"], rb, ALU.add), [bR, brb], [bR])
                    for gg in range(8):
                        k.v(lambda e: e.max(out=R["m8"][:, gg, :], in_=R["bi"][:, gg * 32:(gg + 1) * 32]), [bR], [bR])
                    k.v(lambda e: e.tensor_tensor(R["gs"], R["m8"][:, :, 0], R["m8"][:, :, 1], ALU.add), [bR], [bR])
                    k.v(lambda e: e.max(out=R["g8"], in_=R["gs"]), [bR], [bR])
                    k.v(lambda e: e.tensor_scalar(R["pen"], R["gs"], R["g8"][:, 3:4], None, ALU.is_ge), [bR], [bR])
                    k.v(lambda e: e.tensor_scalar(R["pen"], R["pen"], -1.0, 1.0e4, ALU.add, ALU.mult), [bR], [bR])
                    for gg in range(8):
                        k.v(lambda e: e.tensor_scalar(R["mk"][:, gg * 32:(gg + 1) * 32], R["bi"][:, gg * 32:(gg + 1) * 32],
                                                      R["pen"][:, gg:gg + 1], None, ALU.add), [bR], [bR])
                    k.v(lambda e: e.max(out=R["t8"], in_=R["mk"]), [bR], [bR])
                    k.v(lambda e: e.tensor_scalar(R["s01"], R["mk"], R["t8"][:, 7:8], None, ALU.is_ge), [bR], [bR])
                    k.v(lambda e: e.tensor_tensor(R["gd"], R["s01"], R["sc"], ALU.mult), [bR], [bR])
                    k.v(lambda e: e.reduce_sum(R["gsum"][:, 0:1], R["gd"], AX.X), [bR], [bR])
                    k.v(lambda e: e.reciprocal(R["gsum"][:, 1:2], R["gsum"][:, 0:1]), [bR], [bR])
                    k.v(lambda e: e.tensor_scalar(R["gd"], R["gd"], R["gsum"][:, 1:2], 2.5, ALU.mult, ALU.mult), [bR], [bR])
                    k.v(lambda e: e.tensor_copy(s01b, R["s01"]), [bR], [bR])
                    pp_ = self.PS[2]
                    k.mm(pp_[:, 0:E], self.onesb, saccb, True, False, [self.bones, bsacc], [self.bPS[2]])
                    k.mm(pp_[:, 0:E], self.usb, s01b, False, True, [self.bones, bR], [self.bPS[2]])
                    k.v(lambda e: e.tensor_tensor(R["slotm"], pp_[:, 0:E], self.ebase1, ALU.add), [self.bPS[2]], [bR])
                    k.v(lambda e: e.tensor_tensor(R["slotm"], R["slotm"], R["s01"], ALU.mult), [bR], [bR])
                    k.v(lambda e: e.tensor_tensor(R["selacc"], R["selacc"], R["s01"], ALU.add), [bR, bsacc], [bsacc])
                    k.v(lambda e: e.tensor_copy(saccb, R["selacc"]), [bsacc], [bsacc])
                    k.v(lambda e: e.max(out=R["s8"], in_=R["slotm"]), [bR], [bR])
                    k.v(lambda e: e.tensor_scalar(self.slotI[:, g, :], R["s8"], -1.0, None, ALU.add), [bR], [self.bslot[g]])
                    if self.dbg_out is not None:
                        k.v(lambda e: e.tensor_copy(self.s8all[:, g, :], R["s8"]), [bR], [self.bslot[g]])
                    for kk in range(8):
                        k.v(lambda e: e.scalar_tensor_tensor(R["junk"], R["slotm"], R["s8"][:, kk:kk + 1], R["gd"], ALU.is_equal, ALU.mult,
                                                             accum_out=self.gate8[:, g, kk:kk + 1]), [bR], [self.bslot[g], bR])
                    p = cnt[0] % 2
                    cnt[0] += 1
                    pa, pb_ = self.PS[0], self.PS[1]
                    for c in range(8):
                        pst = (pa if c < 4 else pb_)[:, (c % 4) * 128:(c % 4 + 1) * 128]
                        k.tr(pst, h32[:, c, ti * 128:(ti + 1) * 128], self.ident, [bh32, self.bident], [self.bPS[c // 4]])
                    k.act(htm[p][:, 0:512], pa, AF.Copy, [self.bPS[0]], [bhtm[p]])
                    k.v(lambda e: e.tensor_copy(htm[p][:, 512:1024], pb_), [self.bPS[1]], [bhtm[p]])
                    for kk in range(8 if MDBG >= 2 else 0):
                        k.idma(self.xs, bass.IndirectOffsetOnAxis(ap=self.slotI[:, g, kk:kk + 1], axis=0), htm[p], None,
                               [bhtm[p], self.bslot[g]], [k.buf("xsw")])
                    k.dma("sp", self.xsh[g * 128:(g + 1) * 128, :], htm[p], [bhtm[p]], [k.buf("xsw")])

                self.norm_mod(None, None, b, l, 1, t_end, router=(rw, brw, route))
            k.barrier()

        if MDBG < 3:
            return
        with ExitStack() as es:
            Wgu = [self.sb(es, "wgu", [128, 8, 512], BF16) for _ in range(2)]
            Wd = [self.sb(es, "wd", [128, 2, D], BF16) for _ in range(2)]
            bWe = k.bufs(2, "we")
            xtm = [self.sb(es, "extm", [128, D], BF16) for _ in range(2)]
            bxtm = k.bufs(2, "extm")
            xTe = [self.sb(es, "exT", [128, 8, 512], BF16) for _ in range(2)]
            bxTe = k.bufs(2, "exT")
            sgl = self.sb(es, "msg", [128, 512], F32)
            bsgl = k.buf("msg")
            hid = [[self.sb(es, "mhid", [128, 512], BF16) for _ in range(2)] for _ in range(2)]
            bhid = k.bufs(2, "mhid")
            ytm = [self.sb(es, "eytm", [128, D], F32) for _ in range(2)]
            bytm = k.bufs(2, "eytm")
            W32 = [[self.sb(es, "wg32", [128, 8, 256], F32), self.sb(es, "wu32", [128, 8, 256], F32),
                    self.sb(es, "wd32", [128, 2, D], F32)] for _ in range(2)]
            bW32 = k.bufs(2, "w32")
            elist = list(self.experts) + [E]

            def wsrc(e_):
                if e_ == E:
                    return I["ws_gate"][l], I["ws_up"][l], I["ws_down"][l]
                return I["w_gate"][l, e_], I["w_up"][l, e_], I["w_down"][l, e_]

            def stage(ei):
                p_ = ei % 2
                sg_, su_, sd_ = wsrc(elist[ei])
                k.dma("sp", W32[p_][0], sg_.rearrange("(c p) n -> p c n", p=128), [], [bW32[p_]])
                k.dma("sp", W32[p_][1], su_.rearrange("(c p) n -> p c n", p=128), [], [bW32[p_]])
                k.dma("sp", W32[p_][2], sd_.rearrange("(c p) n -> p c n", p=128), [], [bW32[p_]])

            def cast(ei):
                p_ = ei % 2
                k.act(Wgu[p_][:, :, 0:256], W32[p_][0], AF.Copy, [bW32[p_]], [bWe[p_]])
                k.v(lambda e: e.tensor_copy(Wgu[p_][:, :, 256:512], W32[p_][1]), [bW32[p_]], [bWe[p_]])
                k.act(Wd[p_][:, 0, :], W32[p_][2][:, 0, :], AF.Copy, [bW32[p_]], [bWe[p_]])
                k.v(lambda e: e.tensor_copy(Wd[p_][:, 1, :], W32[p_][2][:, 1, :]), [bW32[p_]], [bWe[p_]])

            stage(0)
            cast(0)
            if len(elist) > 1:
                stage(1)
            items = []
            for ei, e_ in enumerate(elist):
                if e_ == E:
                    sblocks = []
                    for b in range(nbl):
                        sblocks += [(b * T + t0, n) for (t0, n) in blocks(0, t_end, 512)]
                else:
                    sblocks = [(e_ * CAP, CAP)]
                for bi_, (r0, n) in enumerate(sblocks):
                    items.append((ei, e_, r0, n, bi_ == len(sblocks) - 1))
            cnt = {"xq": 0, "yq": 0, "tq": 0}

            def prefetch_x(j):
                ei, e_, r0, n, _ = items[j]
                xp = j % 2
                xsrc = self.xsh if e_ == E else self.xs
                for st in range(n // 128):
                    q = cnt["xq"] % 2
                    cnt["xq"] += 1
                    k.dma("pool", xtm[q], xsrc[r0 + st * 128:r0 + (st + 1) * 128, :], [], [bxtm[q]])
                    for c in range(8):
                        k.tr(self.PSB[:, c * 128:(c + 1) * 128], xtm[q][:, c * 128:(c + 1) * 128], self.identb,
                             [bxtm[q], self.bidentb], [self.bPS[7]])
                    k.act(xTe[xp][:, :, st * 128:(st + 1) * 128], self.PSB.rearrange("p (c t) -> p c t", c=8), AF.Copy,
                          [self.bPS[7]], [bxTe[xp]])

            prefetch_x(0)
            for j, (ei, e_, r0, n, last) in enumerate(items):
                p = ei % 2
                xp = j % 2
                nst = n // 128
                for m in range(2):
                    self.lin_fm(self.PS[m], self.bPS[m], Wgu[p], bWe[p], m * 128, 128, xTe[xp], [bxTe[xp]], 0, n)
                    self.lin_fm(self.PS[2 + m], self.bPS[2 + m], Wgu[p], bWe[p], 256 + m * 128, 128, xTe[xp], [bxTe[xp]], 0, n)
                for m in range(2):
                    k.act(sgl[:, 0:n], self.PS[m][:, 0:n], AF.Silu, [self.bPS[m]], [bsgl])
                    k.v(lambda e: e.tensor_tensor(hid[xp][m][:, 0:n], sgl[:, 0:n], self.PS[2 + m][:, 0:n], ALU.mult),
                        [bsgl, self.bPS[2 + m]], [bhid[xp]])
                if last:
                    if ei + 1 < len(elist):
                        cast(ei + 1)
                    if ei + 2 < len(elist):
                        stage(ei + 2)
                if j + 1 < len(items):
                    prefetch_x(j + 1)
                for st in range(nst):
                    q = cnt["tq"] % 2
                    cnt["tq"] += 1
                    for hf in range(2):
                        py = self.PS[4 + cnt["yq"] % 3]
                        bpy = self.bPS[4 + cnt["yq"] % 3]
                        cnt["yq"] += 1
                        for m in range(2):
                            k.mm(py, hid[xp][m][:, st * 128:(st + 1) * 128], Wd[p][:, m, hf * 512:(hf + 1) * 512], m == 0, m == 1,
                                 [bhid[xp], bWe[p]], [bpy])
                        if hf == 0:
                            k.act(ytm[q][:, 0:512], py, AF.Copy, [bpy], [bytm[q]])
                        else:
                            k.v(lambda e: e.tensor_copy(ytm[q][:, 512:1024], py), [bpy], [bytm[q]])
                    rr = slice(r0 + st * 128, r0 + (st + 1) * 128)
                    if e_ == E:
                        k.dma("sp", self.ysh_d[rr, :], ytm[q], [bytm[q]], [k.buf("ysw")])
                    else:
                        for hf in range(2):
                            k.dma("sp", self.ys2[hf][rr, :], ytm[q][:, hf * 512:(hf + 1) * 512], [bytm[q]], [k.buf("ysw")])
            k.barrier()

        if MDBG < 4:
            return
        with ExitStack() as es:
            yg = [self.sb(es, "yg", [128, 8, D], F32) for _ in range(2)]
            byg = k.bufs(2, "yg")
            ysh = [self.sb(es, "ysh", [128, D], F32) for _ in range(2)]
            bysh = k.bufs(2, "ysh")
            acc = self.sb(es, "pacc", [128, D], F32)
            bacc = k.buf("pacc")
            ssq = self.sb(es, "pssq", [128, 2], F32)
            bss = k.buf("pssq")
            junk = self.sb(es, "pjunk", [128, D], F32)
            yn = self.sb(es, "pyn", [128, D], F32)
            byn = k.buf("pyn")
            xb = [self.sb(es, "px", [128, 8, 128], F32) for _ in range(2)]
            bxb = k.bufs(2, "px")
            it = 0
            for b in range(nbl):
                for tile in range(ntl):
                    g = b * NT + tile
                    p = it % 2
                    it += 1
                    v = b if tile < 16 else 2
                    G = self.modvec(l, 1, 2, v)
                    for kk in range(8):
                        for hf in range(2):
                            k.idma(yg[p][:, kk, hf * 512:(hf + 1) * 512], None, self.ys2[hf],
                                   bass.IndirectOffsetOnAxis(ap=self.slotI[:, g, kk:kk + 1], axis=0), [self.bslot[g]], [byg[p]])
                    k.dma("sp", ysh[p], self.ysh_d[g * 128:(g + 1) * 128, :], [], [bysh[p]])
                    k.dma("sp", xb[p], self.xres[b][:, :, tile * 128:(tile + 1) * 128], [self.bxres[b][tile]], [bxb[p]])
                    k.v(lambda e: e.scalar_tensor_tensor(acc, yg[p][:, 0, :], self.gate8[:, g, 0:1], ysh[p], ALU.mult, ALU.add),
                        [byg[p], bysh[p], self.bslot[g]], [bacc])
                    for kk in range(1, 8):
                        k.v(lambda e: e.scalar_tensor_tensor(acc, yg[p][:, kk, :], self.gate8[:, g, kk:kk + 1], acc, ALU.mult, ALU.add),
                            [byg[p], self.bslot[g], bacc], [bacc])
                    k.act(junk, acc, AF.Square, [bacc], [bss], accum_out=ssq[:, 0:1])
                    k.act(ssq[:, 1:2], ssq[:, 0:1], AF.Sqrt, [bss], [bss], scale=1.0 / D, bias=RMS_EPS)
                    k.v(lambda e: e.reciprocal(ssq[:, 1:2], ssq[:, 1:2]), [bss], [bss])
                    k.v(lambda e: e.tensor_scalar(yn, acc, ssq[:, 1:2], None, ALU.mult), [bss, bacc], [byn])
                    pa, pb_ = self.PS[2 * p], self.PS[2 * p + 1]
                    for c in range(8):
                        ps = (pa if c < 4 else pb_)[:, (c % 4) * 128:(c % 4 + 1) * 128]
                        k.tr(ps, yn[:, c * 128:(c + 1) * 128], self.ident, [byn, self.bident], [self.bPS[2 * p + c // 4]])
                    for c in range(8):
                        ps = (pa if c < 4 else pb_)[:, (c % 4) * 128:(c % 4 + 1) * 128]
                        k.v(lambda e: e.scalar_tensor_tensor(xb[p][:, c, :], ps, G[:, c:c + 1], xb[p][:, c, :], ALU.mult, ALU.add),
                            [self.bPS[2 * p + c // 4], self.bmod, bxb[p]], [bxb[p]])
                    k.dma("sp", self.xres[b][:, :, tile * 128:(tile + 1) * 128], xb[p], [bxb[p]], [self.bxres[b][tile]])
            k.barrier()


def host_consts():
    ident = np.eye(128, dtype=np.float32)
    s = np.arange(128)[:, None]
    t = np.arange(128)[None, :]
    same = (s // 64) == (t // 64)
    tri = np.stack([np.where(same & (s <= t), -1.0 / 16, 0.0), np.where(same & (s >= t), -1.0 / 16, 0.0)]).astype(np.float32)
    s6 = np.arange(64)[:, None]
    t6 = np.arange(64)[None, :]
    mf = (s6 <= t6).astype(np.float32)
    mb = (s6 >= t6).astype(np.float32)
    amask = np.stack([np.concatenate([mf, mf], 0), np.concatenate([mb, mb], 0)]).astype(np.float32)
    kk = np.arange(128) % 64
    f = kk % 16
    inv = (10000.0 ** (-(np.arange(16, dtype=np.float32)) / 16)).astype(np.float32)
    tt = np.arange(S)
    rows = (tt // 64).astype(np.float32)
    cols = (tt % 64).astype(np.float32)
    pos = np.where((kk < 32)[:, None], rows[None, :], cols[None, :]).astype(np.float32)
    ang = pos * inv[f][:, None]
    cosT = np.cos(ang).astype(np.float32)
    sinT = np.sin(ang).astype(np.float32)
    sign = np.where((kk % 32) < 16, -1.0, 1.0).astype(np.float32)[:, None]
    rope = np.stack([cosT, sinT * sign, cosT, sinT * sign]).astype(np.float32)
    ustrict = (s < t).astype(np.float32)
    ebase1 = (np.arange(E, dtype=np.float32) * CAP + 1.0)[None, :]
    return ident, tri, amask, rope, ustrict, ebase1


def na_bias_table(rpb):
    qc = np.arange(64)
    cstart = np.clip(qc - 8, 0, 64 - 16)
    kc = np.arange(64)
    ok = (kc[None, :] >= cstart[:, None]) & (kc[None, :] < cstart[:, None] + 16)
    idx = np.clip(kc[None, :] - qc[:, None] + 15, 0, 30)
    g = rpb[:, :, idx]
    g = np.transpose(g, (0, 2, 1, 3))
    out = np.where(ok[None, :, None, :], g, np.float32(NEG)).astype(np.float32)
    return np.ascontiguousarray(out.reshape(16, 64, 960))


def fm(vec):
    return np.ascontiguousarray(np.asarray(vec, np.float32).reshape(8, 128).T)


def prep_shared(inp):
    f32 = lambda a: np.ascontiguousarray(np.asarray(a, dtype=np.float32))
    ident, tri, amask, rope, ustrict, ebase1 = host_consts()
    w_in = f32(inp["ab_w_in"][0])
    q, kk_, v, g, lrf, lrb, ga, gb = np.split(w_in, np.cumsum([256, 256, 512, 512, 16, 16, 512, 512])[:-1], axis=1)
    i64 = np.arange(64)
    partner = np.where((i64 % 32) < 16, i64 + 16, i64 - 16)
    perm = (np.arange(256) // 64) * 64 + partner[np.arange(256) % 64]
    sh = {
        "ada_w": f32(inp["ada_w"]),
        "ada_bT": np.ascontiguousarray(f32(inp["ada_b"]).reshape(2, 48, 128).transpose(0, 2, 1)),
        "gvec": np.ascontiguousarray(np.stack([np.stack([fm(inp[n][l]) for n in ("g_pre_mix", "g_post_mix", "g_pre_ffn", "g_post_ffn")], 1)
                                               for l in range(2)])),
        "w_qk": np.ascontiguousarray(np.concatenate([q, kk_, q[:, perm], kk_[:, perm]], 1)),
        "w_vg": np.ascontiguousarray(np.concatenate([v, g], 1)),
        "w_lr": np.ascontiguousarray(np.concatenate([lrf, lrb], 1)),
        "w_glu": np.ascontiguousarray(np.concatenate([ga, gb], 1)),
        "ab_w_out": f32(inp["ab_w_out"][0]),
        "dwb_f": np.ascontiguousarray(np.concatenate([f32(inp["gla_dw_f"][0]), f32(inp["gla_db_f"][0])[None]], 0)),
        "dwb_b": np.ascontiguousarray(np.concatenate([f32(inp["gla_dw_b"][0]), f32(inp["gla_db_b"][0])[None]], 0)),
        "gla_g": f32(inp["gla_norm_g"][0])[None],
        "convw": np.ascontiguousarray(f32(inp["conv_w"][0]).reshape(31, 4, 128).transpose(2, 1, 0)),
        "convv": np.ascontiguousarray(np.stack([f32(inp[n][0]).reshape(4, 128).T for n in ("conv_b", "conv_ln_g", "conv_ln_b")], 1)),
        "rope": rope, "tri": tri, "amask": amask, "ident": ident, "ustrict": ustrict, "ebase1": ebase1,
        "na_w_in": f32(inp["na_w_in"][0]),
        "na_w_out": f32(inp["na_w_out"][0]),
        "na_bias": na_bias_table(f32(inp["na_rpb"][0])),
        "router_w": f32(inp["moe_router_w"]),
        "router_b": f32(inp["moe_router_b"])[:, None, :],
        "w_gate": f32(inp["moe_w_gate"]), "w_up": f32(inp["moe_w_up"]), "w_down": f32(inp["moe_w_down"]),
        "ws_gate": f32(inp["moe_ws_gate"]), "ws_up": f32(inp["moe_ws_up"]), "ws_down": f32(inp["moe_ws_down"]),
    }
    return sh


def core_inputs(inp, sh, b0, nbl):
    x = np.ascontiguousarray(np.asarray(inp["x"], np.float32)[b0:b0 + nbl])
    ctx = np.ascontiguousarray(np.asarray(inp["ctx"], np.float32)[b0:b0 + nbl])
    c = np.asarray(inp["c"], np.float32)
    cols = [c[b0 + j] if j < nbl else np.zeros(D, np.float32) for j in range(2)] + [np.asarray(inp["c_ctx"], np.float32), np.zeros(D, np.float32)]
    cvec = np.ascontiguousarray(np.stack([fm(v) for v in cols], axis=2))
    m = dict(sh)
    m.update({"x": x, "ctx": ctx, "cvec": cvec})
    return m


_PROG = {}


def kernel(**inputs):
    if "full" not in _PROG:
        _PROG["full"] = Prog()
    prog = _PROG["full"]
    sh = prep_shared(inputs)
    in_maps = [core_inputs(inputs, sh, 2 * c, 2) for c in range(8)]
    res = run_bass_kernel_spmd(prog.nc, in_maps, core_ids=list(range(8)))
    return np.concatenate([r["out"] for r in res.results], axis=0).astype(np.float32)
```
